# Optimizing a Trainium2 kernel written in Bass

```python
import jax, jax.numpy as jnp
from jax import lax
import numpy as np

D_MODEL = 1024
BATCH = 2
SEQ = 8192
DEPTH = 4
DEC_BATCH = 32
DEC_SEQ = 32
PAST_LEN = 1024

CHUNK = 64
EPS = 1e-6
N_HEADS = 8
QK_NOPE = 64
QK_ROPE = 32
V_HEAD = 64
Q_LORA = 512
KV_LORA = 256
ROPE_THETA = 10000.0
Q_BLOCK = 128
ATTN_SCALE = (QK_NOPE + QK_ROPE) ** -0.5
NEG_INF = -1e30
POOL_WINDOWS = (2, 4, 8, 16)
POOL_GROUP = 128
POOL_W = POOL_GROUP * len(POOL_WINDOWS)
POOL_HIST = max(POOL_WINDOWS) - 1
CONV_W = 512
CONV_K = 3
BRANCH_W = 512
N_BRANCH = 3
COL_SIZES = (Q_LORA, KV_LORA, QK_ROPE, POOL_W, CONV_W, CONV_W, CONV_W, N_BRANCH * D_MODEL)
D_IN = sum(COL_SIZES)
PEER_HEADS = 8
N_KEYS = 128
N_EXPERTS = N_KEYS * N_KEYS
D_KEY = 256
PEER_TOPK = 16
TOKEN_BLOCK = 128

kernel_name = 'hybrid_mla_pool_conv_peer_stream_step'


def _rmsnorm(x, g):
    xf = x.astype(jnp.float32)
    y = xf * lax.rsqrt(jnp.mean(xf * xf, axis=-1, keepdims=True) + EPS)
    return (y * g.astype(jnp.float32)).astype(x.dtype)


def _modulate(x, g, shift, scale):
    return _rmsnorm(x, g) * (1 + scale[:, None, :]) + shift[:, None, :]


def _rope(x, pos):
    half = x.shape[-1] // 2
    inv = ROPE_THETA ** (-jnp.arange(half, dtype=jnp.float32) / half)
    ang = pos.astype(jnp.float32)[:, None] * inv[None, :]
    ang = ang.reshape((ang.shape[0],) + (1,) * (x.ndim - 3) + (half,))
    cos, sin = jnp.cos(ang), jnp.sin(ang)
    xf = x.astype(jnp.float32)
    x1, x2 = xf[..., :half], xf[..., half:]
    return jnp.concatenate([x1 * cos - x2 * sin, x1 * sin + x2 * cos], axis=-1).astype(x.dtype)


def _split_cols(z):
    outs, off = [], 0
    for n in COL_SIZES:
        outs.append(z[..., off:off + n])
        off += n
    return outs


def _attn_core(qn, qr, kn, kr, v, mask):
    s = (jnp.einsum('bqhd,bshd->bhqs', qn, kn).astype(jnp.float32)
         + jnp.einsum('bqhr,bsr->bhqs', qr, kr).astype(jnp.float32)) * ATTN_SCALE
    if mask is not None:
        s = jnp.where(mask[None, None], s, NEG_INF)
    p = jax.nn.softmax(s, axis=-1)
    return jnp.einsum('bhqs,bshd->bqhd', p.astype(v.dtype), v)


def _expand_latent(ckv, w_ukv):
    kv = jnp.einsum('bsl,lhd->bshd', ckv, w_ukv)
    return kv[..., :QK_NOPE], kv[..., QK_NOPE:]


def _mla_prompt(qn, qr, ckv, kr, w_ukv):
    B, T = qn.shape[0], qn.shape[1]
    kn, v = _expand_latent(ckv, w_ukv)
    k_chunk = jnp.arange(T) // CHUNK

    def one_block(i):
        s0 = i * Q_BLOCK
        qn_b = lax.dynamic_slice_in_dim(qn, s0, Q_BLOCK, axis=1)
        qr_b = lax.dynamic_slice_in_dim(qr, s0, Q_BLOCK, axis=1)
        q_chunk = (s0 + jnp.arange(Q_BLOCK)) // CHUNK
        mask = k_chunk[None, :] <= q_chunk[:, None]
        return _attn_core(qn_b, qr_b, kn, kr, v, mask)

    out = lax.map(one_block, jnp.arange(T // Q_BLOCK))
    return jnp.moveaxis(out, 0, 1).reshape(B, T, N_HEADS * V_HEAD)


def _mla_sample(qn, qr, ckv_all, kr_all, w_ukv):
    B, T = qn.shape[0], qn.shape[1]
    kn, v = _expand_latent(ckv_all, w_ukv)
    return _attn_core(qn, qr, kn, kr_all, v, None).reshape(B, T, N_HEADS * V_HEAD)


def _pool_mixer(u, hist, n_hist, w_pool, pool_scale):
    T = u.shape[1]
    ext = jnp.concatenate([hist, u], axis=1)
    extf = ext.astype(jnp.float32)
    cs = jnp.pad(jnp.cumsum(extf, axis=1), ((0, 0), (1, 0), (0, 0)))
    t = jnp.arange(T)
    outs = []
    for g, w in enumerate(POOL_WINDOWS):
        sl = slice(g * POOL_GROUP, (g + 1) * POOL_GROUP)
        hi = cs[:, POOL_HIST + 1:POOL_HIST + 1 + T, sl]
        lo = cs[:, POOL_HIST + 1 - w:POOL_HIST + 1 - w + T, sl]
        cnt = jnp.minimum(t + 1 + n_hist, w).astype(jnp.float32)[None, :, None]
        d = (hi - lo) / cnt - extf[:, POOL_HIST:, sl]
        outs.append(jnp.einsum('btc,cd->btd', d.astype(u.dtype), w_pool[g]))
    return jnp.concatenate(outs, axis=-1) * pool_scale, ext[:, -POOL_HIST:]


def _conv_mixer(b_gate, c_gate, h, hist, w):
    u = c_gate * h
    ext = jnp.concatenate([hist, u], axis=1)
    T = u.shape[1]
    y = w[0] * ext[:, 0:T] + w[1] * ext[:, 1:T + 1] + w[2] * ext[:, 2:T + 2]
    return b_gate * y, ext[:, -(CONV_K - 1):]


def _peer(h, w_pq, sub_keys, u_tab, v_tab):
    n_tok = h.shape[0]
    pad = (-n_tok) % TOKEN_BLOCK
    hb = jnp.pad(h, ((0, pad), (0, 0))).reshape(-1, TOKEN_BLOCK, D_MODEL)
    half = D_KEY // 2
    k1 = sub_keys[0].astype(jnp.float32)
    k2 = sub_keys[1].astype(jnp.float32)

    def one_block(xb):
        q = jnp.einsum('nd,dhk->nhk', xb, w_pq).astype(jnp.float32)
        s1 = jnp.einsum('nhk,mk->nhm', q[..., :half], k1)
        s2 = jnp.einsum('nhk,mk->nhm', q[..., half:], k2)
        v1, i1 = lax.top_k(s1, PEER_TOPK)
        v2, i2 = lax.top_k(s2, PEER_TOPK)
        nb = xb.shape[0]
        cand = (v1[..., :, None] + v2[..., None, :]).reshape(nb, PEER_HEADS, PEER_TOPK * PEER_TOPK)
        cidx = (i1[..., :, None] * N_KEYS + i2[..., None, :]).reshape(nb, PEER_HEADS, PEER_TOPK * PEER_TOPK)
        sc, sel = lax.top_k(cand, PEER_TOPK)
        e = jnp.take_along_axis(cidx, sel, axis=-1)
        g = jax.nn.softmax(sc, axis=-1)
        ue = u_tab[e]
        ve = v_tab[e]
        a = jax.nn.gelu(jnp.einsum('nhkd,nd->nhk', ue, xb).astype(jnp.float32), approximate=False)
        return jnp.einsum('nhk,nhkd->nd', (g * a).astype(ve.dtype), ve)

    out = lax.map(one_block, hb).reshape(-1, D_MODEL)
    return out[:n_tok]


def _trunk(x, c, pos, n_hist, cache_kv, cache_kr, hist_pool, hist_conv, p):
    B, T, _ = x.shape
    new_kv, new_kr, new_pool, new_conv = [], [], [], []
    c_act = jax.nn.silu(c)
    for l in range(DEPTH):
        mod = c_act @ p['w_ada'][l] + p['b_ada'][l]
        sh1, sc1, gt1, sh2, sc2, gt2 = jnp.split(mod, 6, axis=-1)
        h = _modulate(x, p['g_mix'][l], sh1, sc1)
        z = h @ p['w_in'][l]
        cq, ckv_raw, kr_raw, u_pool, b_gate, c_gate, h_conv, g_raw = _split_cols(z)
        q = jnp.einsum('btl,lhd->bthd', _rmsnorm(cq, p['g_q'][l]), p['w_uq'][l])
        q_nope = q[..., :QK_NOPE]
        q_rope = _rope(q[..., QK_NOPE:], pos)
        ckv = _rmsnorm(ckv_raw, p['g_kv'][l])
        k_rope = _rope(kr_raw, pos)
        new_kv.append(ckv)
        new_kr.append(k_rope)
        if cache_kv is None:
            attn = _mla_prompt(q_nope, q_rope, ckv, k_rope, p['w_ukv'][l])
        else:
            attn = _mla_sample(q_nope, q_rope,
                               jnp.concatenate([cache_kv[l], ckv], axis=1),
                               jnp.concatenate([cache_kr[l], k_rope], axis=1),
                               p['w_ukv'][l])
        pool, st_p = _pool_mixer(u_pool, hist_pool[l], n_hist, p['w_pool'][l], p['pool_scale'][l])
        conv, st_c = _conv_mixer(b_gate, c_gate, h_conv, hist_conv[l], p['conv_w'][l])
        new_pool.append(st_p)
        new_conv.append(st_c)
        br = jnp.einsum('btnc,ncd->btnd', jnp.stack([attn, pool, conv], axis=2), p['w_branch'][l])
        gates = jax.nn.sigmoid(g_raw.reshape(B, T, N_BRANCH, D_MODEL))
        mixed = jnp.sum(gates * br, axis=2) @ p['w_out'][l]
        x = x + gt1[:, None, :] * mixed
        h2 = _modulate(x, p['g_ffn'][l], sh2, sc2)
        ffn = _peer(h2.reshape(B * T, D_MODEL), p['peer_wq'][l], p['peer_keys'][l],
                    p['peer_u'][l], p['peer_v'][l]).reshape(B, T, D_MODEL)
        x = x + gt2[:, None, :] * ffn
    y = _rmsnorm(x, p['g_final'])
    return y, jnp.stack(new_kv), jnp.stack(new_kr), jnp.stack(new_pool), jnp.stack(new_conv)


def setup_inputs(seed: int = 0) -> dict:
    key = jax.random.key(seed)
    ks = jax.random.split(key, 32)
    f32 = jnp.float32
    nrm = lambda k, shape, s: jax.random.normal(k, shape, f32) * s
    return {
        'x_prompt': nrm(ks[0], (BATCH, SEQ, D_MODEL), 1.0),
        'x_sample': nrm(ks[1], (DEC_BATCH, DEC_SEQ, D_MODEL), 1.0),
        'cache_kv_latent': nrm(ks[2], (DEPTH, DEC_BATCH, PAST_LEN, KV_LORA), 1.0),
        'cache_k_rope': nrm(ks[3], (DEPTH, DEC_BATCH, PAST_LEN, QK_ROPE), 1.0),
        'state_pool': nrm(ks[4], (DEPTH, DEC_BATCH, POOL_HIST, POOL_W), 1.0),
        'state_conv': nrm(ks[5], (DEPTH, DEC_BATCH, CONV_K - 1, CONV_W), 1.0),
        'c_prompt': nrm(ks[6], (BATCH, D_MODEL), 1.0),
        'c_sample': nrm(ks[7], (DEC_BATCH, D_MODEL), 1.0),
        'w_ada': nrm(ks[8], (DEPTH, D_MODEL, 6 * D_MODEL), 0.5 * D_MODEL ** -0.5),
        'b_ada': nrm(ks[9], (DEPTH, 6 * D_MODEL), 0.01),
        'g_mix': 1.0 + nrm(ks[10], (DEPTH, D_MODEL), 0.05),
        'w_in': nrm(ks[11], (DEPTH, D_MODEL, D_IN), D_MODEL ** -0.5),
        'g_q': 1.0 + nrm(ks[12], (DEPTH, Q_LORA), 0.05),
        'w_uq': nrm(ks[13], (DEPTH, Q_LORA, N_HEADS, QK_NOPE + QK_ROPE), Q_LORA ** -0.5),
        'g_kv': 1.0 + nrm(ks[14], (DEPTH, KV_LORA), 0.05),
        'w_ukv': nrm(ks[15], (DEPTH, KV_LORA, N_HEADS, QK_NOPE + V_HEAD), KV_LORA ** -0.5),
        'w_pool': nrm(ks[16], (DEPTH, len(POOL_WINDOWS), POOL_GROUP, POOL_GROUP), POOL_GROUP ** -0.5),
        'pool_scale': 1.0 + nrm(ks[17], (DEPTH, POOL_W), 0.1),
        'conv_w': nrm(ks[18], (DEPTH, CONV_K, CONV_W), CONV_K ** -0.5),
        'w_branch': nrm(ks[19], (DEPTH, N_BRANCH, BRANCH_W, D_MODEL), BRANCH_W ** -0.5),
        'w_out': nrm(ks[20], (DEPTH, D_MODEL, D_MODEL), D_MODEL ** -0.5),
        'g_ffn': 1.0 + nrm(ks[21], (DEPTH, D_MODEL), 0.05),
        'peer_wq': nrm(ks[22], (DEPTH, D_MODEL, PEER_HEADS, D_KEY), D_MODEL ** -0.5),
        'peer_keys': nrm(ks[23], (DEPTH, 2, N_KEYS, D_KEY // 2), (D_KEY // 2) ** -0.5),
        'peer_u': nrm(ks[24], (DEPTH, N_EXPERTS, D_MODEL), D_MODEL ** -0.5),
        'peer_v': nrm(ks[25], (DEPTH, N_EXPERTS, D_MODEL), PEER_HEADS ** -0.5),
        'g_final': 1.0 + nrm(ks[26], (D_MODEL,), 0.05),
    }


def reference(x_prompt, x_sample, cache_kv_latent, cache_k_rope, state_pool, state_conv,
              c_prompt, c_sample, w_ada, b_ada, g_mix, w_in, g_q, w_uq, g_kv, w_ukv,
              w_pool, pool_scale, conv_w, w_branch, w_out, g_ffn, peer_wq, peer_keys,
              peer_u, peer_v, g_final):
    p = {'w_ada': w_ada, 'b_ada': b_ada, 'g_mix': g_mix, 'w_in': w_in, 'g_q': g_q,
         'w_uq': w_uq, 'g_kv': g_kv, 'w_ukv': w_ukv, 'w_pool': w_pool,
         'pool_scale': pool_scale, 'conv_w': conv_w, 'w_branch': w_branch,
         'w_out': w_out, 'g_ffn': g_ffn, 'peer_wq': peer_wq, 'peer_keys': peer_keys,
         'peer_u': peer_u, 'peer_v': peer_v, 'g_final': g_final}
    B, T = x_prompt.shape[0], x_prompt.shape[1]
    zp = jnp.zeros((DEPTH, B, POOL_HIST, POOL_W), x_prompt.dtype)
    zc = jnp.zeros((DEPTH, B, CONV_K - 1, CONV_W), x_prompt.dtype)
    y_prompt, p_kv, p_kr, p_pool, p_conv = _trunk(
        x_prompt, c_prompt, jnp.arange(T), 0, None, None, zp, zc, p)
    Ts = x_sample.shape[1]
    y_sample, s_kv, s_kr, s_pool, s_conv = _trunk(
        x_sample, c_sample, PAST_LEN + jnp.arange(Ts), min(PAST_LEN, POOL_HIST),
        cache_kv_latent, cache_k_rope, state_pool, state_conv, p)
    return (y_prompt, y_sample, p_kv, p_kr, p_pool, p_conv, s_kv, s_kr, s_pool, s_conv)
```

```python
import contextlib
import numpy as np
import concourse.bass as bass
import concourse.mybir as mybir
from concourse.bass_utils import run_bass_kernel_spmd

F32 = mybir.dt.float32
BF16 = mybir.dt.bfloat16
U32 = mybir.dt.uint32
AF = mybir.ActivationFunctionType
ALU = mybir.AluOpType
AX = mybir.AxisListType

D = 1024
DEPTH = 4
NSS = 4
TS = 32
PAST = 1024
NH = 8
D_IN = 5920
EPS = 1e-6
ATTN_SCALE = 96.0 ** -0.5
NEG = -1e30


def C(*a, **k):
    return (a, k)


class Buf:
    __slots__ = ("name", "w", "r")

    def __init__(self, name=""):
        self.name = name
        self.w = None
        self.r = []


class Prog:
    ENGS = ["tensor", "vector", "scalar", "gpsimd", "sync"]

    def __init__(self, nc, nrot=24, nslot=40):
        self.nc = nc
        ndma = nrot + nslot
        self.nrot = nrot
        self.nslot = nslot
        self.nslot_used = 0
        self.slots = {}
        self.ops = {e: [] for e in self.ENGS}
        self.cnt = {e: 0 for e in self.ENGS}
        self.known = {e: {} for e in self.ENGS}
        self.ndma = ndma
        self.dma_val = [0] * ndma
        self.dma_next = 0
        self.esem = {}
        self.dsem = []

    def _deps(self, eng, reads, writes):
        ev = {}

        def add(e):
            if e is None:
                return
            k, v = e
            if ev.get(k, 0) < v:
                ev[k] = v
        me = ("e", eng)
        for b in reads:
            add(b.w)
        for b in writes:
            if b.w is not None and b.w[0] != me:
                add(b.w)
            for r in b.r:
                if r[0] != me:
                    add(r)
        kn = self.known[eng]
        waits = []
        for k, v in ev.items():
            if kn.get(k, 0) < v:
                kn[k] = v
                waits.append((k, v))
        return waits

    def _commit(self, event, reads, writes):
        for b in reads:
            b.r.append(event)
            if len(b.r) > 64:
                m = {}
                for k, v in b.r:
                    if m.get(k, 0) < v:
                        m[k] = v
                b.r = list(m.items())
        for b in writes:
            b.w = event
            b.r = []

    def op(self, eng, fn, reads=(), writes=()):
        waits = self._deps(eng, reads, writes)
        if eng == "tensor":
            waits = [(k, v) for (k, v) in waits if k != ("e", eng)]
        self.cnt[eng] += 1
        event = (("e", eng), self.cnt[eng])
        self.known[eng][("e", eng)] = max(self.known[eng].get(("e", eng), 0), 0)
        self.ops[eng].append((waits, fn, event))
        self._commit(event, reads, writes)

    def dma(self, eng, fn, reads=(), writes=(), slot=None):
        waits = self._deps(eng, reads, writes)
        if slot is not None:
            if slot not in self.slots:
                assert self.nslot_used < self.nslot
                self.slots[slot] = self.nrot + self.nslot_used
                self.nslot_used += 1
            i = self.slots[slot]
            key = ("d", i)
        else:
            i = self.dma_next
            self.dma_next = (i + 1) % self.nrot
            key = ("d", i)
            if self.dma_val[i] > 0 and self.known[eng].get(key, 0) < self.dma_val[i]:
                self.known[eng][key] = self.dma_val[i]
                waits.append((key, self.dma_val[i]))
        self.dma_val[i] += 16
        event = (key, self.dma_val[i])
        self.ops[eng].append((waits, fn, event))
        self._commit(event, reads, writes)

    def barrier(self):
        allev = {}
        for e in self.ENGS:
            if self.cnt[e] > 0:
                allev[("e", e)] = self.cnt[e]
        for i in range(self.ndma):
            if self.dma_val[i] > 0:
                allev[("d", i)] = self.dma_val[i]
        waits = []
        for k, v in allev.items():
            if k == ("e", "sync"):
                continue
            if self.known["sync"].get(k, 0) < v:
                waits.append((k, v))
        self.cnt["sync"] += 1
        ev = (("e", "sync"), self.cnt["sync"])
        self.ops["sync"].append((waits, ("nop", C()), ev))
        allev[ev[0]] = ev[1]
        for e in self.ENGS:
            if e != "sync":
                self.ops[e].append(([ev], None, None))
            for k, v in allev.items():
                if self.known[e].get(k, 0) < v:
                    self.known[e][k] = v

    def emit(self):
        nc = self.nc
        with contextlib.ExitStack() as st:
            for e in self.ENGS:
                self.esem[e] = st.enter_context(nc.semaphore("s_" + e))
            for i in range(self.ndma):
                self.dsem.append(st.enter_context(nc.semaphore("d_%d" % i)))
            block = st.enter_context(nc.Block())

            def sem_of(k):
                return self.esem[k[1]] if k[0] == "e" else self.dsem[k[1]]

            def make(engname):
                def body(eng):
                    pend = []
                    for waits, fn, event in self.ops[engname]:
                        pend.extend(waits)
                        if fn is None:
                            continue
                        m = {}
                        for k, v in pend:
                            if m.get(k, 0) < v:
                                m[k] = v
                        pend = []
                        items = list(m.items())
                        for k, v in items[:-1]:
                            eng.wait_ge(sem_of(k), v)
                        ins = getattr(eng, fn[0])(*fn[1][0], **fn[1][1])
                        if items:
                            k, v = items[-1]
                            ins._wait_ge(sem_of(k), v)
                        k, v = event
                        ins.then_inc(sem_of(k), 16 if k[0] == "d" else 1)
                    for k, v in pend:
                        eng.wait_ge(sem_of(k), v)
                return body

            block.tensor(make("tensor"))
            block.vector(make("vector"))
            block.scalar(make("scalar"))
            block.gpsimd(make("gpsimd"))
            block.sync(make("sync"))


def build_program(SEQ):
    NTOK = SEQ + NSS * TS
    NKBP = SEQ // 128
    SKS = 9 * 128
    SK = SEQ + NSS * SKS
    NKB = NKBP + NSS * 9
    nc = bass.Bass("TRN2", target_bir_lowering=False)
    P = Prog(nc)

    def din(name, shape, dt=F32):
        return nc.dram_tensor(name, list(shape), dt, kind="ExternalInput").ap()

    def dout(name, shape, dt=F32):
        return nc.dram_tensor(name, list(shape), dt, kind="ExternalOutput").ap()

    def dscr(name, shape, dt):
        return nc.dram_tensor(name, list(shape), dt, kind="Internal").ap()

    x_p = din("x_p", [SEQ, D]); x_s = din("x_s", [NSS * TS, D])
    c_all = din("c_all", [1 + NSS, D])
    ckv_c = din("ckv_c", [DEPTH, NSS, PAST, 256]); kr_c = din("kr_c", [DEPTH, NSS, PAST, 32])
    st_pool = din("st_pool", [DEPTH, NSS, 15, 512]); st_conv = din("st_conv", [DEPTH, NSS, 2, 512])
    w_ada = din("w_ada", [DEPTH, D, 6 * D]); b_ada = din("b_ada", [DEPTH, 6 * D])
    g_mix = din("g_mix", [DEPTH, D]); w_in = din("w_in", [DEPTH, D, D_IN])
    g_q = din("g_q", [DEPTH, 512]); w_uq = din("w_uq", [DEPTH, 512, NH, 96])
    g_kv = din("g_kv", [DEPTH, 256]); w_ukv = din("w_ukv", [DEPTH, 256, NH, 128])
    w_pool = din("w_pool", [DEPTH, 4, 128, 128]); pool_scale = din("pool_scale", [DEPTH, 512])
    conv_w = din("conv_w", [DEPTH, 3, 512]); w_branch = din("w_branch", [DEPTH, 3, 512, D])
    w_out = din("w_out", [DEPTH, D, D]); g_ffn = din("g_ffn", [DEPTH, D])
    peer_wq = din("peer_wq", [DEPTH, D, NH * 256]); peer_keys = din("peer_keys", [DEPTH, 2, 128, 128])
    peer_u = din("peer_u", [DEPTH, 16384, D]); peer_v = din("peer_v", [DEPTH, 16384, D])
    g_final = din("g_final", [D])
    rope_tab = din("rope_tab", [4, 96, NTOK]); rc0 = din("rc0", [128, 4, 16])

    y_p = dout("y_p", [SEQ, D]); y_s = dout("y_s", [NSS * TS, D])
    kv_p = dout("kv_p", [DEPTH, SEQ, 256]); kr_p = dout("kr_p", [DEPTH, SEQ, 32])
    pool_p = dout("pool_p", [DEPTH, 15, 512]); conv_p = dout("conv_p", [DEPTH, 2, 512])
    kv_s = dout("kv_s", [DEPTH, NSS * TS, 256]); kr_s = dout("kr_s", [DEPTH, NSS * TS, 32])
    pool_s = dout("pool_s", [DEPTH, NSS, 15, 512]); conv_s = dout("conv_s", [DEPTH, NSS, 2, 512])

    xT_scr = dscr("xT_scr", [128, 8, NTOK], F32)
    KT_scr = dscr("KT_scr", [NH, 96, SK], BF16)
    V_scr = dscr("V_scr", [NH, 128, NKB, 65], BF16)
    uv_scr = dscr("uv_scr", [128, 128, 2 * D], BF16)
    b_uvs = Buf("uv_scr")
    b_xT = Buf("xT_scr"); b_KT = Buf("KT_scr"); b_V = Buf("V_scr"); b_out = Buf("outs")

    blocks = []
    for t0 in range(0, SEQ, 512):
        blocks.append(dict(seq=0, t0=t0, W=min(512, SEQ - t0), off=t0))
    for s in range(NSS):
        blocks.append(dict(seq=1 + s, t0=0, W=TS, off=SEQ + s * TS))

    with contextlib.ExitStack() as st:
        def sb(name, shape, dt):
            return st.enter_context(nc.sbuf_tensor(name, list(shape), dt))

        def pst(name, shape, dt):
            return st.enter_context(nc.psum_tensor(name, list(shape), dt))

        V = lambda fn, r=(), w=(): P.op("vector", fn, r, w)
        A = lambda fn, r=(), w=(): P.op("scalar", fn, r, w)
        G = lambda fn, r=(), w=(): P.op("gpsimd", fn, r, w)
        T = lambda fn, r=(), w=(): P.op("tensor", fn, r, w)
        DS = lambda fn, r=(), w=(), slot=None: P.dma("sync", fn, r, w, slot)
        DG = lambda fn, r=(), w=(), slot=None: P.dma("gpsimd", fn, r, w, slot)

        uid = [0]
        ps = [None] * 8
        pb = [None] * 8
        ps_ctx = []

        def renew_psum():
            while ps_ctx:
                ps_ctx.pop().__exit__(None, None, None)
            for i in range(8):
                uid[0] += 1
                ctx = nc.psum_tensor("ps%d_%d" % (i, uid[0]), [128, 512], F32)
                ps[i] = ctx.__enter__()
                ps_ctx.append(ctx)
                pb[i] = Buf("ps%d" % i)

        def phase():
            P.barrier()
            renew_psum()
        rot = [0]
        tgl = [0]

        def gps():
            i = rot[0]
            rot[0] = (i + 1) % 3
            return ps[i], pb[i]

        ident = sb("ident", [128, 128], F32); identb = sb("identb", [128, 128], BF16)
        iot = sb("iot", [128, 128], F32); pidx = sb("pidx", [128, 1], F32)
        onesb = sb("onesb", [128, 128], BF16); epsc = sb("epsc", [128, 1], F32)
        sel65 = sb("sel65", [128, 64], F32)
        b_const = Buf("const")
        xT = sb("xT", [128, 8, 512], F32); b_x = Buf("xT")
        hT = sb("hT", [128, 8, 512], BF16); b_h = Buf("hT")
        wbuf = [sb("wbuf%d" % i, [128, 8, 512], BF16) for i in range(2)]
        b_wbuf = [Buf("wbuf0"), Buf("wbuf1")]
        wrot = [0]
        cT = sb("cT", [128, 8, 1 + NSS], BF16); cTf = sb("cTf", [128, 8, 1 + NSS], F32); b_cT = Buf("cT")
        modT = sb("modT", [128, 48, 1 + NSS], F32); b_mod = Buf("modT")
        G1 = sb("G1", [128, 8, 1 + NSS], F32); G2 = sb("G2", [128, 8, 1 + NSS], F32)
        vecs = sb("vecs", [128, 64], F32)
        b_vecs = Buf("vecs")
        badaT = sb("badaT", [128, 48], F32)
        wpool = sb("wpool", [128, 4, 128], BF16); b_wpool = Buf("wpool")
        kT12 = sb("kT12", [128, 2, 128], BF16); b_kT12 = Buf("kT12")
        halo_p = sb("halo_p", [128, 4, 16], F32); b_halo_p = Buf("halo_p")
        halo_c = sb("halo_c", [128, 4, 2], F32); b_halo_c = Buf("halo_c")
        stg = sb("stg", [128, 1024], F32); b_stg = Buf("stg")
        stg2 = sb("stg2", [128, 1024], F32); b_stg2 = Buf("stg2")

        class AR:
            def __init__(self):
                self.stack = []

            @property
            def off(self):
                return len(self.stack)

            @off.setter
            def off(self, mark):
                while len(self.stack) > mark:
                    self.stack.pop().__exit__(None, None, None)

            def reset(self):
                self.off = 0

            def alloc(self, name, nelem, dt):
                uid[0] += 1
                ctx = nc.sbuf_tensor("%s_%d" % (name, uid[0]), [128, nelem], dt)
                t = ctx.__enter__()
                self.stack.append(ctx)
                return t[:, 0:nelem], Buf(name)
        ar = AR()

        renew_psum()
        G(("iota", C(iot[:], [[1, 128]], base=0, channel_multiplier=0, allow_small_or_imprecise_dtypes=True)), w=[b_const])
        G(("iota", C(pidx[:], [[0, 1]], base=0, channel_multiplier=1, allow_small_or_imprecise_dtypes=True)), w=[b_const])
        V(("tensor_scalar", C(out=ident[:], in0=iot[:], scalar1=pidx[:, 0:1], scalar2=None, op0=ALU.is_equal)), r=[b_const], w=[b_const])
        V(("tensor_copy", C(out=identb[:], in_=ident[:])), r=[b_const], w=[b_const])
        V(("memset", C(onesb[:], 1.0)), w=[b_const])
        V(("memset", C(epsc[:], EPS)), w=[b_const])
        V(("tensor_scalar", C(out=sel65[:], in0=iot[:, 0:64], scalar1=0.0, scalar2=None, op0=ALU.mult)), r=[b_const], w=[b_const])
        V(("tensor_scalar", C(out=sel65[:], in0=sel65[:], scalar1=pidx[:, 0:1], scalar2=64.0, op0=ALU.add, op1=ALU.is_equal)), r=[b_const], w=[b_const])

        for s_i in range(1 + NSS):
            DS(("dma_start", C(out=cTf[:, :, s_i], in_=c_all[s_i].rearrange("(k p) -> p k", p=128), allow_slow_non_contiguous=True)), w=[b_cT])
        A(("activation", C(out=cT[:], in_=cTf[:], func=AF.Silu)), r=[b_cT], w=[b_cT])

        def load_w(src2d, c0, ncols, nk):
            i = wrot[0]
            wrot[0] = 1 - i
            wt, bw = wbuf[i], b_wbuf[i]
            view = wt[:, 0:nk, 0:ncols]
            DG(("dma_start", C(out=view, in_=src2d.rearrange("(k p) c -> p k c", p=128)[:, :, c0:c0 + ncols])), w=[bw], slot="wbuf%d" % i)
            return wt, bw

        def mm(psap, pbuf, pairs, rbufs):
            n = len(pairs)
            for i, (l, r) in enumerate(pairs):
                T(("matmul", C(psap, lhsT=l, rhs=r, start=(i == 0), stop=(i == n - 1))), r=rbufs, w=[pbuf])

        def rms_stats(src_fn, nk, dim, W, srcbufs, name):
            sq, bsq = ar.alloc(name + "_sq", 2 * 512, BF16)
            rstd, brs = ar.alloc(name + "_rstd", 512, F32)
            p_, pb_ = gps()
            for k in range(nk):
                sqk = sq[:, (k % 2) * 512:(k % 2) * 512 + W]
                A(("activation", C(out=sqk, in_=src_fn(k), func=AF.Square)), r=srcbufs, w=[bsq])
                T(("matmul", C(p_[:, 0:W], lhsT=onesb[:], rhs=sqk, start=(k == 0), stop=(k == nk - 1))), r=[bsq, b_const], w=[pb_])
            A(("activation", C(out=rstd[:, 0:W], in_=p_[:, 0:W], func=AF.Sqrt, scale=1.0 / dim, bias=epsc[:, 0:1])), r=[pb_, b_const], w=[brs])
            V(("reciprocal", C(out=rstd[:, 0:W], in_=rstd[:, 0:W])), r=[brs], w=[brs])
            return rstd, brs

        def transpose_out(src_fn, nk, W, dst_fn, srcbufs, rows_per_k=128, part0=0):
            for t0 in range(0, W, 128):
                n = min(128, W - t0)
                for kg in range(0, nk, 4):
                    ng = min(4, nk - kg)
                    p_, pb_ = gps()
                    for k in range(ng):
                        T(("transpose", C(p_[0:n, k * rows_per_k:(k + 1) * rows_per_k], src_fn(kg + k)[:, t0:t0 + n],
                                                                ident[part0:part0 + rows_per_k, part0:part0 + rows_per_k])), r=srcbufs + [b_const], w=[pb_])
                    tgl[0] = 1 - tgl[0]
                    s_, bs_ = (stg, b_stg) if tgl[0] == 0 else (stg2, b_stg2)
                    A(("copy", C(out=s_[0:n, 0:ng * rows_per_k], in_=p_[0:n, 0:ng * rows_per_k])), r=[pb_], w=[bs_])
                    DS(("dma_start", C(out=dst_fn(t0, n, kg * rows_per_k, (kg + ng) * rows_per_k), in_=s_[0:n, 0:ng * rows_per_k])), r=[bs_], w=[b_out])

        for blk in blocks:
            W, off = blk["W"], blk["off"]
            for t0 in range(0, W, 128):
                n = min(128, W - t0)
                src = (x_p[off + t0:off + t0 + n, :] if blk["seq"] == 0 else x_s[off - SEQ + t0:off - SEQ + t0 + n, :])
                DS(("dma_start", C(out=stg[0:n, :], in_=src)), w=[b_stg])
                for k in range(8):
                    p_, pb_ = gps()
                    T(("transpose", C(p_[:, 0:n], stg[0:n, k * 128:(k + 1) * 128], ident[0:n, 0:n])), r=[b_stg, b_const], w=[pb_])
                    A(("copy", C(out=xT[:, k, t0:t0 + n], in_=p_[:, 0:n])), r=[pb_], w=[b_x])
            DS(("dma_start", C(out=xT_scr[:, :, off:off + W], in_=xT[:, :, 0:W])), r=[b_x], w=[b_xT])

        for l in range(DEPTH):
            phase()
            ar.reset()
            def colload(dst, src1d, n):
                DS(("dma_start", C(out=dst, in_=src1d.rearrange("(k p) -> p k", p=128), allow_slow_non_contiguous=True)), w=[b_vecs])
            colload(vecs[:, 0:8], g_mix[l], 8); colload(vecs[:, 8:16], g_ffn[l], 8)
            colload(vecs[:, 16:20], g_q[l], 4); colload(vecs[:, 20:22], g_kv[l], 2)
            colload(vecs[:, 22:26], pool_scale[l], 4)
            for j in range(3):
                colload(vecs[:, 26 + 4 * j:30 + 4 * j], conv_w[l, j], 4)
            colload(vecs[:, 38:46], g_final, 8)
            colload(badaT[:, :], b_ada[l], 48)
            DG(("dma_start", C(out=wpool[:], in_=w_pool[l].rearrange("g c d -> c g d"))), w=[b_wpool])
            for s2 in range(2):
                DS(("dma_start", C(out=stg[:, 0:128], in_=peer_keys[l, s2])), w=[b_stg])
                p_, pb_ = gps()
                T(("transpose", C(p_[:, 0:128], stg[:, 0:128], ident[:])), r=[b_stg, b_const], w=[pb_])
                A(("copy", C(out=kT12[:, s2, :], in_=p_[:, 0:128])), r=[pb_], w=[b_kT12])
            for g in range(12):
                wt, bw = load_w(w_ada[l], g * 512, 512, 8)
                p_, pb_ = gps()
                for mi in range(4):
                    mm(p_[:, mi * 8:mi * 8 + 1 + NSS], pb_, [(wt[:, k, mi * 128:(mi + 1) * 128], cT[:, k, :]) for k in range(8)], [bw, b_cT])
                V(("tensor_tensor", C(out=modT[:, 4 * g:4 * g + 4, :], in0=p_[:, 0:32].rearrange("p (m s) -> p m s", s=8)[:, :, 0:1 + NSS],
                                                        in1=badaT[:, 4 * g:4 * g + 4].unsqueeze(2).to_broadcast([128, 4, 1 + NSS]), op=ALU.add)),
                  r=[pb_, b_vecs], w=[b_mod])
            for (Gx, c0, m0) in ((G1, 0, 8), (G2, 8, 32)):
                V(("tensor_scalar", C(out=Gx[:], in0=modT[:, m0:m0 + 8, :], scalar1=1.0, scalar2=None, op0=ALU.add)), r=[b_mod], w=[b_mod])
                V(("tensor_tensor", C(out=Gx[:], in0=Gx[:], in1=vecs[:, c0:c0 + 8].unsqueeze(2).to_broadcast([128, 8, 1 + NSS]), op=ALU.mult)), r=[b_mod, b_vecs], w=[b_mod])

            phase(); ar.reset()
            ub = [ar.alloc("pub%d" % i, 1024, BF16) for i in range(2)]
            vb = [ar.alloc("pvb%d" % i, 1024, BF16) for i in range(2)]
            uTp = [ar.alloc("puT%d" % i, 1024, BF16) for i in range(2)]
            ptb = ps[3][:].bitcast(BF16)
            for i in range(128):
                u_, bu_ = ub[i % 2]; v_, bv_ = vb[i % 2]; uT_, buT_ = uTp[i % 2]
                DG(("dma_start", C(out=u_, in_=peer_u[l, i * 128:(i + 1) * 128, :])), w=[bu_], slot="pub%d" % (i % 2))
                DG(("dma_start", C(out=v_, in_=peer_v[l, i * 128:(i + 1) * 128, :])), w=[bv_], slot="pvb%d" % (i % 2))
                for k in range(8):
                    T(("transpose", C(ptb[:, k * 128:(k + 1) * 128], u_[:, k * 128:(k + 1) * 128], identb[:])), r=[bu_, b_const], w=[pb[3]])
                if i % 2 == 0:
                    A(("copy", C(out=uT_, in_=ptb[:, 0:1024])), r=[pb[3]], w=[buT_])
                else:
                    V(("tensor_copy", C(out=uT_, in_=ptb[:, 0:1024])), r=[pb[3]], w=[buT_])
                DS(("dma_start", C(out=uv_scr[i, :, 0:D], in_=uT_)), r=[buT_], w=[b_uvs], slot="puTs%d" % (i % 2))
                DS(("dma_start", C(out=uv_scr[i, :, D:2 * D], in_=v_)), r=[bv_], w=[b_uvs], slot="pvs%d" % (i % 2))
            for s in range(NSS):
                phase(); ar.reset()
                ckvb, b_ckvb = ar.alloc("c_ckvb", 2 * PAST, BF16)
                krT, b_krT = ar.alloc("c_krT", PAST, BF16)
                ckvb3 = ckvb.rearrange("p (k t) -> p k t", k=2)
                for kb in range(PAST // 128):
                    DS(("dma_start", C(out=stg[:, 0:256], in_=ckv_c[l, s, kb * 128:(kb + 1) * 128, :])), w=[b_stg])
                    V(("memset", C(stg2[:, 0:64], 0.0)), w=[b_stg2])
                    DS(("dma_start", C(out=stg2[:, 64:96], in_=kr_c[l, s, kb * 128:(kb + 1) * 128, :])), w=[b_stg2])
                    for k in range(2):
                        p_, pb_ = gps()
                        T(("transpose", C(p_[:, 0:128], stg[:, k * 128:(k + 1) * 128], ident[:])), r=[b_stg, b_const], w=[pb_])
                        A(("copy", C(out=ckvb3[:, k, kb * 128:(kb + 1) * 128], in_=p_[:, 0:128])), r=[pb_], w=[b_ckvb])
                    p_, pb_ = gps()
                    T(("transpose", C(p_[0:96, 0:128], stg2[:, 0:96], ident[:])), r=[b_stg2, b_const], w=[pb_])
                    A(("copy", C(out=krT[64:96, kb * 128:(kb + 1) * 128], in_=p_[64:96, 0:128])), r=[pb_], w=[b_krT])
                wukv, b_wukv = ar.alloc("c_wukv", 2 * 1024, BF16)
                wukv4 = wukv.rearrange("p (k h c) -> p k h c", k=2, h=NH)
                DG(("dma_start", C(out=wukv4, in_=w_ukv[l].rearrange("(k p) h c -> p k h c", p=128))), w=[b_wukv])
                for c0 in range(0, PAST, 512):
                    expand_kv_args = (ckvb3, b_ckvb, krT, b_krT, wukv4, b_wukv, c0, 512, SEQ + s * SKS + c0)
                    _expand_kv(P, ar, T, A, V, DS, gps, KT_scr, V_scr, b_KT, b_V, *expand_kv_args)

            for bi, blk in enumerate(blocks):
                seq, t0b, W, off = blk["seq"], blk["t0"], blk["W"], blk["off"]
                nt = (W + 127) // 128
                kbase = 0 if seq == 0 else SEQ + (seq - 1) * SKS
                kpos = t0b if seq == 0 else PAST
                phase(); ar.reset()
                DS(("dma_start", C(out=xT[:, :, 0:W], in_=xT_scr[:, :, off:off + W])), r=[b_xT], w=[b_x])
                tabs, b_tabs = ar.alloc("tabs", 4 * 512, F32)
                tabs3 = tabs.rearrange("p (a t) -> p a t", a=4)
                QT, b_QT = ar.alloc("QT", NH * 512, BF16)
                QT3 = QT.rearrange("p (h t) -> p h t", h=NH)
                poolT, b_poolT = ar.alloc("poolT", 4 * 512, BF16); pool3 = poolT.rearrange("p (k t) -> p k t", k=4)
                convT, b_convT = ar.alloc("convT", 4 * 512, BF16); conv3 = convT.rearrange("p (k t) -> p k t", k=4)
                markA = ar.off
                DS(("dma_start", C(out=tabs3[64:96, :, 0:W], in_=rope_tab[:, 64:96, off:off + W].rearrange("a p t -> p a t"))), w=[b_tabs])
                rstd, brs = rms_stats(lambda k: xT[:, k, 0:W], 8, D, W, [b_x], "n1")
                tmp, btmp = ar.alloc("n1_tmp", 2 * 512, F32)
                for k in range(8):
                    tk = tmp[:, (k % 2) * 512:(k % 2) * 512 + W]
                    V(("tensor_tensor", C(out=tk, in0=xT[:, k, 0:W], in1=rstd[:, 0:W], op=ALU.mult)), r=[b_x, brs], w=[btmp])
                    A(("activation", C(out=hT[:, k, 0:W], in_=tk, func=AF.Identity, scale=G1[:, k, seq:seq + 1], bias=modT[:, k, seq:seq + 1])),
                      r=[btmp, b_mod], w=[b_h])

                def proj_cols(c0, ncols, evac):
                    wt, bw = load_w(w_in[l], c0, ncols, 8)
                    for mi in range((ncols + 127) // 128):
                        m = min(128, ncols - mi * 128)
                        p_, pb_ = gps()
                        mm(p_[0:m, 0:W], pb_, [(wt[:, k, mi * 128:mi * 128 + m], hT[:, k, 0:W]) for k in range(8)], [bw, b_h])
                        evac(mi, p_, pb_)

                phase(); ar.off = markA
                cqT, b_cq = ar.alloc("cqT", 4 * 512, F32)
                cq3 = cqT.rearrange("p (k t) -> p k t", k=4)
                proj_cols(0, 512, lambda mi, p_, pb_: A(("copy", C(out=cq3[:, mi, 0:W], in_=p_[:, 0:W])), r=[pb_], w=[b_cq]))
                rq, brq = rms_stats(lambda k: cq3[:, k, 0:W], 4, 512, W, [b_cq], "nq")
                cqn, b_cqn = ar.alloc("cqn", 4 * 512, BF16)
                cqn3 = cqn.rearrange("p (k t) -> p k t", k=4)
                for k in range(4):
                    V(("scalar_tensor_tensor", C(out=cqn3[:, k, 0:W], in0=cq3[:, k, 0:W], scalar=vecs[:, 16 + k:17 + k], in1=rq[:, 0:W], op0=ALU.mult, op1=ALU.mult)),
                      r=[b_cq, brq, b_vecs], w=[b_cqn])
                wuq, b_wuq = ar.alloc("wuq", 4 * 768, BF16); wuqs, b_wuqs = ar.alloc("wuqs", 4 * 768, BF16)
                wuq4 = wuq.rearrange("p (k h c) -> p k h c", k=4, h=NH); wuqs4 = wuqs.rearrange("p (k h c) -> p k h c", k=4, h=NH)
                wsrc = w_uq[l].rearrange("(k p) h c -> p k h c", p=128)
                DG(("dma_start", C(out=wuq4, in_=wsrc)), w=[b_wuq])
                for k in range(4):
                    DG(("dma_start", C(out=wuqs4[:, k, :, 0:64], in_=wsrc[:, k, :, 0:64])), w=[b_wuqs])
                    DG(("dma_start", C(out=wuqs4[:, k, :, 64:80], in_=wsrc[:, k, :, 80:96])), w=[b_wuqs])
                    DG(("dma_start", C(out=wuqs4[:, k, :, 80:96], in_=wsrc[:, k, :, 64:80])), w=[b_wuqs])
                rtmp, b_rtmp = ar.alloc("rtmp", 512, F32)
                for h in range(NH):
                    p1, pb1 = gps()
                    mm(p1[0:96, 0:W], pb1, [(wuq4[:, k, h, :], cqn3[:, k, 0:W]) for k in range(4)], [b_wuq, b_cqn])
                    p2, pb2 = gps()
                    mm(p2[0:96, 0:W], pb2, [(wuqs4[:, k, h, :], cqn3[:, k, 0:W]) for k in range(4)], [b_wuqs, b_cqn])
                    A(("activation", C(out=QT3[0:64, h, 0:W], in_=p1[0:64, 0:W], func=AF.Identity, scale=ATTN_SCALE)), r=[pb1], w=[b_QT])
                    V(("tensor_tensor", C(out=rtmp[64:96, 0:W], in0=p1[64:96, 0:W], in1=tabs3[64:96, 0, 0:W], op=ALU.mult)), r=[pb1, b_tabs], w=[b_rtmp])
                    V(("tensor_tensor", C(out=QT3[64:96, h, 0:W], in0=p2[64:96, 0:W], in1=tabs3[64:96, 1, 0:W], op=ALU.mult)), r=[pb2, b_tabs], w=[b_QT])
                    V(("tensor_tensor", C(out=QT3[64:96, h, 0:W], in0=QT3[64:96, h, 0:W], in1=rtmp[64:96, 0:W], op=ALU.add)), r=[b_rtmp, b_QT], w=[b_QT])

                phase(); ar.off = markA
                rtmp, b_rtmp = ar.alloc("rtmp2", 512, F32)
                ckvT, b_ckv = ar.alloc("ckvT", 2 * 512, F32)
                ckv3 = ckvT.rearrange("p (k t) -> p k t", k=2)
                proj_cols(512, 256, lambda mi, p_, pb_: A(("copy", C(out=ckv3[:, mi, 0:W], in_=p_[:, 0:W])), r=[pb_], w=[b_ckv]))
                rk, brk = rms_stats(lambda k: ckv3[:, k, 0:W], 2, 256, W, [b_ckv], "nk")
                ckvb, b_ckvb = ar.alloc("ckvb", 2 * 512, BF16)
                ckvb3 = ckvb.rearrange("p (k t) -> p k t", k=2)
                for k in range(2):
                    V(("scalar_tensor_tensor", C(out=ckv3[:, k, 0:W], in0=ckv3[:, k, 0:W], scalar=vecs[:, 20 + k:21 + k], in1=rk[:, 0:W], op0=ALU.mult, op1=ALU.mult)),
                      r=[b_ckv, brk, b_vecs], w=[b_ckv])
                    A(("copy", C(out=ckvb3[:, k, 0:W], in_=ckv3[:, k, 0:W])), r=[b_ckv], w=[b_ckvb])
                kvdst = (lambda t0, n, a, b: kv_p[l, off + t0:off + t0 + n, a:b]) if seq == 0 else (lambda t0, n, a, b: kv_s[l, off - SEQ + t0:off - SEQ + t0 + n, a:b])
                transpose_out(lambda k: ckv3[:, k, 0:W], 2, W, kvdst, [b_ckv])

                krT, b_krT = ar.alloc("krT", 512, BF16)
                krF, b_krF = ar.alloc("krF", 512, F32)
                wkr, b_wkr = ar.alloc("wkr", 2 * 8 * 96, BF16)
                wkr4 = wkr.rearrange("p (a k c) -> p a k c", a=2, k=8)
                V(("memset", C(wkr[:], 0.0)), w=[b_wkr])
                wi = w_in[l].rearrange("(k p) c -> p k c", p=128)
                DG(("dma_start", C(out=wkr4[:, 0, :, 64:96], in_=wi[:, :, 768:800])), w=[b_wkr])
                DG(("dma_start", C(out=wkr4[:, 1, :, 64:80], in_=wi[:, :, 784:800])), w=[b_wkr])
                DG(("dma_start", C(out=wkr4[:, 1, :, 80:96], in_=wi[:, :, 768:784])), w=[b_wkr])
                p1, pb1 = gps()
                mm(p1[0:96, 0:W], pb1, [(wkr4[:, 0, k, :], hT[:, k, 0:W]) for k in range(8)], [b_wkr, b_h])
                p2, pb2 = gps()
                mm(p2[0:96, 0:W], pb2, [(wkr4[:, 1, k, :], hT[:, k, 0:W]) for k in range(8)], [b_wkr, b_h])
                V(("tensor_tensor", C(out=rtmp[64:96, 0:W], in0=p1[64:96, 0:W], in1=tabs3[64:96, 2, 0:W], op=ALU.mult)), r=[pb1, b_tabs], w=[b_rtmp])
                V(("tensor_tensor", C(out=krF[64:96, 0:W], in0=p2[64:96, 0:W], in1=tabs3[64:96, 3, 0:W], op=ALU.mult)), r=[pb2, b_tabs], w=[b_krF])
                V(("tensor_tensor", C(out=krF[64:96, 0:W], in0=krF[64:96, 0:W], in1=rtmp[64:96, 0:W], op=ALU.add)), r=[b_rtmp, b_krF], w=[b_krF])
                A(("copy", C(out=krT[64:96, 0:W], in_=krF[64:96, 0:W])), r=[b_krF], w=[b_krT])
                krdst = (lambda t0, n, a, b: kr_p[l, off + t0:off + t0 + n, a:b]) if seq == 0 else (lambda t0, n, a, b: kr_s[l, off - SEQ + t0:off - SEQ + t0 + n, a:b])
                transpose_out(lambda k: krF[64:96, 0:W], 1, W, krdst, [b_krF], rows_per_k=32, part0=64)

                wukv, b_wukv = ar.alloc("wukv", 2 * 1024, BF16)
                wukv4 = wukv.rearrange("p (k h c) -> p k h c", k=2, h=NH)
                DG(("dma_start", C(out=wukv4, in_=w_ukv[l].rearrange("(k p) h c -> p k h c", p=128))), w=[b_wukv])
                _expand_kv(P, ar, T, A, V, DS, gps, KT_scr, V_scr, b_KT, b_V, ckvb3, b_ckvb, krT, b_krT, wukv4, b_wukv, 0, W, kbase + kpos)

                phase(); ar.off = markA
                upe, b_upe = ar.alloc("upe", 4 * 528, F32); upe3 = upe.rearrange("p (k t) -> p k t", k=4)
                if t0b == 0:
                    if seq == 0:
                        V(("memset", C(halo_p[:], 0.0)), w=[b_halo_p])
                        V(("memset", C(halo_c[:], 0.0)), w=[b_halo_c])
                    else:
                        V(("memset", C(halo_p[:], 0.0)), w=[b_halo_p])
                        for k in range(4):
                            DS(("dma_start", C(out=halo_p[:, k, 1:16], in_=st_pool[l, seq - 1][:, k * 128:(k + 1) * 128].rearrange("t p -> p t"), allow_slow_non_contiguous=True)), w=[b_halo_p])
                            DS(("dma_start", C(out=halo_c[:, k, :], in_=st_conv[l, seq - 1][:, k * 128:(k + 1) * 128].rearrange("t p -> p t"), allow_slow_non_contiguous=True)), w=[b_halo_c])
                V(("tensor_copy", C(out=upe3[:, :, 0:16], in_=halo_p[:])), r=[b_halo_p], w=[b_upe])
                proj_cols(800, 512, lambda mi, p_, pb_: A(("copy", C(out=upe3[:, mi, 16:16 + W], in_=p_[:, 0:W])), r=[pb_], w=[b_upe]))
                V(("tensor_copy", C(out=halo_p[:], in_=upe3[:, :, W:W + 16])), r=[b_upe], w=[b_halo_p])
                pp, b_pp = ar.alloc("pp", 2 * 528, F32); pp3 = pp.rearrange("p (a t) -> p a t", a=2)
                dT, b_dT = ar.alloc("dT", 512, BF16)
                if seq == 0 and t0b == 0:
                    rc0t, b_rc0 = ar.alloc("rc0t", 64, F32); rc03 = rc0t.rearrange("p (k t) -> p k t", k=4)
                    DS(("dma_start", C(out=rc03, in_=rc0)), w=[b_rc0])
                for g, wdw in enumerate((2, 4, 8, 16)):
                    cur = upe3[:, g, :]
                    step, a = 1, 0
                    L = 16 + W
                    while step < wdw:
                        dst = pp3[:, a, :]
                        V(("tensor_tensor", C(out=dst[:, step:L], in0=cur[:, step:L], in1=cur[:, 0:L - step], op=ALU.add)),
                          r=[b_upe, b_pp], w=[b_pp])
                        cur = dst
                        step *= 2
                        a = 1 - a
                    V(("scalar_tensor_tensor", C(out=dT[:, 0:W], in0=cur[:, 16:16 + W], scalar=1.0 / wdw, in1=upe3[:, g, 16:16 + W], op0=ALU.mult, op1=ALU.subtract)),
                      r=[b_pp, b_upe], w=[b_dT])
                    if seq == 0 and t0b == 0:
                        V(("tensor_tensor", C(out=pp3[:, 1 - a if False else a, 0:16], in0=cur[:, 16:32], in1=rc03[:, g, :], op=ALU.mult)), r=[b_pp, b_rc0], w=[b_pp])
                        V(("tensor_tensor", C(out=dT[:, 0:16], in0=pp3[:, a, 0:16], in1=upe3[:, g, 16:32], op=ALU.subtract)), r=[b_pp, b_upe], w=[b_dT])
                    p_, pb_ = gps()
                    T(("matmul", C(p_[:, 0:W], lhsT=wpool[:, g, :], rhs=dT[:, 0:W], start=True, stop=True)), r=[b_wpool, b_dT], w=[pb_])
                    A(("activation", C(out=pool3[:, g, 0:W], in_=p_[:, 0:W], func=AF.Identity, scale=vecs[:, 22 + g:23 + g])), r=[pb_, b_vecs], w=[b_poolT])
                last_of_seq = (seq != 0) or (t0b + W == SEQ)
                if last_of_seq:
                    pdst = pool_p[l] if seq == 0 else pool_s[l, seq - 1]
                    p_, pb_ = gps()
                    for k in range(4):
                        T(("transpose", C(p_[0:15, k * 128:(k + 1) * 128], upe3[:, k, W + 1:W + 16], ident[:])), r=[b_upe, b_const], w=[pb_])
                    A(("copy", C(out=stg[0:15, 0:512], in_=p_[0:15, 0:512])), r=[pb_], w=[b_stg])
                    DS(("dma_start", C(out=pdst, in_=stg[0:15, 0:512])), r=[b_stg], w=[b_out])

                phase(); ar.off = markA
                uce, b_uce = ar.alloc("uce", 4 * 520, F32); uce3 = uce.rearrange("p (k t) -> p k t", k=4)
                bg, b_bg = ar.alloc("bg", 4 * 512, F32); bg3 = bg.rearrange("p (k t) -> p k t", k=4)
                cg, b_cg = ar.alloc("cg", 4 * 512, F32); cg3 = cg.rearrange("p (k t) -> p k t", k=4)
                V(("tensor_copy", C(out=uce3[:, :, 0:2], in_=halo_c[:])), r=[b_halo_c], w=[b_uce])
                proj_cols(1312, 512, lambda mi, p_, pb_: A(("copy", C(out=bg3[:, mi, 0:W], in_=p_[:, 0:W])), r=[pb_], w=[b_bg]))
                proj_cols(1824, 512, lambda mi, p_, pb_: A(("copy", C(out=cg3[:, mi, 0:W], in_=p_[:, 0:W])), r=[pb_], w=[b_cg]))
                proj_cols(2336, 512, lambda mi, p_, pb_: V(("tensor_tensor", C(out=uce3[:, mi, 2:2 + W], in0=p_[:, 0:W], in1=cg3[:, mi, 0:W], op=ALU.mult)), r=[pb_, b_cg], w=[b_uce]))
                V(("tensor_copy", C(out=halo_c[:], in_=uce3[:, :, W:W + 2])), r=[b_uce], w=[b_halo_c])
                for k in range(4):
                    yk = cg3[:, k, 0:W]
                    V(("tensor_scalar", C(out=yk, in0=uce3[:, k, 0:W], scalar1=vecs[:, 26 + k:27 + k], scalar2=None, op0=ALU.mult)), r=[b_uce, b_vecs, b_cg], w=[b_cg])
                    V(("scalar_tensor_tensor", C(out=yk, in0=uce3[:, k, 1:1 + W], scalar=vecs[:, 30 + k:31 + k], in1=yk, op0=ALU.mult, op1=ALU.add)), r=[b_uce, b_vecs, b_cg], w=[b_cg])
                    V(("scalar_tensor_tensor", C(out=yk, in0=uce3[:, k, 2:2 + W], scalar=vecs[:, 34 + k:35 + k], in1=yk, op0=ALU.mult, op1=ALU.add)), r=[b_uce, b_vecs, b_cg], w=[b_cg])
                    V(("tensor_tensor", C(out=conv3[:, k, 0:W], in0=yk, in1=bg3[:, k, 0:W], op=ALU.mult)), r=[b_cg, b_bg], w=[b_convT])
                if last_of_seq:
                    cdst = conv_p[l] if seq == 0 else conv_s[l, seq - 1]
                    p_, pb_ = gps()
                    for k in range(4):
                        T(("transpose", C(p_[0:2, k * 128:(k + 1) * 128], uce3[:, k, W:W + 2], ident[:])), r=[b_uce, b_const], w=[pb_])
                    A(("copy", C(out=stg2[0:2, 0:512], in_=p_[0:2, 0:512])), r=[pb_], w=[b_stg2])
                    DS(("dma_start", C(out=cdst, in_=stg2[0:2, 0:512])), r=[b_stg2], w=[b_out])

                phase()
                ar.off = markA
                mark = ar.off
                attnT, b_attn = ar.alloc("attnT", NH * 512, BF16); attn3 = attnT.rearrange("p (h t) -> p h t", h=NH)
                nkeys = (t0b + W) if seq == 0 else (PAST + TS)
                nkb = (nkeys + 127) // 128
                kb0 = kbase // 128
                KTh = []; Vh = []
                for i in range(2):
                    a_, b_ = ar.alloc("KTh%d" % i, nkb * 128, BF16); KTh.append((a_, b_))
                    a_, b_ = ar.alloc("Vh%d" % i, nkb * 65, BF16); Vh.append((a_.rearrange("p (k c) -> p k c", c=65), b_))
                PT = [ar.alloc("PT%d" % i, 512, BF16) for i in range(3)]
                osb, b_osb = ar.alloc("osb", 512, F32); rcs, b_rcs = ar.alloc("rcs", 512, F32)
                pti = 0
                for h in range(NH):
                    kt, bkt = KTh[h % 2]; vt, bvt = Vh[h % 2]
                    DS(("dma_start", C(out=kt[0:96, 0:nkeys], in_=KT_scr[h, :, kbase:kbase + nkeys])), r=[b_KT], w=[bkt])
                    DS(("dma_start", C(out=vt[:, 0:nkb, :], in_=V_scr[h, :, kb0:kb0 + nkb, :])), r=[b_V], w=[bvt])
                    po, pbo = ps[4 + h % 2], pb[4 + h % 2]
                    for kb in range(nkb):
                        kk = min(128, nkeys - kb * 128)
                        q0 = 0
                        diag = False
                        if seq == 0 and kb * 128 >= t0b:
                            q0 = kb * 128 - t0b
                            diag = True
                        nq = W - q0
                        p_, pb_ = gps()
                        T(("matmul", C(p_[0:kk, 0:nq], lhsT=kt[0:96, kb * 128:kb * 128 + kk], rhs=QT3[0:96, h, q0:W], start=True, stop=True)),
                          r=[bkt, b_QT], w=[pb_])
                        pt_, bpt_ = PT[pti]; pti = (pti + 1) % 3
                        A(("activation", C(out=pt_[0:kk, 0:nq], in_=p_[0:kk, 0:nq], func=AF.Exp)), r=[pb_], w=[bpt_])
                        if diag:
                            V(("memset", C(pt_[64:128, 0:64], 0.0)), w=[bpt_])
                        T(("matmul", C(po[0:65, q0:W], lhsT=vt[0:kk, kb, :], rhs=pt_[0:kk, 0:nq], start=(kb == 0), stop=(kb == nkb - 1))),
                          r=[bvt, bpt_], w=[pbo])
                    A(("copy", C(out=osb[0:65, 0:W], in_=po[0:65, 0:W])), r=[pbo], w=[b_osb])
                    p_, pb_ = gps()
                    T(("matmul", C(p_[0:64, 0:W], lhsT=sel65[0:65, :], rhs=osb[0:65, 0:W], start=True, stop=True)), r=[b_osb, b_const], w=[pb_])
                    V(("reciprocal", C(out=rcs[0:64, 0:W], in_=p_[0:64, 0:W])), r=[pb_], w=[b_rcs])
                    V(("tensor_tensor", C(out=attn3[0:64, h, 0:W], in0=osb[0:64, 0:W], in1=rcs[0:64, 0:W], op=ALU.mult)), r=[b_osb, b_rcs], w=[b_attn])

                phase()
                ar.off = mark + 1
                mixT, b_mix = ar.alloc("mixT", 8 * 512, BF16); mix3 = mixT.rearrange("p (k t) -> p k t", k=8)
                gts, b_gts = ar.alloc("gts", 3 * 512, F32); gts3 = gts.rearrange("p (n t) -> p n t", n=3)
                wba, b_wba = ar.alloc("wba", 8 * 128, BF16); wba3 = wba.rearrange("p (h c) -> p h c", h=NH)
                wbp, b_wbp = ar.alloc("wbp", 2 * 4 * 128, BF16); wbp4 = wbp.rearrange("p (n k c) -> p n k c", n=2, k=4)
                acc, b_acc = ar.alloc("acc", 512, F32)
                for m in range(8):
                    DG(("dma_start", C(out=wba3[0:64], in_=w_branch[l, 0].rearrange("(h p) c -> p h c", p=64)[:, :, m * 128:(m + 1) * 128])), w=[b_wba])
                    for n2 in range(2):
                        DG(("dma_start", C(out=wbp4[:, n2], in_=w_branch[l, 1 + n2].rearrange("(k p) c -> p k c", p=128)[:, :, m * 128:(m + 1) * 128])), w=[b_wbp])
                    for n in range(3):
                        wt, bw = load_w(w_in[l], 2848 + n * 1024 + m * 128, 128, 8)
                        p_, pb_ = gps()
                        mm(p_[:, 0:W], pb_, [(wt[:, k, 0:128], hT[:, k, 0:W]) for k in range(8)], [bw, b_h])
                        A(("activation", C(out=gts3[:, n, 0:W], in_=p_[:, 0:W], func=AF.Sigmoid)), r=[pb_], w=[b_gts])
                    for n in range(3):
                        p_, pb_ = gps()
                        if n == 0:
                            mm(p_[:, 0:W], pb_, [(wba3[0:64, h, :], attn3[0:64, h, 0:W]) for h in range(NH)], [b_wba, b_attn])
                        else:
                            src3, bsrc = (pool3, b_poolT) if n == 1 else (conv3, b_convT)
                            mm(p_[:, 0:W], pb_, [(wbp4[:, n - 1, k, :], src3[:, k, 0:W]) for k in range(4)], [b_wbp, bsrc])
                        if n == 0:
                            V(("tensor_tensor", C(out=acc[:, 0:W], in0=p_[:, 0:W], in1=gts3[:, 0, 0:W], op=ALU.mult)), r=[pb_, b_gts], w=[b_acc])
                        else:
                            V(("tensor_tensor", C(out=gts3[:, n, 0:W], in0=p_[:, 0:W], in1=gts3[:, n, 0:W], op=ALU.mult)), r=[pb_, b_gts], w=[b_gts])
                            if n == 1:
                                V(("tensor_tensor", C(out=acc[:, 0:W], in0=acc[:, 0:W], in1=gts3[:, 1, 0:W], op=ALU.add)), r=[b_gts, b_acc], w=[b_acc])
                            else:
                                V(("tensor_tensor", C(out=mix3[:, m, 0:W], in0=acc[:, 0:W], in1=gts3[:, 2, 0:W], op=ALU.add)), r=[b_gts, b_acc], w=[b_mix])
                for g in range(2):
                    wt, bw = load_w(w_out[l], g * 512, 512, 8)
                    for mi in range(4):
                        m = g * 4 + mi
                        p_, pb_ = gps()
                        mm(p_[:, 0:W], pb_, [(wt[:, k, mi * 128:(mi + 1) * 128], mix3[:, k, 0:W]) for k in range(8)], [bw, b_mix])
                        V(("scalar_tensor_tensor", C(out=xT[:, m, 0:W], in0=p_[:, 0:W], scalar=modT[:, 16 + m, seq:seq + 1], in1=xT[:, m, 0:W], op0=ALU.mult, op1=ALU.add)),
                          r=[pb_, b_mod, b_x], w=[b_x])

                phase(); ar.reset()
                rstd, brs = rms_stats(lambda k: xT[:, k, 0:W], 8, D, W, [b_x], "n2")
                tmp, btmp = ar.alloc("n2_tmp", 2 * 512, F32)
                for k in range(8):
                    tk = tmp[:, (k % 2) * 512:(k % 2) * 512 + W]
                    V(("tensor_tensor", C(out=tk, in0=xT[:, k, 0:W], in1=rstd[:, 0:W], op=ALU.mult)), r=[b_x, brs], w=[btmp])
                    A(("activation", C(out=hT[:, k, 0:W], in_=tk, func=AF.Identity, scale=G2[:, k, seq:seq + 1], bias=modT[:, 24 + k, seq:seq + 1])),
                      r=[btmp, b_mod], w=[b_h])
                for sb0 in range(0, W, 256):
                    SW = min(256, W - sb0)
                    phase(); ar.reset()
                    GT, b_GT = ar.alloc("GT", 128 * 256, BF16); GT3 = GT.rearrange("p (i n) -> p i n", i=128)
                    qT, b_qT = ar.alloc("qT", 16 * 256, BF16); qT3 = qT.rearrange("p (m t) -> p m t", m=16)
                    for g in range(4):
                        wt, bw = load_w(peer_wq[l], g * 512, 512, 8)
                        for mi in range(4):
                            p_, pb_ = gps()
                            mm(p_[:, 0:SW], pb_, [(wt[:, k, mi * 128:(mi + 1) * 128], hT[:, k, sb0:sb0 + SW]) for k in range(8)], [bw, b_h])
                            A(("copy", C(out=qT3[:, g * 4 + mi, 0:SW], in_=p_[:, 0:SW])), r=[pb_], w=[b_qT])
                    mark_t = ar.off
                    for tt in range(0, SW, 128):
                        n = min(128, SW - tt)
                        phase(); ar.off = mark_t
                        _peer_topk(P, ar, T, A, V, G, gps, ps, pb, n, tt, qT3, b_qT, kT12, b_kT12, ident, iot, b_const, GT3, b_GT)
                    phase(); ar.off = mark_t
                    uvt = [ar.alloc("uvt%d" % i, 2048, BF16) for i in range(3)]
                    gl = [ar.alloc("gl%d" % i, 256, F32) for i in range(2)]
                    wT = [ar.alloc("wT%d" % i, 256, BF16) for i in range(2)]
                    osb2, b_osb2 = ar.alloc("osb2", 1024, F32)
                    nts = (SW + 127) // 128
                    for i in range(128):
                        uv_, buv_ = uvt[i % 3]; g_, bg_ = gl[i % 2]; w_, bw_ = wT[i % 2]
                        uT_ = uv_[:, 0:D]; v_ = uv_[:, D:2 * D]; buT_ = buv_; bv_ = buv_
                        DS(("dma_start", C(out=uv_, in_=uv_scr[i])), r=[b_uvs], w=[buv_], slot="uvt%d" % (i % 3))
                        p_, pb_ = gps()
                        mm(p_[:, 0:SW], pb_, [(uT_[:, k * 128:(k + 1) * 128], hT[:, k, sb0:sb0 + SW]) for k in range(8)], [buT_, b_h])
                        A(("activation", C(out=g_[:, 0:SW], in_=p_[:, 0:SW], func=AF.Gelu)), r=[pb_], w=[bg_])
                        V(("tensor_tensor", C(out=w_[:, 0:SW], in0=g_[:, 0:SW], in1=GT3[:, i, 0:SW], op=ALU.mult)), r=[bg_, b_GT], w=[bw_])
                        for ti in range(nts):
                            n = min(128, SW - ti * 128)
                            for hf in range(2):
                                bk = 4 + ti * 2 + hf
                                T(("matmul", C(ps[bk][0:n, 0:512], lhsT=w_[:, ti * 128:ti * 128 + n], rhs=v_[:, hf * 512:(hf + 1) * 512],
                                                                                             start=(i == 0), stop=(i == 127))), r=[bv_, bw_], w=[pb[bk]])
                    for ti in range(nts):
                        n = min(128, SW - ti * 128)
                        for hf in range(2):
                            bk = 4 + ti * 2 + hf
                            A(("copy", C(out=osb2[0:n, hf * 512:(hf + 1) * 512], in_=ps[bk][0:n, 0:512])), r=[pb[bk]], w=[b_osb2])
                        for k in range(8):
                            p_, pb_ = gps()
                            T(("transpose", C(p_[:, 0:n], osb2[0:n, k * 128:(k + 1) * 128], ident[0:n, 0:n])), r=[b_osb2, b_const], w=[pb_])
                            c0 = sb0 + ti * 128
                            V(("scalar_tensor_tensor", C(out=xT[:, k, c0:c0 + n], in0=p_[:, 0:n], scalar=modT[:, 40 + k, seq:seq + 1],
                                                                                    in1=xT[:, k, c0:c0 + n], op0=ALU.mult, op1=ALU.add)), r=[pb_, b_mod, b_x], w=[b_x])

                if l < DEPTH - 1:
                    DS(("dma_start", C(out=xT_scr[:, :, off:off + W], in_=xT[:, :, 0:W])), r=[b_x], w=[b_xT])
                else:
                    phase(); ar.reset()
                    rstd, brs = rms_stats(lambda k: xT[:, k, 0:W], 8, D, W, [b_x], "nf")
                    yT, b_yT = ar.alloc("yT", 8 * 512, F32); yT3 = yT.rearrange("p (k t) -> p k t", k=8)
                    for k in range(8):
                        V(("scalar_tensor_tensor", C(out=yT3[:, k, 0:W], in0=xT[:, k, 0:W], scalar=vecs[:, 38 + k:39 + k], in1=rstd[:, 0:W], op0=ALU.mult, op1=ALU.mult)),
                          r=[b_x, brs, b_vecs], w=[b_yT])
                    ydst = (lambda t0, n, a, b: y_p[off + t0:off + t0 + n, a:b]) if seq == 0 else (lambda t0, n, a, b: y_s[off - SEQ + t0:off - SEQ + t0 + n, a:b])
                    transpose_out(lambda k: yT3[:, k, 0:W], 8, W, ydst, [b_yT])

        P.barrier()
        ar.reset()
        while ps_ctx:
            ps_ctx.pop().__exit__(None, None, None)
        P.emit()
    return nc


def _expand_kv(P, ar, T, A, V, DS, gps, KT_scr, V_scr, b_KT, b_V, ckvb3, b_ckvb, krT, b_krT, wukv4, b_wukv, c0, W, kslot):
    P.barrier()
    mark = ar.off
    KTb, b_KTb = ar.alloc("KTb", NH * 512, BF16); KTb3 = KTb.rearrange("p (h t) -> p h t", h=NH)
    Vb, b_Vb = ar.alloc("Vb", 4 * NH * 65, BF16); Vb4 = Vb.rearrange("p (t h c) -> p t h c", t=4, h=NH)
    V(("memset", C(Vb[:], 1.0)), w=[b_Vb])
    for h in range(NH):
        p_, pb_ = gps()
        for k in range(2):
            T(("matmul", C(p_[0:64, 0:W], lhsT=wukv4[:, k, h, 0:64], rhs=ckvb3[:, k, c0:c0 + W], start=(k == 0), stop=(k == 1))), r=[b_wukv, b_ckvb], w=[pb_])
        A(("copy", C(out=KTb3[0:64, h, 0:W], in_=p_[0:64, 0:W])), r=[pb_], w=[b_KTb])
        V(("tensor_copy", C(out=KTb3[64:96, h, 0:W], in_=krT[64:96, c0:c0 + W])), r=[b_krT], w=[b_KTb])
    DS(("dma_start", C(out=KT_scr[:, :, kslot:kslot + W].rearrange("h p t -> p h t"), in_=KTb3[0:96, :, 0:W])), r=[b_KTb], w=[b_KT])
    nt = (W + 127) // 128
    for t in range(nt):
        n = min(128, W - t * 128)
        p_, pb_ = gps()
        for k in range(2):
            T(("matmul", C(p_[0:n, 0:512].rearrange("p (h c) -> p h c", h=NH), lhsT=ckvb3[:, k, c0 + t * 128:c0 + t * 128 + n], rhs=wukv4[:, k, :, 64:128],
                                                       start=(k == 0), stop=(k == 1))), r=[b_wukv, b_ckvb], w=[pb_])
        A(("copy", C(out=Vb4[0:n, t, :, 0:64], in_=p_[0:n, 0:512].rearrange("p (h c) -> p h c", h=NH))), r=[pb_], w=[b_Vb])
    kb0 = kslot // 128
    p0 = kslot % 128
    if p0 == 0:
        full = W // 128
        if full > 0:
            for h in range(NH):
                DS(("dma_start", C(out=V_scr[h, :, kb0:kb0 + full, :], in_=Vb4[:, 0:full, h, :])), r=[b_Vb], w=[b_V])
        rem = W - full * 128
        if rem > 0:
            DS(("dma_start", C(out=V_scr[:, 0:rem, kb0 + full, :].rearrange("h p c -> p h c"), in_=Vb4[0:rem, full, :, :])), r=[b_Vb], w=[b_V])
    else:
        assert p0 + W <= 128
        DS(("dma_start", C(out=V_scr[:, p0:p0 + W, kb0, :].rearrange("h p c -> p h c"), in_=Vb4[0:W, 0, :, :])), r=[b_Vb], w=[b_V])
    ar.off = mark


def _peer_topk(P, ar, T, A, V, G, gps, ps, pb, n, tt, qT3, b_qT, kT12, b_kT12, ident, iot, b_const, GT3, b_GT):
    SS, bSS = ar.alloc("SS", 2048, F32)
    S2, bS2 = ar.alloc("S2", 1024, F32); S23 = S2.rearrange("p (h m) -> p h m", h=NH)
    cand, bc = ar.alloc("cand", 2048, F32); cand4 = cand.rearrange("p (h a b) -> p h a b", h=NH, a=16)
    oh, boh = ar.alloc("oh", 2048, F32); oh4 = oh.rearrange("p (h k a) -> p h k a", h=NH, k=16)
    S = []
    for s2 in range(2):
        s_ = SS[:, s2 * 1024:(s2 + 1) * 1024]
        s3 = s_.rearrange("p (h m) -> p h m", h=NH)
        for half in range(2):
            p_, pb_ = gps()
            for hh in range(4):
                h = half * 4 + hh
                T(("matmul", C(p_[0:n, hh * 128:(hh + 1) * 128], lhsT=qT3[:, 2 * h + s2, tt:tt + n], rhs=kT12[:, s2, :], start=True, stop=True)),
                  r=[b_qT, b_kT12], w=[pb_])
            A(("copy", C(out=s_[0:n, half * 512:(half + 1) * 512], in_=p_[0:n, 0:512])), r=[pb_], w=[bSS])
        S.append(s3)
    V16 = []; I16 = []
    for s2 in range(2):
        v_, bv_ = ar.alloc("V16_%d" % s2, 128, F32); i_, bi_ = ar.alloc("I16_%d" % s2, 128, U32); if_, bif_ = ar.alloc("I16f_%d" % s2, 128, F32)
        v3 = v_.rearrange("p (h k) -> p h k", h=NH); i3 = i_.rearrange("p (h k) -> p h k", h=NH)
        s3 = S[s2]
        for h in range(NH):
            V(("max", C(out=v3[0:n, h, 0:8], in_=s3[0:n, h, :])), r=[bSS], w=[bv_])
            V(("match_replace", C(out=S23[0:n, h, :], in_to_replace=v3[0:n, h, 0:8], in_values=s3[0:n, h, :], imm_value=NEG)), r=[bSS, bv_], w=[bS2])
            V(("max", C(out=v3[0:n, h, 8:16], in_=S23[0:n, h, :])), r=[bS2], w=[bv_])
            V(("max_index", C(out=i3[0:n, h, 0:8], in_max=v3[0:n, h, 0:8], in_values=s3[0:n, h, :])), r=[bSS, bv_], w=[bi_])
            V(("max_index", C(out=i3[0:n, h, 8:16], in_max=v3[0:n, h, 8:16], in_values=S23[0:n, h, :])), r=[bS2, bv_], w=[bi_])
        V(("tensor_copy", C(out=if_[0:n, :], in_=i_[0:n, :])), r=[bi_], w=[bif_])
        V16.append((v3, bv_)); I16.append((if_.rearrange("p (h k) -> p h k", h=NH), bif_))
    V(("tensor_tensor", C(out=cand4[0:n], in0=V16[0][0][0:n].unsqueeze(3).to_broadcast([n, NH, 16, 16]), in1=V16[1][0][0:n].unsqueeze(2).to_broadcast([n, NH, 16, 16]), op=ALU.add)),
      r=[V16[0][1], V16[1][1]], w=[bc])
    P.barrier()
    bc2 = Buf("cand2")
    SC, bSC = ar.alloc("SC", 128, F32); SC3 = SC.rearrange("p (h k) -> p h k", h=NH)
    SEL, bSEL = ar.alloc("SEL", 128, U32); SEL3 = SEL.rearrange("p (h k) -> p h k", h=NH)
    candf = cand.rearrange("p (h c) -> p h c", h=NH); cand2f = SS.rearrange("p (h c) -> p h c", h=NH)
    for h in range(NH):
        V(("max", C(out=SC3[0:n, h, 0:8], in_=candf[0:n, h, :])), r=[bc], w=[bSC])
        V(("match_replace", C(out=cand2f[0:n, h, :], in_to_replace=SC3[0:n, h, 0:8], in_values=candf[0:n, h, :], imm_value=NEG)), r=[bc, bSC], w=[bc2])
        V(("max", C(out=SC3[0:n, h, 8:16], in_=cand2f[0:n, h, :])), r=[bc2], w=[bSC])
        V(("max_index", C(out=SEL3[0:n, h, 0:8], in_max=SC3[0:n, h, 0:8], in_values=candf[0:n, h, :])), r=[bc, bSC], w=[bSEL])
        V(("max_index", C(out=SEL3[0:n, h, 8:16], in_max=SC3[0:n, h, 8:16], in_values=cand2f[0:n, h, :])), r=[bc2, bSC], w=[bSEL])
    AB = []
    for which in range(2):
        u_, bu_ = ar.alloc("abu%d" % which, 128, U32); f_, bf_ = ar.alloc("abf%d" % which, 128, F32)
        if which == 0:
            V(("tensor_scalar", C(out=u_[0:n, :], in0=SEL[0:n, :], scalar1=4, scalar2=None, op0=ALU.logical_shift_right)), r=[bSEL], w=[bu_])
        else:
            V(("tensor_scalar", C(out=u_[0:n, :], in0=SEL[0:n, :], scalar1=15, scalar2=None, op0=ALU.bitwise_and)), r=[bSEL], w=[bu_])
        V(("tensor_copy", C(out=f_[0:n, :], in_=u_[0:n, :])), r=[bu_], w=[bf_])
        AB.append((f_.rearrange("p (h k) -> p h k", h=NH), bf_))
    P.barrier()
    bio = Buf("io16")
    io16 = cand; io4 = io16.rearrange("p (h k a) -> p h k a", h=NH, k=16)
    G(("iota", C(io16[:], [[0, 128], [1, 16]], base=0, channel_multiplier=0, allow_small_or_imprecise_dtypes=True)), w=[bio])
    slots = []
    for which in range(2):
        sl_, bsl_ = ar.alloc("slot%d" % which, 128, F32)
        ab3, bab = AB[which]; i3f, bif = I16[which]
        V(("tensor_tensor", C(out=oh4[0:n], in0=io4[0:n], in1=ab3[0:n].unsqueeze(3).to_broadcast([n, NH, 16, 16]), op=ALU.is_equal)), r=[bio, bab], w=[boh])
        V(("tensor_tensor", C(out=oh4[0:n], in0=oh4[0:n], in1=i3f[0:n].unsqueeze(2).to_broadcast([n, NH, 16, 16]), op=ALU.mult)), r=[boh, bif], w=[boh])
        V(("tensor_reduce", C(out=sl_[0:n, :], in_=oh.rearrange("p (s a) -> p s a", a=16)[0:n], axis=AX.X, op=ALU.add)), r=[boh], w=[bsl_])
        slots.append((sl_, bsl_))
    wg, bwg = ar.alloc("wg", 128, F32); wg3 = wg.rearrange("p (h k) -> p h k", h=NH)
    zs, bzs = ar.alloc("zs", 8, F32)
    V(("tensor_tensor", C(out=wg3[0:n], in0=SC3[0:n], in1=SC3[0:n, :, 0:1].to_broadcast([n, NH, 16]), op=ALU.subtract)), r=[bSC], w=[bwg])
    A(("activation", C(out=wg[0:n, :], in_=wg[0:n, :], func=AF.Exp)), r=[bwg], w=[bwg])
    V(("tensor_reduce", C(out=zs[0:n, :], in_=wg3[0:n], axis=AX.X, op=ALU.add)), r=[bwg], w=[bzs])
    V(("reciprocal", C(out=zs[0:n, :], in_=zs[0:n, :])), r=[bzs], w=[bzs])
    V(("tensor_tensor", C(out=wg3[0:n], in0=wg3[0:n], in1=zs[0:n, :].unsqueeze(2).to_broadcast([n, NH, 16]), op=ALU.mult)), r=[bwg, bzs], w=[bwg])
    tr, btr = ar.alloc("trn", 3 * 128, F32); tr3 = tr.rearrange("p (a t) -> p a t", a=3)
    for a, (src, bsrc) in enumerate((slots[0], slots[1], (wg, bwg))):
        p_, pb_ = gps()
        T(("transpose", C(p_[:, 0:n], src[0:n, :], ident[0:n, 0:n])), r=[bsrc, b_const], w=[pb_])
        A(("copy", C(out=tr3[:, a, 0:n], in_=p_[:, 0:n])), r=[pb_], w=[btr])
    A4 = [ar.alloc("A4_%d" % i, 512, BF16) for i in range(2)]
    B4 = [ar.alloc("B4_%d" % i, 512, BF16) for i in range(2)]
    gi = 0
    for t4 in range(0, n, 4):
        pg, pbg = ps[6 + gi % 2], pb[6 + gi % 2]
        a_, ba_ = A4[gi % 2]; b_, bb_ = B4[gi % 2]
        a3 = a_.rearrange("p (t i) -> p t i", t=4); b3 = b_.rearrange("p (t i) -> p t i", t=4)
        gi += 1
        m4 = min(4, n - t4)
        iob = iot[:, :].unsqueeze(1).to_broadcast([128, m4, 128])
        V(("tensor_tensor", C(out=a3[:, 0:m4, :], in0=iob, in1=tr3[:, 0, t4:t4 + m4].unsqueeze(2).to_broadcast([128, m4, 128]), op=ALU.is_equal)), r=[btr, b_const], w=[ba_])
        V(("tensor_tensor", C(out=a3[:, 0:m4, :], in0=a3[:, 0:m4, :], in1=tr3[:, 2, t4:t4 + m4].unsqueeze(2).to_broadcast([128, m4, 128]), op=ALU.mult)), r=[btr, ba_], w=[ba_])
        V(("tensor_tensor", C(out=b3[:, 0:m4, :], in0=iob, in1=tr3[:, 1, t4:t4 + m4].unsqueeze(2).to_broadcast([128, m4, 128]), op=ALU.is_equal)), r=[btr, b_const], w=[bb_])
        for j in range(m4):
            T(("matmul", C(pg[:, j * 128:(j + 1) * 128], lhsT=b3[:, j, :], rhs=a3[:, j, :], start=True, stop=True)), r=[ba_, bb_], w=[pbg])
        A(("copy", C(out=GT3[:, :, tt + t4:tt + t4 + m4].rearrange("p i n -> p n i"), in_=pg[:, 0:m4 * 128].rearrange("p (n i) -> p n i", i=128))), r=[pbg], w=[b_GT])


_CACHE = {}


def _rope_tables(SEQ):
    NTOK = SEQ + NSS * TS
    half = 16
    inv = (10000.0 ** (-np.arange(half, dtype=np.float32) / half)).astype(np.float32)
    pos = np.concatenate([np.arange(SEQ, dtype=np.float32)] + [PAST + np.arange(TS, dtype=np.float32)] * NSS).astype(np.float32)
    ang = (pos[None, :] * inv[:, None]).astype(np.float32)
    cos = np.cos(ang).astype(np.float32); sin = np.sin(ang).astype(np.float32)
    cosf = np.concatenate([cos, cos], 0); sins = np.concatenate([-sin, sin], 0)
    tab = np.zeros((4, 96, NTOK), np.float32)
    sc = np.float32(ATTN_SCALE)
    tab[0, 64:96] = cosf * sc; tab[1, 64:96] = sins * sc
    tab[2, 64:96] = cosf; tab[3, 64:96] = sins
    rc0 = np.zeros((128, 4, 16), np.float32)
    for g, w in enumerate((2, 4, 8, 16)):
        rc0[:, g, :] = 1.0 / np.minimum(np.arange(16) + 1, w)
    return tab, rc0


def kernel(x_prompt, x_sample, cache_kv_latent, cache_k_rope, state_pool, state_conv, c_prompt, c_sample,
           w_ada, b_ada, g_mix, w_in, g_q, w_uq, g_kv, w_ukv, w_pool, pool_scale, conv_w, w_branch, w_out,
           g_ffn, peer_wq, peer_keys, peer_u, peer_v, g_final):
    f = lambda a: np.ascontiguousarray(np.asarray(a, dtype=np.float32))
    x_prompt = f(x_prompt); x_sample = f(x_sample)
    B, SEQ, _ = x_prompt.shape
    DB = x_sample.shape[0]
    ncores = DB // NSS
    assert ncores == 8 and x_sample.shape[1] == TS
    if SEQ not in _CACHE:
        _CACHE[SEQ] = build_program(SEQ)
    nc = _CACHE[SEQ]
    tab, rc0 = _rope_tables(SEQ)
    ckv = f(cache_kv_latent); krc = f(cache_k_rope); sp = f(state_pool); scv = f(state_conv)
    cp = f(c_prompt); cs = f(c_sample)
    shared = {"w_ada": f(w_ada), "b_ada": f(b_ada), "g_mix": f(g_mix), "w_in": f(w_in), "g_q": f(g_q), "w_uq": f(w_uq),
              "g_kv": f(g_kv), "w_ukv": f(w_ukv), "w_pool": f(w_pool), "pool_scale": f(pool_scale), "conv_w": f(conv_w),
              "w_branch": f(w_branch), "w_out": f(w_out), "g_ffn": f(g_ffn), "peer_wq": f(peer_wq).reshape(DEPTH, D, NH * 256),
              "peer_keys": f(peer_keys), "peer_u": f(peer_u), "peer_v": f(peer_v), "g_final": f(g_final),
              "rope_tab": tab, "rc0": rc0}
    in_maps = []
    for c in range(ncores):
        b = c % B
        sl = slice(NSS * c, NSS * c + NSS)
        m = dict(shared)
        m["x_p"] = x_prompt[b]
        m["x_s"] = np.ascontiguousarray(x_sample[sl].reshape(NSS * TS, D))
        m["c_all"] = np.ascontiguousarray(np.concatenate([cp[b:b + 1], cs[sl]], 0))
        m["ckv_c"] = np.ascontiguousarray(ckv[:, sl]); m["kr_c"] = np.ascontiguousarray(krc[:, sl])
        m["st_pool"] = np.ascontiguousarray(sp[:, sl]); m["st_conv"] = np.ascontiguousarray(scv[:, sl])
        in_maps.append(m)
    res = run_bass_kernel_spmd(nc, in_maps, core_ids=list(range(ncores))).results
    y_prompt = np.stack([res[b]["y_p"] for b in range(B)], 0)
    y_sample = np.concatenate([res[c]["y_s"].reshape(NSS, TS, D) for c in range(ncores)], 0)
    p_kv = np.stack([res[b]["kv_p"] for b in range(B)], 1)
    p_kr = np.stack([res[b]["kr_p"] for b in range(B)], 1)
    p_pool = np.stack([res[b]["pool_p"] for b in range(B)], 1)
    p_conv = np.stack([res[b]["conv_p"] for b in range(B)], 1)
    s_kv = np.concatenate([res[c]["kv_s"].reshape(DEPTH, NSS, TS, 256) for c in range(ncores)], 1)
    s_kr = np.concatenate([res[c]["kr_s"].reshape(DEPTH, NSS, TS, 32) for c in range(ncores)], 1)
    s_pool = np.concatenate([res[c]["pool_s"] for c in range(ncores)], 1)
    s_conv = np.concatenate([res[c]["conv_s"] for c in range(ncores)], 1)
    return (y_prompt, y_sample, p_kv, p_kr, p_pool, p_conv, s_kv, s_kr, s_pool, s_conv)
```

```python
import contextlib
import numpy as np
import concourse.bass as bass
import concourse.mybir as mybir
from concourse.bass_utils import run_bass_kernel_spmd

F32 = mybir.dt.float32
BF16 = mybir.dt.bfloat16
U32 = mybir.dt.uint32
AF = mybir.ActivationFunctionType
ALU = mybir.AluOpType
AX = mybir.AxisListType

D = 1024
DEPTH = 4
NSS = 4
TS = 32
PAST = 1024
NH = 8
D_IN = 5920
EPS = 1e-6
ATTN_SCALE = 96.0 ** -0.5
NEG = -1e30


def C(*a, **k):
    return (a, k)


class Buf:
    __slots__ = ("name", "w", "r")

    def __init__(self, name=""):
        self.name = name
        self.w = None
        self.r = []


class Prog:
    ENGS = ["tensor", "vector", "scalar", "gpsimd", "sync"]

    def __init__(self, nc, nrot=24, nslot=40):
        self.nc = nc
        ndma = nrot + nslot
        self.nrot = nrot
        self.nslot = nslot
        self.nslot_used = 0
        self.slots = {}
        self.ops = {e: [] for e in self.ENGS}
        self.cnt = {e: 0 for e in self.ENGS}
        self.known = {e: {} for e in self.ENGS}
        self.ndma = ndma
        self.dma_val = [0] * ndma
        self.dma_next = 0
        self.esem = {}
        self.dsem = []

    def _deps(self, eng, reads, writes):
        ev = {}

        def add(e):
            if e is None:
                return
            k, v = e
            if ev.get(k, 0) < v:
                ev[k] = v
        me = ("e", eng)
        for b in reads:
            add(b.w)
        for b in writes:
            if b.w is not None and b.w[0] != me:
                add(b.w)
            for r in b.r:
                if r[0] != me:
                    add(r)
        kn = self.known[eng]
        waits = []
        for k, v in ev.items():
            if kn.get(k, 0) < v:
                kn[k] = v
                waits.append((k, v))
        return waits

    def _commit(self, event, reads, writes):
        for b in reads:
            b.r.append(event)
            if len(b.r) > 64:
                m = {}
                for k, v in b.r:
                    if m.get(k, 0) < v:
                        m[k] = v
                b.r = list(m.items())
        for b in writes:
            b.w = event
            b.r = []

    def op(self, eng, fn, reads=(), writes=()):
        waits = self._deps(eng, reads, writes)
        if eng == "tensor":
            waits = [(k, v) for (k, v) in waits if k != ("e", eng)]
        self.cnt[eng] += 1
        event = (("e", eng), self.cnt[eng])
        self.known[eng][("e", eng)] = max(self.known[eng].get(("e", eng), 0), 0)
        self.ops[eng].append((waits, fn, event))
        self._commit(event, reads, writes)

    def dma(self, eng, fn, reads=(), writes=(), slot=None):
        waits = self._deps(eng, reads, writes)
        if slot is not None:
            if slot not in self.slots:
                assert self.nslot_used < self.nslot
                self.slots[slot] = self.nrot + self.nslot_used
                self.nslot_used += 1
            i = self.slots[slot]
            key = ("d", i)
        else:
            i = self.dma_next
            self.dma_next = (i + 1) % self.nrot
            key = ("d", i)
            if self.dma_val[i] > 0 and self.known[eng].get(key, 0) < self.dma_val[i]:
                self.known[eng][key] = self.dma_val[i]
                waits.append((key, self.dma_val[i]))
        self.dma_val[i] += 16
        event = (key, self.dma_val[i])
        self.ops[eng].append((waits, fn, event))
        self._commit(event, reads, writes)

    def barrier(self):
        allev = {}
        for e in self.ENGS:
            if self.cnt[e] > 0:
                allev[("e", e)] = self.cnt[e]
        for i in range(self.ndma):
            if self.dma_val[i] > 0:
                allev[("d", i)] = self.dma_val[i]
        waits = []
        for k, v in allev.items():
            if k == ("e", "sync"):
                continue
            if self.known["sync"].get(k, 0) < v:
                waits.append((k, v))
        self.cnt["sync"] += 1
        ev = (("e", "sync"), self.cnt["sync"])
        self.ops["sync"].append((waits, ("nop", C()), ev))
        allev[ev[0]] = ev[1]
        for e in self.ENGS:
            if e != "sync":
                self.ops[e].append(([ev], None, None))
            for k, v in allev.items():
                if self.known[e].get(k, 0) < v:
                    self.known[e][k] = v

    def emit(self):
        nc = self.nc
        with contextlib.ExitStack() as st:
            for e in self.ENGS:
                self.esem[e] = st.enter_context(nc.semaphore("s_" + e))
            for i in range(self.ndma):
                self.dsem.append(st.enter_context(nc.semaphore("d_%d" % i)))
            block = st.enter_context(nc.Block())

            def sem_of(k):
                return self.esem[k[1]] if k[0] == "e" else self.dsem[k[1]]

            def make(engname):
                def body(eng):
                    pend = []
                    for waits, fn, event in self.ops[engname]:
                        pend.extend(waits)
                        if fn is None:
                            continue
                        m = {}
                        for k, v in pend:
                            if m.get(k, 0) < v:
                                m[k] = v
                        pend = []
                        items = list(m.items())
                        for k, v in items[:-1]:
                            eng.wait_ge(sem_of(k), v)
                        ins = getattr(eng, fn[0])(*fn[1][0], **fn[1][1])
                        if items:
                            k, v = items[-1]
                            ins._wait_ge(sem_of(k), v)
                        k, v = event
                        ins.then_inc(sem_of(k), 16 if k[0] == "d" else 1)
                    for k, v in pend:
                        eng.wait_ge(sem_of(k), v)
                return body

            block.tensor(make("tensor"))
            block.vector(make("vector"))
            block.scalar(make("scalar"))
            block.gpsimd(make("gpsimd"))
            block.sync(make("sync"))


def build_program(SEQ):
    NTOK = SEQ + NSS * TS
    NKBP = SEQ // 128
    SKS = 9 * 128
    SK = SEQ + NSS * SKS
    NKB = NKBP + NSS * 9
    nc = bass.Bass("TRN2", target_bir_lowering=False)
    P = Prog(nc)

    def din(name, shape, dt=F32):
        return nc.dram_tensor(name, list(shape), dt, kind="ExternalInput").ap()

    def dout(name, shape, dt=F32):
        return nc.dram_tensor(name, list(shape), dt, kind="ExternalOutput").ap()

    def dscr(name, shape, dt):
        return nc.dram_tensor(name, list(shape), dt, kind="Internal").ap()

    x_p = din("x_p", [SEQ, D]); x_s = din("x_s", [NSS * TS, D])
    c_all = din("c_all", [1 + NSS, D])
    ckv_c = din("ckv_c", [DEPTH, NSS, PAST, 256]); kr_c = din("kr_c", [DEPTH, NSS, PAST, 32])
    st_pool = din("st_pool", [DEPTH, NSS, 15, 512]); st_conv = din("st_conv", [DEPTH, NSS, 2, 512])
    w_ada = din("w_ada", [DEPTH, D, 6 * D]); b_ada = din("b_ada", [DEPTH, 6 * D])
    g_mix = din("g_mix", [DEPTH, D]); w_in = din("w_in", [DEPTH, D, D_IN])
    g_q = din("g_q", [DEPTH, 512]); w_uq = din("w_uq", [DEPTH, 512, NH, 96])
    g_kv = din("g_kv", [DEPTH, 256]); w_ukv = din("w_ukv", [DEPTH, 256, NH, 128])
    w_pool = din("w_pool", [DEPTH, 4, 128, 128]); pool_scale = din("pool_scale", [DEPTH, 512])
    conv_w = din("conv_w", [DEPTH, 3, 512]); w_branch = din("w_branch", [DEPTH, 3, 512, D])
    w_out = din("w_out", [DEPTH, D, D]); g_ffn = din("g_ffn", [DEPTH, D])
    peer_wq = din("peer_wq", [DEPTH, D, NH * 256]); peer_keys = din("peer_keys", [DEPTH, 2, 128, 128])
    peer_u = din("peer_u", [DEPTH, 16384, D]); peer_v = din("peer_v", [DEPTH, 16384, D])
    g_final = din("g_final", [D])
    rope_tab = din("rope_tab", [4, 96, NTOK]); rc0 = din("rc0", [128, 4, 16])

    y_p = dout("y_p", [SEQ, D]); y_s = dout("y_s", [NSS * TS, D])
    kv_p = dout("kv_p", [DEPTH, SEQ, 256]); kr_p = dout("kr_p", [DEPTH, SEQ, 32])
    pool_p = dout("pool_p", [DEPTH, 15, 512]); conv_p = dout("conv_p", [DEPTH, 2, 512])
    kv_s = dout("kv_s", [DEPTH, NSS * TS, 256]); kr_s = dout("kr_s", [DEPTH, NSS * TS, 32])
    pool_s = dout("pool_s", [DEPTH, NSS, 15, 512]); conv_s = dout("conv_s", [DEPTH, NSS, 2, 512])

    xT_scr = dscr("xT_scr", [128, 8, NTOK], F32)
    KT_scr = dscr("KT_scr", [NH, 96, SK], BF16)
    V_scr = dscr("V_scr", [NH, 128, NKB, 65], BF16)
    uv_scr = dscr("uv_scr", [128, 128, 2 * D], BF16)
    b_uvs = Buf("uv_scr")
    b_xT = Buf("xT_scr"); b_KT = Buf("KT_scr"); b_V = Buf("V_scr"); b_out = Buf("outs")

    blocks = []
    for t0 in range(0, SEQ, 512):
        blocks.append(dict(seq=0, t0=t0, W=min(512, SEQ - t0), off=t0))
    for s in range(NSS):
        blocks.append(dict(seq=1 + s, t0=0, W=TS, off=SEQ + s * TS))

    with contextlib.ExitStack() as st:
        def sb(name, shape, dt):
            return st.enter_context(nc.sbuf_tensor(name, list(shape), dt))

        def pst(name, shape, dt):
            return st.enter_context(nc.psum_tensor(name, list(shape), dt))

        V = lambda fn, r=(), w=(): P.op("vector", fn, r, w)
        A = lambda fn, r=(), w=(): P.op("scalar", fn, r, w)
        G = lambda fn, r=(), w=(): P.op("gpsimd", fn, r, w)
        T = lambda fn, r=(), w=(): P.op("tensor", fn, r, w)
        DS = lambda fn, r=(), w=(), slot=None: P.dma("sync", fn, r, w, slot)
        DG = lambda fn, r=(), w=(), slot=None: P.dma("gpsimd", fn, r, w, slot)

        uid = [0]
        ps = [None] * 8
        pb = [None] * 8
        ps_ctx = []

        def renew_psum():
            while ps_ctx:
                ps_ctx.pop().__exit__(None, None, None)
            for i in range(8):
                uid[0] += 1
                ctx = nc.psum_tensor("ps%d_%d" % (i, uid[0]), [128, 512], F32)
                ps[i] = ctx.__enter__()
                ps_ctx.append(ctx)
                pb[i] = Buf("ps%d" % i)

        def phase():
            P.barrier()
            renew_psum()
        rot = [0]
        tgl = [0]

        def gps():
            i = rot[0]
            rot[0] = (i + 1) % 3
            return ps[i], pb[i]

        ident = sb("ident", [128, 128], F32); identb = sb("identb", [128, 128], BF16)
        iot = sb("iot", [128, 128], F32); pidx = sb("pidx", [128, 1], F32)
        onesb = sb("onesb", [128, 128], BF16); epsc = sb("epsc", [128, 1], F32)
        sel65 = sb("sel65", [128, 64], F32)
        b_const = Buf("const")
        xT = sb("xT", [128, 8, 512], F32); b_x = Buf("xT")
        hT = sb("hT", [128, 8, 512], BF16); b_h = Buf("hT")
        wbuf = [sb("wbuf%d" % i, [128, 8, 512], BF16) for i in range(2)]
        b_wbuf = [Buf("wbuf0"), Buf("wbuf1")]
        wrot = [0]
        cT = sb("cT", [128, 8, 1 + NSS], BF16); cTf = sb("cTf", [128, 8, 1 + NSS], F32); b_cT = Buf("cT")
        modT = sb("modT", [128, 48, 1 + NSS], F32); b_mod = Buf("modT")
        G1 = sb("G1", [128, 8, 1 + NSS], F32); G2 = sb("G2", [128, 8, 1 + NSS], F32)
        vecs = sb("vecs", [128, 64], F32)
        b_vecs = Buf("vecs")
        badaT = sb("badaT", [128, 48], F32)
        wpool = sb("wpool", [128, 4, 128], BF16); b_wpool = Buf("wpool")
        kT12 = sb("kT12", [128, 2, 128], BF16); b_kT12 = Buf("kT12")
        halo_p = sb("halo_p", [128, 4, 16], F32); b_halo_p = Buf("halo_p")
        halo_c = sb("halo_c", [128, 4, 2], F32); b_halo_c = Buf("halo_c")
        stg = sb("stg", [128, 1024], F32); b_stg = Buf("stg")
        stg2 = sb("stg2", [128, 1024], F32); b_stg2 = Buf("stg2")

        class AR:
            def __init__(self):
                self.stack = []

            @property
            def off(self):
                return len(self.stack)

            @off.setter
            def off(self, mark):
                while len(self.stack) > mark:
                    self.stack.pop().__exit__(None, None, None)

            def reset(self):
                self.off = 0

            def alloc(self, name, nelem, dt):
                uid[0] += 1
                ctx = nc.sbuf_tensor("%s_%d" % (name, uid[0]), [128, nelem], dt)
                t = ctx.__enter__()
                self.stack.append(ctx)
                return t[:, 0:nelem], Buf(name)
        ar = AR()

        renew_psum()
        G(("iota", C(iot[:], [[1, 128]], base=0, channel_multiplier=0, allow_small_or_imprecise_dtypes=True)), w=[b_const])
        G(("iota", C(pidx[:], [[0, 1]], base=0, channel_multiplier=1, allow_small_or_imprecise_dtypes=True)), w=[b_const])
        V(("tensor_scalar", C(out=ident[:], in0=iot[:], scalar1=pidx[:, 0:1], scalar2=None, op0=ALU.is_equal)), r=[b_const], w=[b_const])
        V(("tensor_copy", C(out=identb[:], in_=ident[:])), r=[b_const], w=[b_const])
        V(("memset", C(onesb[:], 1.0)), w=[b_const])
        V(("memset", C(epsc[:], EPS)), w=[b_const])
        V(("tensor_scalar", C(out=sel65[:], in0=iot[:, 0:64], scalar1=0.0, scalar2=None, op0=ALU.mult)), r=[b_const], w=[b_const])
        V(("tensor_scalar", C(out=sel65[:], in0=sel65[:], scalar1=pidx[:, 0:1], scalar2=64.0, op0=ALU.add, op1=ALU.is_equal)), r=[b_const], w=[b_const])

        for s_i in range(1 + NSS):
            DS(("dma_start", C(out=cTf[:, :, s_i], in_=c_all[s_i].rearrange("(k p) -> p k", p=128), allow_slow_non_contiguous=True)), w=[b_cT])
        A(("activation", C(out=cT[:], in_=cTf[:], func=AF.Silu)), r=[b_cT], w=[b_cT])

        def load_w(src2d, c0, ncols, nk):
            i = wrot[0]
            wrot[0] = 1 - i
            wt, bw = wbuf[i], b_wbuf[i]
            view = wt[:, 0:nk, 0:ncols]
            DG(("dma_start", C(out=view, in_=src2d.rearrange("(k p) c -> p k c", p=128)[:, :, c0:c0 + ncols])), w=[bw], slot="wbuf%d" % i)
            return wt, bw

        def mm(psap, pbuf, pairs, rbufs):
            n = len(pairs)
            for i, (l, r) in enumerate(pairs):
                T(("matmul", C(psap, lhsT=l, rhs=r, start=(i == 0), stop=(i == n - 1))), r=rbufs, w=[pbuf])

        def rms_stats(src_fn, nk, dim, W, srcbufs, name):
            sq, bsq = ar.alloc(name + "_sq", 2 * 512, BF16)
            rstd, brs = ar.alloc(name + "_rstd", 512, F32)
            p_, pb_ = gps()
            for k in range(nk):
                sqk = sq[:, (k % 2) * 512:(k % 2) * 512 + W]
                A(("activation", C(out=sqk, in_=src_fn(k), func=AF.Square)), r=srcbufs, w=[bsq])
                T(("matmul", C(p_[:, 0:W], lhsT=onesb[:], rhs=sqk, start=(k == 0), stop=(k == nk - 1))), r=[bsq, b_const], w=[pb_])
            A(("activation", C(out=rstd[:, 0:W], in_=p_[:, 0:W], func=AF.Sqrt, scale=1.0 / dim, bias=epsc[:, 0:1])), r=[pb_, b_const], w=[brs])
            V(("reciprocal", C(out=rstd[:, 0:W], in_=rstd[:, 0:W])), r=[brs], w=[brs])
            return rstd, brs

        def transpose_out(src_fn, nk, W, dst_fn, srcbufs, rows_per_k=128, part0=0):
            for t0 in range(0, W, 128):
                n = min(128, W - t0)
                for kg in range(0, nk, 4):
                    ng = min(4, nk - kg)
                    p_, pb_ = gps()
                    for k in range(ng):
                        T(("transpose", C(p_[0:n, k * rows_per_k:(k + 1) * rows_per_k], src_fn(kg + k)[:, t0:t0 + n],
                                                                ident[part0:part0 + rows_per_k, part0:part0 + rows_per_k])), r=srcbufs + [b_const], w=[pb_])
                    tgl[0] = 1 - tgl[0]
                    s_, bs_ = (stg, b_stg) if tgl[0] == 0 else (stg2, b_stg2)
                    A(("copy", C(out=s_[0:n, 0:ng * rows_per_k], in_=p_[0:n, 0:ng * rows_per_k])), r=[pb_], w=[bs_])
                    DS(("dma_start", C(out=dst_fn(t0, n, kg * rows_per_k, (kg + ng) * rows_per_k), in_=s_[0:n, 0:ng * rows_per_k])), r=[bs_], w=[b_out])

        for blk in blocks:
            W, off = blk["W"], blk["off"]
            for t0 in range(0, W, 128):
                n = min(128, W - t0)
                src = (x_p[off + t0:off + t0 + n, :] if blk["seq"] == 0 else x_s[off - SEQ + t0:off - SEQ + t0 + n, :])
                DS(("dma_start", C(out=stg[0:n, :], in_=src)), w=[b_stg])
                for k in range(8):
                    p_, pb_ = gps()
                    T(("transpose", C(p_[:, 0:n], stg[0:n, k * 128:(k + 1) * 128], ident[0:n, 0:n])), r=[b_stg, b_const], w=[pb_])
                    A(("copy", C(out=xT[:, k, t0:t0 + n], in_=p_[:, 0:n])), r=[pb_], w=[b_x])
            DS(("dma_start", C(out=xT_scr[:, :, off:off + W], in_=xT[:, :, 0:W])), r=[b_x], w=[b_xT])

        for l in range(DEPTH):
            phase()
            ar.reset()
            def colload(dst, src1d, n):
                DS(("dma_start", C(out=dst, in_=src1d.rearrange("(k p) -> p k", p=128), allow_slow_non_contiguous=True)), w=[b_vecs])
            colload(vecs[:, 0:8], g_mix[l], 8); colload(vecs[:, 8:16], g_ffn[l], 8)
            colload(vecs[:, 16:20], g_q[l], 4); colload(vecs[:, 20:22], g_kv[l], 2)
            colload(vecs[:, 22:26], pool_scale[l], 4)
            for j in range(3):
                colload(vecs[:, 26 + 4 * j:30 + 4 * j], conv_w[l, j], 4)
            colload(vecs[:, 38:46], g_final, 8)
            colload(badaT[:, :], b_ada[l], 48)
            DG(("dma_start", C(out=wpool[:], in_=w_pool[l].rearrange("g c d -> c g d"))), w=[b_wpool])
            for s2 in range(2):
                DS(("dma_start", C(out=stg[:, 0:128], in_=peer_keys[l, s2])), w=[b_stg])
                p_, pb_ = gps()
                T(("transpose", C(p_[:, 0:128], stg[:, 0:128], ident[:])), r=[b_stg, b_const], w=[pb_])
                A(("copy", C(out=kT12[:, s2, :], in_=p_[:, 0:128])), r=[pb_], w=[b_kT12])
            for g in range(12):
                wt, bw = load_w(w_ada[l], g * 512, 512, 8)
                p_, pb_ = gps()
                for mi in range(4):
                    mm(p_[:, mi * 8:mi * 8 + 1 + NSS], pb_, [(wt[:, k, mi * 128:(mi + 1) * 128], cT[:, k, :]) for k in range(8)], [bw, b_cT])
                V(("tensor_tensor", C(out=modT[:, 4 * g:4 * g + 4, :], in0=p_[:, 0:32].rearrange("p (m s) -> p m s", s=8)[:, :, 0:1 + NSS],
                                                        in1=badaT[:, 4 * g:4 * g + 4].unsqueeze(2).to_broadcast([128, 4, 1 + NSS]), op=ALU.add)),
                  r=[pb_, b_vecs], w=[b_mod])
            for (Gx, c0, m0) in ((G1, 0, 8), (G2, 8, 32)):
                V(("tensor_scalar", C(out=Gx[:], in0=modT[:, m0:m0 + 8, :], scalar1=1.0, scalar2=None, op0=ALU.add)), r=[b_mod], w=[b_mod])
                V(("tensor_tensor", C(out=Gx[:], in0=Gx[:], in1=vecs[:, c0:c0 + 8].unsqueeze(2).to_broadcast([128, 8, 1 + NSS]), op=ALU.mult)), r=[b_mod, b_vecs], w=[b_mod])

            phase(); ar.reset()
            ub = [ar.alloc("pub%d" % i, 1024, BF16) for i in range(2)]
            vb = [ar.alloc("pvb%d" % i, 1024, BF16) for i in range(2)]
            uTp = [ar.alloc("puT%d" % i, 1024, BF16) for i in range(2)]
            ptb = ps[3][:].bitcast(BF16)
            for i in range(128):
                u_, bu_ = ub[i % 2]; v_, bv_ = vb[i % 2]; uT_, buT_ = uTp[i % 2]
                DG(("dma_start", C(out=u_, in_=peer_u[l, i * 128:(i + 1) * 128, :])), w=[bu_], slot="pub%d" % (i % 2))
                DG(("dma_start", C(out=v_, in_=peer_v[l, i * 128:(i + 1) * 128, :])), w=[bv_], slot="pvb%d" % (i % 2))
                for k in range(8):
                    T(("transpose", C(ptb[:, k * 128:(k + 1) * 128], u_[:, k * 128:(k + 1) * 128], identb[:])), r=[bu_, b_const], w=[pb[3]])
                if i % 2 == 0:
                    A(("copy", C(out=uT_, in_=ptb[:, 0:1024])), r=[pb[3]], w=[buT_])
                else:
                    V(("tensor_copy", C(out=uT_, in_=ptb[:, 0:1024])), r=[pb[3]], w=[buT_])
                DS(("dma_start", C(out=uv_scr[i, :, 0:D], in_=uT_)), r=[buT_], w=[b_uvs], slot="puTs%d" % (i % 2))
                DS(("dma_start", C(out=uv_scr[i, :, D:2 * D], in_=v_)), r=[bv_], w=[b_uvs], slot="pvs%d" % (i % 2))
            for s in range(NSS):
                phase(); ar.reset()
                ckvb, b_ckvb = ar.alloc("c_ckvb", 2 * PAST, BF16)
                krT, b_krT = ar.alloc("c_krT", PAST, BF16)
                ckvb3 = ckvb.rearrange("p (k t) -> p k t", k=2)
                for kb in range(PAST // 128):
                    DS(("dma_start", C(out=stg[:, 0:256], in_=ckv_c[l, s, kb * 128:(kb + 1) * 128, :])), w=[b_stg])
                    V(("memset", C(stg2[:, 0:64], 0.0)), w=[b_stg2])
                    DS(("dma_start", C(out=stg2[:, 64:96], in_=kr_c[l, s, kb * 128:(kb + 1) * 128, :])), w=[b_stg2])
                    for k in range(2):
                        p_, pb_ = gps()
                        T(("transpose", C(p_[:, 0:128], stg[:, k * 128:(k + 1) * 128], ident[:])), r=[b_stg, b_const], w=[pb_])
                        A(("copy", C(out=ckvb3[:, k, kb * 128:(kb + 1) * 128], in_=p_[:, 0:128])), r=[pb_], w=[b_ckvb])
                    p_, pb_ = gps()
                    T(("transpose", C(p_[0:96, 0:128], stg2[:, 0:96], ident[:])), r=[b_stg2, b_const], w=[pb_])
                    A(("copy", C(out=krT[64:96, kb * 128:(kb + 1) * 128], in_=p_[64:96, 0:128])), r=[pb_], w=[b_krT])
                wukv, b_wukv = ar.alloc("c_wukv", 2 * 1024, BF16)
                wukv4 = wukv.rearrange("p (k h c) -> p k h c", k=2, h=NH)
                DG(("dma_start", C(out=wukv4, in_=w_ukv[l].rearrange("(k p) h c -> p k h c", p=128))), w=[b_wukv])
                for c0 in range(0, PAST, 512):
                    expand_kv_args = (ckvb3, b_ckvb, krT, b_krT, wukv4, b_wukv, c0, 512, SEQ + s * SKS + c0)
                    _expand_kv(P, ar, T, A, V, DS, gps, KT_scr, V_scr, b_KT, b_V, *expand_kv_args)

            for bi, blk in enumerate(blocks):
                seq, t0b, W, off = blk["seq"], blk["t0"], blk["W"], blk["off"]
                nt = (W + 127) // 128
                kbase = 0 if seq == 0 else SEQ + (seq - 1) * SKS
                kpos = t0b if seq == 0 else PAST
                phase(); ar.reset()
                DS(("dma_start", C(out=xT[:, :, 0:W], in_=xT_scr[:, :, off:off + W])), r=[b_xT], w=[b_x])
                tabs, b_tabs = ar.alloc("tabs", 4 * 512, F32)
                tabs3 = tabs.rearrange("p (a t) -> p a t", a=4)
                QT, b_QT = ar.alloc("QT", NH * 512, BF16)
                QT3 = QT.rearrange("p (h t) -> p h t", h=NH)
                poolT, b_poolT = ar.alloc("poolT", 4 * 512, BF16); pool3 = poolT.rearrange("p (k t) -> p k t", k=4)
                convT, b_convT = ar.alloc("convT", 4 * 512, BF16); conv3 = convT.rearrange("p (k t) -> p k t", k=4)
                markA = ar.off
                DS(("dma_start", C(out=tabs3[64:96, :, 0:W], in_=rope_tab[:, 64:96, off:off + W].rearrange("a p t -> p a t"))), w=[b_tabs])
                rstd, brs = rms_stats(lambda k: xT[:, k, 0:W], 8, D, W, [b_x], "n1")
                tmp, btmp = ar.alloc("n1_tmp", 2 * 512, F32)
                for k in range(8):
                    tk = tmp[:, (k % 2) * 512:(k % 2) * 512 + W]
                    V(("tensor_tensor", C(out=tk, in0=xT[:, k, 0:W], in1=rstd[:, 0:W], op=ALU.mult)), r=[b_x, brs], w=[btmp])
                    A(("activation", C(out=hT[:, k, 0:W], in_=tk, func=AF.Identity, scale=G1[:, k, seq:seq + 1], bias=modT[:, k, seq:seq + 1])),
                      r=[btmp, b_mod], w=[b_h])

                def proj_cols(c0, ncols, evac):
                    wt, bw = load_w(w_in[l], c0, ncols, 8)
                    for mi in range((ncols + 127) // 128):
                        m = min(128, ncols - mi * 128)
                        p_, pb_ = gps()
                        mm(p_[0:m, 0:W], pb_, [(wt[:, k, mi * 128:mi * 128 + m], hT[:, k, 0:W]) for k in range(8)], [bw, b_h])
                        evac(mi, p_, pb_)

                phase(); ar.off = markA
                cqT, b_cq = ar.alloc("cqT", 4 * 512, F32)
                cq3 = cqT.rearrange("p (k t) -> p k t", k=4)
                proj_cols(0, 512, lambda mi, p_, pb_: A(("copy", C(out=cq3[:, mi, 0:W], in_=p_[:, 0:W])), r=[pb_], w=[b_cq]))
                rq, brq = rms_stats(lambda k: cq3[:, k, 0:W], 4, 512, W, [b_cq], "nq")
                cqn, b_cqn = ar.alloc("cqn", 4 * 512, BF16)
                cqn3 = cqn.rearrange("p (k t) -> p k t", k=4)
                for k in range(4):
                    V(("scalar_tensor_tensor", C(out=cqn3[:, k, 0:W], in0=cq3[:, k, 0:W], scalar=vecs[:, 16 + k:17 + k], in1=rq[:, 0:W], op0=ALU.mult, op1=ALU.mult)),
                      r=[b_cq, brq, b_vecs], w=[b_cqn])
                wuq, b_wuq = ar.alloc("wuq", 4 * 768, BF16); wuqs, b_wuqs = ar.alloc("wuqs", 4 * 768, BF16)
                wuq4 = wuq.rearrange("p (k h c) -> p k h c", k=4, h=NH); wuqs4 = wuqs.rearrange("p (k h c) -> p k h c", k=4, h=NH)
                wsrc = w_uq[l].rearrange("(k p) h c -> p k h c", p=128)
                DG(("dma_start", C(out=wuq4, in_=wsrc)), w=[b_wuq])
                for k in range(4):
                    DG(("dma_start", C(out=wuqs4[:, k, :, 0:64], in_=wsrc[:, k, :, 0:64])), w=[b_wuqs])
                    DG(("dma_start", C(out=wuqs4[:, k, :, 64:80], in_=wsrc[:, k, :, 80:96])), w=[b_wuqs])
                    DG(("dma_start", C(out=wuqs4[:, k, :, 80:96], in_=wsrc[:, k, :, 64:80])), w=[b_wuqs])
                rtmp, b_rtmp = ar.alloc("rtmp", 512, F32)
                for h in range(NH):
                    p1, pb1 = gps()
                    mm(p1[0:96, 0:W], pb1, [(wuq4[:, k, h, :], cqn3[:, k, 0:W]) for k in range(4)], [b_wuq, b_cqn])
                    p2, pb2 = gps()
                    mm(p2[0:96, 0:W], pb2, [(wuqs4[:, k, h, :], cqn3[:, k, 0:W]) for k in range(4)], [b_wuqs, b_cqn])
                    A(("activation", C(out=QT3[0:64, h, 0:W], in_=p1[0:64, 0:W], func=AF.Identity, scale=ATTN_SCALE)), r=[pb1], w=[b_QT])
                    V(("tensor_tensor", C(out=rtmp[64:96, 0:W], in0=p1[64:96, 0:W], in1=tabs3[64:96, 0, 0:W], op=ALU.mult)), r=[pb1, b_tabs], w=[b_rtmp])
                    V(("tensor_tensor", C(out=QT3[64:96, h, 0:W], in0=p2[64:96, 0:W], in1=tabs3[64:96, 1, 0:W], op=ALU.mult)), r=[pb2, b_tabs], w=[b_QT])
                    V(("tensor_tensor", C(out=QT3[64:96, h, 0:W], in0=QT3[64:96, h, 0:W], in1=rtmp[64:96, 0:W], op=ALU.add)), r=[b_rtmp, b_QT], w=[b_QT])

                phase(); ar.off = markA
                rtmp, b_rtmp = ar.alloc("rtmp2", 512, F32)
                ckvT, b_ckv = ar.alloc("ckvT", 2 * 512, F32)
                ckv3 = ckvT.rearrange("p (k t) -> p k t", k=2)
                proj_cols(512, 256, lambda mi, p_, pb_: A(("copy", C(out=ckv3[:, mi, 0:W], in_=p_[:, 0:W])), r=[pb_], w=[b_ckv]))
                rk, brk = rms_stats(lambda k: ckv3[:, k, 0:W], 2, 256, W, [b_ckv], "nk")
                ckvb, b_ckvb = ar.alloc("ckvb", 2 * 512, BF16)
                ckvb3 = ckvb.rearrange("p (k t) -> p k t", k=2)
                for k in range(2):
                    V(("scalar_tensor_tensor", C(out=ckv3[:, k, 0:W], in0=ckv3[:, k, 0:W], scalar=vecs[:, 20 + k:21 + k], in1=rk[:, 0:W], op0=ALU.mult, op1=ALU.mult)),
                      r=[b_ckv, brk, b_vecs], w=[b_ckv])
                    A(("copy", C(out=ckvb3[:, k, 0:W], in_=ckv3[:, k, 0:W])), r=[b_ckv], w=[b_ckvb])
                kvdst = (lambda t0, n, a, b: kv_p[l, off + t0:off + t0 + n, a:b]) if seq == 0 else (lambda t0, n, a, b: kv_s[l, off - SEQ + t0:off - SEQ + t0 + n, a:b])
                transpose_out(lambda k: ckv3[:, k, 0:W], 2, W, kvdst, [b_ckv])

                krT, b_krT = ar.alloc("krT", 512, BF16)
                krF, b_krF = ar.alloc("krF", 512, F32)
                wkr, b_wkr = ar.alloc("wkr", 2 * 8 * 96, BF16)
                wkr4 = wkr.rearrange("p (a k c) -> p a k c", a=2, k=8)
                V(("memset", C(wkr[:], 0.0)), w=[b_wkr])
                wi = w_in[l].rearrange("(k p) c -> p k c", p=128)
                DG(("dma_start", C(out=wkr4[:, 0, :, 64:96], in_=wi[:, :, 768:800])), w=[b_wkr])
                DG(("dma_start", C(out=wkr4[:, 1, :, 64:80], in_=wi[:, :, 784:800])), w=[b_wkr])
                DG(("dma_start", C(out=wkr4[:, 1, :, 80:96], in_=wi[:, :, 768:784])), w=[b_wkr])
                p1, pb1 = gps()
                mm(p1[0:96, 0:W], pb1, [(wkr4[:, 0, k, :], hT[:, k, 0:W]) for k in range(8)], [b_wkr, b_h])
                p2, pb2 = gps()
                mm(p2[0:96, 0:W], pb2, [(wkr4[:, 1, k, :], hT[:, k, 0:W]) for k in range(8)], [b_wkr, b_h])
                V(("tensor_tensor", C(out=rtmp[64:96, 0:W], in0=p1[64:96, 0:W], in1=tabs3[64:96, 2, 0:W], op=ALU.mult)), r=[pb1, b_tabs], w=[b_rtmp])
                V(("tensor_tensor", C(out=krF[64:96, 0:W], in0=p2[64:96, 0:W], in1=tabs3[64:96, 3, 0:W], op=ALU.mult)), r=[pb2, b_tabs], w=[b_krF])
                V(("tensor_tensor", C(out=krF[64:96, 0:W], in0=krF[64:96, 0:W], in1=rtmp[64:96, 0:W], op=ALU.add)), r=[b_rtmp, b_krF], w=[b_krF])
                A(("copy", C(out=krT[64:96, 0:W], in_=krF[64:96, 0:W])), r=[b_krF], w=[b_krT])
                krdst = (lambda t0, n, a, b: kr_p[l, off + t0:off + t0 + n, a:b]) if seq == 0 else (lambda t0, n, a, b: kr_s[l, off - SEQ + t0:off - SEQ + t0 + n, a:b])
                transpose_out(lambda k: krF[64:96, 0:W], 1, W, krdst, [b_krF], rows_per_k=32, part0=64)

                wukv, b_wukv = ar.alloc("wukv", 2 * 1024, BF16)
                wukv4 = wukv.rearrange("p (k h c) -> p k h c", k=2, h=NH)
                DG(("dma_start", C(out=wukv4, in_=w_ukv[l].rearrange("(k p) h c -> p k h c", p=128))), w=[b_wukv])
                _expand_kv(P, ar, T, A, V, DS, gps, KT_scr, V_scr, b_KT, b_V, ckvb3, b_ckvb, krT, b_krT, wukv4, b_wukv, 0, W, kbase + kpos)

                phase(); ar.off = markA
                upe, b_upe = ar.alloc("upe", 4 * 528, F32); upe3 = upe.rearrange("p (k t) -> p k t", k=4)
                if t0b == 0:
                    if seq == 0:
                        V(("memset", C(halo_p[:], 0.0)), w=[b_halo_p])
                        V(("memset", C(halo_c[:], 0.0)), w=[b_halo_c])
                    else:
                        V(("memset", C(halo_p[:], 0.0)), w=[b_halo_p])
                        for k in range(4):
                            DS(("dma_start", C(out=halo_p[:, k, 1:16], in_=st_pool[l, seq - 1][:, k * 128:(k + 1) * 128].rearrange("t p -> p t"), allow_slow_non_contiguous=True)), w=[b_halo_p])
                            DS(("dma_start", C(out=halo_c[:, k, :], in_=st_conv[l, seq - 1][:, k * 128:(k + 1) * 128].rearrange("t p -> p t"), allow_slow_non_contiguous=True)), w=[b_halo_c])
                V(("tensor_copy", C(out=upe3[:, :, 0:16], in_=halo_p[:])), r=[b_halo_p], w=[b_upe])
                proj_cols(800, 512, lambda mi, p_, pb_: A(("copy", C(out=upe3[:, mi, 16:16 + W], in_=p_[:, 0:W])), r=[pb_], w=[b_upe]))
                V(("tensor_copy", C(out=halo_p[:], in_=upe3[:, :, W:W + 16])), r=[b_upe], w=[b_halo_p])
                pp, b_pp = ar.alloc("pp", 2 * 528, F32); pp3 = pp.rearrange("p (a t) -> p a t", a=2)
                dT, b_dT = ar.alloc("dT", 512, BF16)
                if seq == 0 and t0b == 0:
                    rc0t, b_rc0 = ar.alloc("rc0t", 64, F32); rc03 = rc0t.rearrange("p (k t) -> p k t", k=4)
                    DS(("dma_start", C(out=rc03, in_=rc0)), w=[b_rc0])
                for g, wdw in enumerate((2, 4, 8, 16)):
                    cur = upe3[:, g, :]
                    step, a = 1, 0
                    L = 16 + W
                    while step < wdw:
                        dst = pp3[:, a, :]
                        V(("tensor_tensor", C(out=dst[:, step:L], in0=cur[:, step:L], in1=cur[:, 0:L - step], op=ALU.add)),
                          r=[b_upe, b_pp], w=[b_pp])
                        cur = dst
                        step *= 2
                        a = 1 - a
                    V(("scalar_tensor_tensor", C(out=dT[:, 0:W], in0=cur[:, 16:16 + W], scalar=1.0 / wdw, in1=upe3[:, g, 16:16 + W], op0=ALU.mult, op1=ALU.subtract)),
                      r=[b_pp, b_upe], w=[b_dT])
                    if seq == 0 and t0b == 0:
                        V(("tensor_tensor", C(out=pp3[:, 1 - a if False else a, 0:16], in0=cur[:, 16:32], in1=rc03[:, g, :], op=ALU.mult)), r=[b_pp, b_rc0], w=[b_pp])
                        V(("tensor_tensor", C(out=dT[:, 0:16], in0=pp3[:, a, 0:16], in1=upe3[:, g, 16:32], op=ALU.subtract)), r=[b_pp, b_upe], w=[b_dT])
                    p_, pb_ = gps()
                    T(("matmul", C(p_[:, 0:W], lhsT=wpool[:, g, :], rhs=dT[:, 0:W], start=True, stop=True)), r=[b_wpool, b_dT], w=[pb_])
                    A(("activation", C(out=pool3[:, g, 0:W], in_=p_[:, 0:W], func=AF.Identity, scale=vecs[:, 22 + g:23 + g])), r=[pb_, b_vecs], w=[b_poolT])
                last_of_seq = (seq != 0) or (t0b + W == SEQ)
                if last_of_seq:
                    pdst = pool_p[l] if seq == 0 else pool_s[l, seq - 1]
                    p_, pb_ = gps()
                    for k in range(4):
                        T(("transpose", C(p_[0:15, k * 128:(k + 1) * 128], upe3[:, k, W + 1:W + 16], ident[:])), r=[b_upe, b_const], w=[pb_])
                    A(("copy", C(out=stg[0:15, 0:512], in_=p_[0:15, 0:512])), r=[pb_], w=[b_stg])
                    DS(("dma_start", C(out=pdst, in_=stg[0:15, 0:512])), r=[b_stg], w=[b_out])

                phase(); ar.off = markA
                uce, b_uce = ar.alloc("uce", 4 * 520, F32); uce3 = uce.rearrange("p (k t) -> p k t", k=4)
                bg, b_bg = ar.alloc("bg", 4 * 512, F32); bg3 = bg.rearrange("p (k t) -> p k t", k=4)
                cg, b_cg = ar.alloc("cg", 4 * 512, F32); cg3 = cg.rearrange("p (k t) -> p k t", k=4)
                V(("tensor_copy", C(out=uce3[:, :, 0:2], in_=halo_c[:])), r=[b_halo_c], w=[b_uce])
                proj_cols(1312, 512, lambda mi, p_, pb_: A(("copy", C(out=bg3[:, mi, 0:W], in_=p_[:, 0:W])), r=[pb_], w=[b_bg]))
                proj_cols(1824, 512, lambda mi, p_, pb_: A(("copy", C(out=cg3[:, mi, 0:W], in_=p_[:, 0:W])), r=[pb_], w=[b_cg]))
                proj_cols(2336, 512, lambda mi, p_, pb_: V(("tensor_tensor", C(out=uce3[:, mi, 2:2 + W], in0=p_[:, 0:W], in1=cg3[:, mi, 0:W], op=ALU.mult)), r=[pb_, b_cg], w=[b_uce]))
                V(("tensor_copy", C(out=halo_c[:], in_=uce3[:, :, W:W + 2])), r=[b_uce], w=[b_halo_c])
                for k in range(4):
                    yk = cg3[:, k, 0:W]
                    V(("tensor_scalar", C(out=yk, in0=uce3[:, k, 0:W], scalar1=vecs[:, 26 + k:27 + k], scalar2=None, op0=ALU.mult)), r=[b_uce, b_vecs, b_cg], w=[b_cg])
                    V(("scalar_tensor_tensor", C(out=yk, in0=uce3[:, k, 1:1 + W], scalar=vecs[:, 30 + k:31 + k], in1=yk, op0=ALU.mult, op1=ALU.add)), r=[b_uce, b_vecs, b_cg], w=[b_cg])
                    V(("scalar_tensor_tensor", C(out=yk, in0=uce3[:, k, 2:2 + W], scalar=vecs[:, 34 + k:35 + k], in1=yk, op0=ALU.mult, op1=ALU.add)), r=[b_uce, b_vecs, b_cg], w=[b_cg])
                    V(("tensor_tensor", C(out=conv3[:, k, 0:W], in0=yk, in1=bg3[:, k, 0:W], op=ALU.mult)), r=[b_cg, b_bg], w=[b_convT])
                if last_of_seq:
                    cdst = conv_p[l] if seq == 0 else conv_s[l, seq - 1]
                    p_, pb_ = gps()
                    for k in range(4):
                        T(("transpose", C(p_[0:2, k * 128:(k + 1) * 128], uce3[:, k, W:W + 2], ident[:])), r=[b_uce, b_const], w=[pb_])
                    A(("copy", C(out=stg2[0:2, 0:512], in_=p_[0:2, 0:512])), r=[pb_], w=[b_stg2])
                    DS(("dma_start", C(out=cdst, in_=stg2[0:2, 0:512])), r=[b_stg2], w=[b_out])

                phase()
                ar.off = markA
                mark = ar.off
                attnT, b_attn = ar.alloc("attnT", NH * 512, BF16); attn3 = attnT.rearrange("p (h t) -> p h t", h=NH)
                nkeys = (t0b + W) if seq == 0 else (PAST + TS)
                nkb = (nkeys + 127) // 128
                kb0 = kbase // 128
                KTh = []; Vh = []
                for i in range(2):
                    a_, b_ = ar.alloc("KTh%d" % i, nkb * 128, BF16); KTh.append((a_, b_))
                    a_, b_ = ar.alloc("Vh%d" % i, nkb * 65, BF16); Vh.append((a_.rearrange("p (k c) -> p k c", c=65), b_))
                PT = [ar.alloc("PT%d" % i, 512, BF16) for i in range(3)]
                osb, b_osb = ar.alloc("osb", 512, F32); rcs, b_rcs = ar.alloc("rcs", 512, F32)
                pti = 0
                for h in range(NH):
                    kt, bkt = KTh[h % 2]; vt, bvt = Vh[h % 2]
                    DS(("dma_start", C(out=kt[0:96, 0:nkeys], in_=KT_scr[h, :, kbase:kbase + nkeys])), r=[b_KT], w=[bkt])
                    DS(("dma_start", C(out=vt[:, 0:nkb, :], in_=V_scr[h, :, kb0:kb0 + nkb, :])), r=[b_V], w=[bvt])
                    po, pbo = ps[4 + h % 2], pb[4 + h % 2]
                    for kb in range(nkb):
                        kk = min(128, nkeys - kb * 128)
                        q0 = 0
                        diag = False
                        if seq == 0 and kb * 128 >= t0b:
                            q0 = kb * 128 - t0b
                            diag = True
                        nq = W - q0
                        p_, pb_ = gps()
                        T(("matmul", C(p_[0:kk, 0:nq], lhsT=kt[0:96, kb * 128:kb * 128 + kk], rhs=QT3[0:96, h, q0:W], start=True, stop=True)),
                          r=[bkt, b_QT], w=[pb_])
                        pt_, bpt_ = PT[pti]; pti = (pti + 1) % 3
                        A(("activation", C(out=pt_[0:kk, 0:nq], in_=p_[0:kk, 0:nq], func=AF.Exp)), r=[pb_], w=[bpt_])
                        if diag:
                            V(("memset", C(pt_[64:128, 0:64], 0.0)), w=[bpt_])
                        T(("matmul", C(po[0:65, q0:W], lhsT=vt[0:kk, kb, :], rhs=pt_[0:kk, 0:nq], start=(kb == 0), stop=(kb == nkb - 1))),
                          r=[bvt, bpt_], w=[pbo])
                    A(("copy", C(out=osb[0:65, 0:W], in_=po[0:65, 0:W])), r=[pbo], w=[b_osb])
                    p_, pb_ = gps()
                    T(("matmul", C(p_[0:64, 0:W], lhsT=sel65[0:65, :], rhs=osb[0:65, 0:W], start=True, stop=True)), r=[b_osb, b_const], w=[pb_])
                    V(("reciprocal", C(out=rcs[0:64, 0:W], in_=p_[0:64, 0:W])), r=[pb_], w=[b_rcs])
                    V(("tensor_tensor", C(out=attn3[0:64, h, 0:W], in0=osb[0:64, 0:W], in1=rcs[0:64, 0:W], op=ALU.mult)), r=[b_osb, b_rcs], w=[b_attn])

                phase()
                ar.off = mark + 1
                mixT, b_mix = ar.alloc("mixT", 8 * 512, BF16); mix3 = mixT.rearrange("p (k t) -> p k t", k=8)
                gts, b_gts = ar.alloc("gts", 3 * 512, F32); gts3 = gts.rearrange("p (n t) -> p n t", n=3)
                wba, b_wba = ar.alloc("wba", 8 * 128, BF16); wba3 = wba.rearrange("p (h c) -> p h c", h=NH)
                wbp, b_wbp = ar.alloc("wbp", 2 * 4 * 128, BF16); wbp4 = wbp.rearrange("p (n k c) -> p n k c", n=2, k=4)
                acc, b_acc = ar.alloc("acc", 512, F32)
                for m in range(8):
                    DG(("dma_start", C(out=wba3[0:64], in_=w_branch[l, 0].rearrange("(h p) c -> p h c", p=64)[:, :, m * 128:(m + 1) * 128])), w=[b_wba])
                    for n2 in range(2):
                        DG(("dma_start", C(out=wbp4[:, n2], in_=w_branch[l, 1 + n2].rearrange("(k p) c -> p k c", p=128)[:, :, m * 128:(m + 1) * 128])), w=[b_wbp])
                    for n in range(3):
                        wt, bw = load_w(w_in[l], 2848 + n * 1024 + m * 128, 128, 8)
                        p_, pb_ = gps()
                        mm(p_[:, 0:W], pb_, [(wt[:, k, 0:128], hT[:, k, 0:W]) for k in range(8)], [bw, b_h])
                        A(("activation", C(out=gts3[:, n, 0:W], in_=p_[:, 0:W], func=AF.Sigmoid)), r=[pb_], w=[b_gts])
                    for n in range(3):
                        p_, pb_ = gps()
                        if n == 0:
                            mm(p_[:, 0:W], pb_, [(wba3[0:64, h, :], attn3[0:64, h, 0:W]) for h in range(NH)], [b_wba, b_attn])
                        else:
                            src3, bsrc = (pool3, b_poolT) if n == 1 else (conv3, b_convT)
                            mm(p_[:, 0:W], pb_, [(wbp4[:, n - 1, k, :], src3[:, k, 0:W]) for k in range(4)], [b_wbp, bsrc])
                        if n == 0:
                            V(("tensor_tensor", C(out=acc[:, 0:W], in0=p_[:, 0:W], in1=gts3[:, 0, 0:W], op=ALU.mult)), r=[pb_, b_gts], w=[b_acc])
                        else:
                            V(("tensor_tensor", C(out=gts3[:, n, 0:W], in0=p_[:, 0:W], in1=gts3[:, n, 0:W], op=ALU.mult)), r=[pb_, b_gts], w=[b_gts])
                            if n == 1:
                                V(("tensor_tensor", C(out=acc[:, 0:W], in0=acc[:, 0:W], in1=gts3[:, 1, 0:W], op=ALU.add)), r=[b_gts, b_acc], w=[b_acc])
                            else:
                                V(("tensor_tensor", C(out=mix3[:, m, 0:W], in0=acc[:, 0:W], in1=gts3[:, 2, 0:W], op=ALU.add)), r=[b_gts, b_acc], w=[b_mix])
                for g in range(2):
                    wt, bw = load_w(w_out[l], g * 512, 512, 8)
                    for mi in range(4):
                        m = g * 4 + mi
                        p_, pb_ = gps()
                        mm(p_[:, 0:W], pb_, [(wt[:, k, mi * 128:(mi + 1) * 128], mix3[:, k, 0:W]) for k in range(8)], [bw, b_mix])
                        V(("scalar_tensor_tensor", C(out=xT[:, m, 0:W], in0=p_[:, 0:W], scalar=modT[:, 16 + m, seq:seq + 1], in1=xT[:, m, 0:W], op0=ALU.mult, op1=ALU.add)),
                          r=[pb_, b_mod, b_x], w=[b_x])

                phase(); ar.reset()
                rstd, brs = rms_stats(lambda k: xT[:, k, 0:W], 8, D, W, [b_x], "n2")
                tmp, btmp = ar.alloc("n2_tmp", 2 * 512, F32)
                for k in range(8):
                    tk = tmp[:, (k % 2) * 512:(k % 2) * 512 + W]
                    V(("tensor_tensor", C(out=tk, in0=xT[:, k, 0:W], in1=rstd[:, 0:W], op=ALU.mult)), r=[b_x, brs], w=[btmp])
                    A(("activation", C(out=hT[:, k, 0:W], in_=tk, func=AF.Identity, scale=G2[:, k, seq:seq + 1], bias=modT[:, 24 + k, seq:seq + 1])),
                      r=[btmp, b_mod], w=[b_h])
                for sb0 in range(0, W, 256):
                    SW = min(256, W - sb0)
                    phase(); ar.reset()
                    GT, b_GT = ar.alloc("GT", 128 * 256, BF16); GT3 = GT.rearrange("p (i n) -> p i n", i=128)
                    qT, b_qT = ar.alloc("qT", 16 * 256, BF16); qT3 = qT.rearrange("p (m t) -> p m t", m=16)
                    for g in range(4):
                        wt, bw = load_w(peer_wq[l], g * 512, 512, 8)
                        for mi in range(4):
                            p_, pb_ = gps()
                            mm(p_[:, 0:SW], pb_, [(wt[:, k, mi * 128:(mi + 1) * 128], hT[:, k, sb0:sb0 + SW]) for k in range(8)], [bw, b_h])
                            A(("copy", C(out=qT3[:, g * 4 + mi, 0:SW], in_=p_[:, 0:SW])), r=[pb_], w=[b_qT])
                    mark_t = ar.off
                    for tt in range(0, SW, 128):
                        n = min(128, SW - tt)
                        phase(); ar.off = mark_t
                        _peer_topk(P, ar, T, A, V, G, gps, ps, pb, n, tt, qT3, b_qT, kT12, b_kT12, ident, iot, b_const, GT3, b_GT)
                    phase(); ar.off = mark_t
                    NUV = 4
                    uvt = [ar.alloc("uvt%d" % i, 2048, BF16) for i in range(NUV)]
                    gl = [ar.alloc("gl%d" % i, 256, F32) for i in range(2)]
                    wT = [ar.alloc("wT%d" % i, 256, BF16) for i in range(2)]
                    osb2, b_osb2 = ar.alloc("osb2", 1024, F32)
                    nts = (SW + 127) // 128

                    def stage2(i):
                        uv_, buv_ = uvt[i % NUV]; w_, bw_ = wT[i % 2]
                        v_ = uv_[:, D:2 * D]
                        for ti in range(nts):
                            n = min(128, SW - ti * 128)
                            for hf in range(2):
                                bk = 4 + ti * 2 + hf
                                T(("matmul", C(ps[bk][0:n, 0:512], lhsT=w_[:, ti * 128:ti * 128 + n], rhs=v_[:, hf * 512:(hf + 1) * 512],
                                               start=(i == 0), stop=(i == 127))), r=[buv_, bw_], w=[pb[bk]])

                    for i in range(128):
                        uv_, buv_ = uvt[i % NUV]; g_, bg_ = gl[i % 2]; w_, bw_ = wT[i % 2]
                        uT_ = uv_[:, 0:D]
                        DS(("dma_start", C(out=uv_, in_=uv_scr[i])), r=[b_uvs], w=[buv_], slot="uvt%d" % (i % NUV))
                        p_, pb_ = gps()
                        mm(p_[:, 0:SW], pb_, [(uT_[:, k * 128:(k + 1) * 128], hT[:, k, sb0:sb0 + SW]) for k in range(8)], [buv_, b_h])
                        A(("activation", C(out=g_[:, 0:SW], in_=p_[:, 0:SW], func=AF.Gelu)), r=[pb_], w=[bg_])
                        V(("tensor_tensor", C(out=w_[:, 0:SW], in0=g_[:, 0:SW], in1=GT3[:, i, 0:SW], op=ALU.mult)), r=[bg_, b_GT], w=[bw_])
                        if i >= 1:
                            stage2(i - 1)
                    stage2(127)
                    for ti in range(nts):
                        n = min(128, SW - ti * 128)
                        for hf in range(2):
                            bk = 4 + ti * 2 + hf
                            A(("copy", C(out=osb2[0:n, hf * 512:(hf + 1) * 512], in_=ps[bk][0:n, 0:512])), r=[pb[bk]], w=[b_osb2])
                        for k in range(8):
                            p_, pb_ = gps()
                            T(("transpose", C(p_[:, 0:n], osb2[0:n, k * 128:(k + 1) * 128], ident[0:n, 0:n])), r=[b_osb2, b_const], w=[pb_])
                            c0 = sb0 + ti * 128
                            V(("scalar_tensor_tensor", C(out=xT[:, k, c0:c0 + n], in0=p_[:, 0:n], scalar=modT[:, 40 + k, seq:seq + 1],
                                                                                    in1=xT[:, k, c0:c0 + n], op0=ALU.mult, op1=ALU.add)), r=[pb_, b_mod, b_x], w=[b_x])

                if l < DEPTH - 1:
                    DS(("dma_start", C(out=xT_scr[:, :, off:off + W], in_=xT[:, :, 0:W])), r=[b_x], w=[b_xT])
                else:
                    phase(); ar.reset()
                    rstd, brs = rms_stats(lambda k: xT[:, k, 0:W], 8, D, W, [b_x], "nf")
                    yT, b_yT = ar.alloc("yT", 8 * 512, F32); yT3 = yT.rearrange("p (k t) -> p k t", k=8)
                    for k in range(8):
                        V(("scalar_tensor_tensor", C(out=yT3[:, k, 0:W], in0=xT[:, k, 0:W], scalar=vecs[:, 38 + k:39 + k], in1=rstd[:, 0:W], op0=ALU.mult, op1=ALU.mult)),
                          r=[b_x, brs, b_vecs], w=[b_yT])
                    ydst = (lambda t0, n, a, b: y_p[off + t0:off + t0 + n, a:b]) if seq == 0 else (lambda t0, n, a, b: y_s[off - SEQ + t0:off - SEQ + t0 + n, a:b])
                    transpose_out(lambda k: yT3[:, k, 0:W], 8, W, ydst, [b_yT])

        P.barrier()
        ar.reset()
        while ps_ctx:
            ps_ctx.pop().__exit__(None, None, None)
        P.emit()
    return nc


def _expand_kv(P, ar, T, A, V, DS, gps, KT_scr, V_scr, b_KT, b_V, ckvb3, b_ckvb, krT, b_krT, wukv4, b_wukv, c0, W, kslot):
    P.barrier()
    mark = ar.off
    KTb, b_KTb = ar.alloc("KTb", NH * 512, BF16); KTb3 = KTb.rearrange("p (h t) -> p h t", h=NH)
    Vb, b_Vb = ar.alloc("Vb", 4 * NH * 65, BF16); Vb4 = Vb.rearrange("p (t h c) -> p t h c", t=4, h=NH)
    V(("memset", C(Vb[:], 1.0)), w=[b_Vb])
    for h in range(NH):
        p_, pb_ = gps()
        for k in range(2):
            T(("matmul", C(p_[0:64, 0:W], lhsT=wukv4[:, k, h, 0:64], rhs=ckvb3[:, k, c0:c0 + W], start=(k == 0), stop=(k == 1))), r=[b_wukv, b_ckvb], w=[pb_])
        A(("copy", C(out=KTb3[0:64, h, 0:W], in_=p_[0:64, 0:W])), r=[pb_], w=[b_KTb])
        V(("tensor_copy", C(out=KTb3[64:96, h, 0:W], in_=krT[64:96, c0:c0 + W])), r=[b_krT], w=[b_KTb])
    DS(("dma_start", C(out=KT_scr[:, :, kslot:kslot + W].rearrange("h p t -> p h t"), in_=KTb3[0:96, :, 0:W])), r=[b_KTb], w=[b_KT])
    nt = (W + 127) // 128
    for t in range(nt):
        n = min(128, W - t * 128)
        p_, pb_ = gps()
        for k in range(2):
            T(("matmul", C(p_[0:n, 0:512].rearrange("p (h c) -> p h c", h=NH), lhsT=ckvb3[:, k, c0 + t * 128:c0 + t * 128 + n], rhs=wukv4[:, k, :, 64:128],
                                                       start=(k == 0), stop=(k == 1))), r=[b_wukv, b_ckvb], w=[pb_])
        A(("copy", C(out=Vb4[0:n, t, :, 0:64], in_=p_[0:n, 0:512].rearrange("p (h c) -> p h c", h=NH))), r=[pb_], w=[b_Vb])
    kb0 = kslot // 128
    p0 = kslot % 128
    if p0 == 0:
        full = W // 128
        if full > 0:
            for h in range(NH):
                DS(("dma_start", C(out=V_scr[h, :, kb0:kb0 + full, :], in_=Vb4[:, 0:full, h, :])), r=[b_Vb], w=[b_V])
        rem = W - full * 128
        if rem > 0:
            DS(("dma_start", C(out=V_scr[:, 0:rem, kb0 + full, :].rearrange("h p c -> p h c"), in_=Vb4[0:rem, full, :, :])), r=[b_Vb], w=[b_V])
    else:
        assert p0 + W <= 128
        DS(("dma_start", C(out=V_scr[:, p0:p0 + W, kb0, :].rearrange("h p c -> p h c"), in_=Vb4[0:W, 0, :, :])), r=[b_Vb], w=[b_V])
    ar.off = mark


def _peer_topk(P, ar, T, A, V, G, gps, ps, pb, n, tt, qT3, b_qT, kT12, b_kT12, ident, iot, b_const, GT3, b_GT):
    SS, bSS = ar.alloc("SS", 2048, F32)
    S2, bS2 = ar.alloc("S2", 1024, F32); S23 = S2.rearrange("p (h m) -> p h m", h=NH)
    cand, bc = ar.alloc("cand", 2048, F32); cand4 = cand.rearrange("p (h a b) -> p h a b", h=NH, a=16)
    oh, boh = ar.alloc("oh", 2048, F32); oh4 = oh.rearrange("p (h k a) -> p h k a", h=NH, k=16)
    S = []
    for s2 in range(2):
        s_ = SS[:, s2 * 1024:(s2 + 1) * 1024]
        s3 = s_.rearrange("p (h m) -> p h m", h=NH)
        for half in range(2):
            p_, pb_ = gps()
            for hh in range(4):
                h = half * 4 + hh
                T(("matmul", C(p_[0:n, hh * 128:(hh + 1) * 128], lhsT=qT3[:, 2 * h + s2, tt:tt + n], rhs=kT12[:, s2, :], start=True, stop=True)),
                  r=[b_qT, b_kT12], w=[pb_])
            A(("copy", C(out=s_[0:n, half * 512:(half + 1) * 512], in_=p_[0:n, 0:512])), r=[pb_], w=[bSS])
        S.append(s3)
    V16 = []; I16 = []
    for s2 in range(2):
        v_, bv_ = ar.alloc("V16_%d" % s2, 128, F32); i_, bi_ = ar.alloc("I16_%d" % s2, 128, U32); if_, bif_ = ar.alloc("I16f_%d" % s2, 128, F32)
        v3 = v_.rearrange("p (h k) -> p h k", h=NH); i3 = i_.rearrange("p (h k) -> p h k", h=NH)
        s3 = S[s2]
        for h in range(NH):
            V(("max", C(out=v3[0:n, h, 0:8], in_=s3[0:n, h, :])), r=[bSS], w=[bv_])
            V(("match_replace", C(out=S23[0:n, h, :], in_to_replace=v3[0:n, h, 0:8], in_values=s3[0:n, h, :], imm_value=NEG)), r=[bSS, bv_], w=[bS2])
            V(("max", C(out=v3[0:n, h, 8:16], in_=S23[0:n, h, :])), r=[bS2], w=[bv_])
            V(("max_index", C(out=i3[0:n, h, 0:8], in_max=v3[0:n, h, 0:8], in_values=s3[0:n, h, :])), r=[bSS, bv_], w=[bi_])
            V(("max_index", C(out=i3[0:n, h, 8:16], in_max=v3[0:n, h, 8:16], in_values=S23[0:n, h, :])), r=[bS2, bv_], w=[bi_])
        V(("tensor_copy", C(out=if_[0:n, :], in_=i_[0:n, :])), r=[bi_], w=[bif_])
        V16.append((v3, bv_)); I16.append((if_.rearrange("p (h k) -> p h k", h=NH), bif_))
    V(("tensor_tensor", C(out=cand4[0:n], in0=V16[0][0][0:n].unsqueeze(3).to_broadcast([n, NH, 16, 16]), in1=V16[1][0][0:n].unsqueeze(2).to_broadcast([n, NH, 16, 16]), op=ALU.add)),
      r=[V16[0][1], V16[1][1]], w=[bc])
    P.barrier()
    bc2 = Buf("cand2")
    SC, bSC = ar.alloc("SC", 128, F32); SC3 = SC.rearrange("p (h k) -> p h k", h=NH)
    SEL, bSEL = ar.alloc("SEL", 128, U32); SEL3 = SEL.rearrange("p (h k) -> p h k", h=NH)
    candf = cand.rearrange("p (h c) -> p h c", h=NH); cand2f = SS.rearrange("p (h c) -> p h c", h=NH)
    for h in range(NH):
        V(("max", C(out=SC3[0:n, h, 0:8], in_=candf[0:n, h, :])), r=[bc], w=[bSC])
        V(("match_replace", C(out=cand2f[0:n, h, :], in_to_replace=SC3[0:n, h, 0:8], in_values=candf[0:n, h, :], imm_value=NEG)), r=[bc, bSC], w=[bc2])
        V(("max", C(out=SC3[0:n, h, 8:16], in_=cand2f[0:n, h, :])), r=[bc2], w=[bSC])
        V(("max_index", C(out=SEL3[0:n, h, 0:8], in_max=SC3[0:n, h, 0:8], in_values=candf[0:n, h, :])), r=[bc, bSC], w=[bSEL])
        V(("max_index", C(out=SEL3[0:n, h, 8:16], in_max=SC3[0:n, h, 8:16], in_values=cand2f[0:n, h, :])), r=[bc2, bSC], w=[bSEL])
    AB = []
    for which in range(2):
        u_, bu_ = ar.alloc("abu%d" % which, 128, U32); f_, bf_ = ar.alloc("abf%d" % which, 128, F32)
        if which == 0:
            V(("tensor_scalar", C(out=u_[0:n, :], in0=SEL[0:n, :], scalar1=4, scalar2=None, op0=ALU.logical_shift_right)), r=[bSEL], w=[bu_])
        else:
            V(("tensor_scalar", C(out=u_[0:n, :], in0=SEL[0:n, :], scalar1=15, scalar2=None, op0=ALU.bitwise_and)), r=[bSEL], w=[bu_])
        V(("tensor_copy", C(out=f_[0:n, :], in_=u_[0:n, :])), r=[bu_], w=[bf_])
        AB.append((f_.rearrange("p (h k) -> p h k", h=NH), bf_))
    P.barrier()
    bio = Buf("io16")
    io16 = cand; io4 = io16.rearrange("p (h k a) -> p h k a", h=NH, k=16)
    G(("iota", C(io16[:], [[0, 128], [1, 16]], base=0, channel_multiplier=0, allow_small_or_imprecise_dtypes=True)), w=[bio])
    slots = []
    for which in range(2):
        sl_, bsl_ = ar.alloc("slot%d" % which, 128, F32)
        ab3, bab = AB[which]; i3f, bif = I16[which]
        V(("tensor_tensor", C(out=oh4[0:n], in0=io4[0:n], in1=ab3[0:n].unsqueeze(3).to_broadcast([n, NH, 16, 16]), op=ALU.is_equal)), r=[bio, bab], w=[boh])
        V(("tensor_tensor", C(out=oh4[0:n], in0=oh4[0:n], in1=i3f[0:n].unsqueeze(2).to_broadcast([n, NH, 16, 16]), op=ALU.mult)), r=[boh, bif], w=[boh])
        V(("tensor_reduce", C(out=sl_[0:n, :], in_=oh.rearrange("p (s a) -> p s a", a=16)[0:n], axis=AX.X, op=ALU.add)), r=[boh], w=[bsl_])
        slots.append((sl_, bsl_))
    wg, bwg = ar.alloc("wg", 128, F32); wg3 = wg.rearrange("p (h k) -> p h k", h=NH)
    zs, bzs = ar.alloc("zs", 8, F32)
    V(("tensor_tensor", C(out=wg3[0:n], in0=SC3[0:n], in1=SC3[0:n, :, 0:1].to_broadcast([n, NH, 16]), op=ALU.subtract)), r=[bSC], w=[bwg])
    A(("activation", C(out=wg[0:n, :], in_=wg[0:n, :], func=AF.Exp)), r=[bwg], w=[bwg])
    V(("tensor_reduce", C(out=zs[0:n, :], in_=wg3[0:n], axis=AX.X, op=ALU.add)), r=[bwg], w=[bzs])
    V(("reciprocal", C(out=zs[0:n, :], in_=zs[0:n, :])), r=[bzs], w=[bzs])
    V(("tensor_tensor", C(out=wg3[0:n], in0=wg3[0:n], in1=zs[0:n, :].unsqueeze(2).to_broadcast([n, NH, 16]), op=ALU.mult)), r=[bwg, bzs], w=[bwg])
    tr, btr = ar.alloc("trn", 3 * 128, F32); tr3 = tr.rearrange("p (a t) -> p a t", a=3)
    for a, (src, bsrc) in enumerate((slots[0], slots[1], (wg, bwg))):
        p_, pb_ = gps()
        T(("transpose", C(p_[:, 0:n], src[0:n, :], ident[0:n, 0:n])), r=[bsrc, b_const], w=[pb_])
        A(("copy", C(out=tr3[:, a, 0:n], in_=p_[:, 0:n])), r=[pb_], w=[btr])
    A4 = [ar.alloc("A4_%d" % i, 512, BF16) for i in range(2)]
    B4 = [ar.alloc("B4_%d" % i, 512, BF16) for i in range(2)]
    gi = 0
    for t4 in range(0, n, 4):
        pg, pbg = ps[6 + gi % 2], pb[6 + gi % 2]
        a_, ba_ = A4[gi % 2]; b_, bb_ = B4[gi % 2]
        a3 = a_.rearrange("p (t i) -> p t i", t=4); b3 = b_.rearrange("p (t i) -> p t i", t=4)
        gi += 1
        m4 = min(4, n - t4)
        iob = iot[:, :].unsqueeze(1).to_broadcast([128, m4, 128])
        V(("tensor_tensor", C(out=a3[:, 0:m4, :], in0=iob, in1=tr3[:, 0, t4:t4 + m4].unsqueeze(2).to_broadcast([128, m4, 128]), op=ALU.is_equal)), r=[btr, b_const], w=[ba_])
        V(("tensor_tensor", C(out=a3[:, 0:m4, :], in0=a3[:, 0:m4, :], in1=tr3[:, 2, t4:t4 + m4].unsqueeze(2).to_broadcast([128, m4, 128]), op=ALU.mult)), r=[btr, ba_], w=[ba_])
        V(("tensor_tensor", C(out=b3[:, 0:m4, :], in0=iob, in1=tr3[:, 1, t4:t4 + m4].unsqueeze(2).to_broadcast([128, m4, 128]), op=ALU.is_equal)), r=[btr, b_const], w=[bb_])
        for j in range(m4):
            T(("matmul", C(pg[:, j * 128:(j + 1) * 128], lhsT=b3[:, j, :], rhs=a3[:, j, :], start=True, stop=True)), r=[ba_, bb_], w=[pbg])
        A(("copy", C(out=GT3[:, :, tt + t4:tt + t4 + m4].rearrange("p i n -> p n i"), in_=pg[:, 0:m4 * 128].rearrange("p (n i) -> p n i", i=128))), r=[pbg], w=[b_GT])


_CACHE = {}


def _rope_tables(SEQ):
    NTOK = SEQ + NSS * TS
    half = 16
    inv = (10000.0 ** (-np.arange(half, dtype=np.float32) / half)).astype(np.float32)
    pos = np.concatenate([np.arange(SEQ, dtype=np.float32)] + [PAST + np.arange(TS, dtype=np.float32)] * NSS).astype(np.float32)
    ang = (pos[None, :] * inv[:, None]).astype(np.float32)
    cos = np.cos(ang).astype(np.float32); sin = np.sin(ang).astype(np.float32)
    cosf = np.concatenate([cos, cos], 0); sins = np.concatenate([-sin, sin], 0)
    tab = np.zeros((4, 96, NTOK), np.float32)
    sc = np.float32(ATTN_SCALE)
    tab[0, 64:96] = cosf * sc; tab[1, 64:96] = sins * sc
    tab[2, 64:96] = cosf; tab[3, 64:96] = sins
    rc0 = np.zeros((128, 4, 16), np.float32)
    for g, w in enumerate((2, 4, 8, 16)):
        rc0[:, g, :] = 1.0 / np.minimum(np.arange(16) + 1, w)
    return tab, rc0


def kernel(x_prompt, x_sample, cache_kv_latent, cache_k_rope, state_pool, state_conv, c_prompt, c_sample,
           w_ada, b_ada, g_mix, w_in, g_q, w_uq, g_kv, w_ukv, w_pool, pool_scale, conv_w, w_branch, w_out,
           g_ffn, peer_wq, peer_keys, peer_u, peer_v, g_final):
    f = lambda a: np.ascontiguousarray(np.asarray(a, dtype=np.float32))
    x_prompt = f(x_prompt); x_sample = f(x_sample)
    B, SEQ, _ = x_prompt.shape
    DB = x_sample.shape[0]
    ncores = DB // NSS
    assert ncores == 8 and x_sample.shape[1] == TS
    if SEQ not in _CACHE:
        _CACHE[SEQ] = build_program(SEQ)
    nc = _CACHE[SEQ]
    tab, rc0 = _rope_tables(SEQ)
    ckv = f(cache_kv_latent); krc = f(cache_k_rope); sp = f(state_pool); scv = f(state_conv)
    cp = f(c_prompt); cs = f(c_sample)
    shared = {"w_ada": f(w_ada), "b_ada": f(b_ada), "g_mix": f(g_mix), "w_in": f(w_in), "g_q": f(g_q), "w_uq": f(w_uq),
              "g_kv": f(g_kv), "w_ukv": f(w_ukv), "w_pool": f(w_pool), "pool_scale": f(pool_scale), "conv_w": f(conv_w),
              "w_branch": f(w_branch), "w_out": f(w_out), "g_ffn": f(g_ffn), "peer_wq": f(peer_wq).reshape(DEPTH, D, NH * 256),
              "peer_keys": f(peer_keys), "peer_u": f(peer_u), "peer_v": f(peer_v), "g_final": f(g_final),
              "rope_tab": tab, "rc0": rc0}
    in_maps = []
    for c in range(ncores):
        b = c % B
        sl = slice(NSS * c, NSS * c + NSS)
        m = dict(shared)
        m["x_p"] = x_prompt[b]
        m["x_s"] = np.ascontiguousarray(x_sample[sl].reshape(NSS * TS, D))
        m["c_all"] = np.ascontiguousarray(np.concatenate([cp[b:b + 1], cs[sl]], 0))
        m["ckv_c"] = np.ascontiguousarray(ckv[:, sl]); m["kr_c"] = np.ascontiguousarray(krc[:, sl])
        m["st_pool"] = np.ascontiguousarray(sp[:, sl]); m["st_conv"] = np.ascontiguousarray(scv[:, sl])
        in_maps.append(m)
    res = run_bass_kernel_spmd(nc, in_maps, core_ids=list(range(ncores))).results
    y_prompt = np.stack([res[b]["y_p"] for b in range(B)], 0)
    y_sample = np.concatenate([res[c]["y_s"].reshape(NSS, TS, D) for c in range(ncores)], 0)
    p_kv = np.stack([res[b]["kv_p"] for b in range(B)], 1)
    p_kr = np.stack([res[b]["kr_p"] for b in range(B)], 1)
    p_pool = np.stack([res[b]["pool_p"] for b in range(B)], 1)
    p_conv = np.stack([res[b]["conv_p"] for b in range(B)], 1)
    s_kv = np.concatenate([res[c]["kv_s"].reshape(DEPTH, NSS, TS, 256) for c in range(ncores)], 1)
    s_kr = np.concatenate([res[c]["kr_s"].reshape(DEPTH, NSS, TS, 32) for c in range(ncores)], 1)
    s_pool = np.concatenate([res[c]["pool_s"] for c in range(ncores)], 1)
    s_conv = np.concatenate([res[c]["conv_s"] for c in range(ncores)], 1)
    return (y_prompt, y_sample, p_kv, p_kr, p_pool, p_conv, s_kv, s_kr, s_pool, s_conv)
```

```python
import contextlib
import numpy as np
import concourse.bass as bass
import concourse.mybir as mybir
from concourse.bass_utils import run_bass_kernel_spmd

F32 = mybir.dt.float32
BF16 = mybir.dt.bfloat16
U32 = mybir.dt.uint32
AF = mybir.ActivationFunctionType
ALU = mybir.AluOpType
AX = mybir.AxisListType

D = 1024
DEPTH = 4
NSS = 4
TS = 32
PAST = 1024
NH = 8
D_IN = 5920
EPS = 1e-6
ATTN_SCALE = 96.0 ** -0.5
NEG = -1e30


def C(*a, **k):
    return (a, k)


class Buf:
    __slots__ = ("name", "w", "r")

    def __init__(self, name=""):
        self.name = name
        self.w = None
        self.r = []


class Prog:
    ENGS = ["tensor", "vector", "scalar", "gpsimd", "sync"]

    def __init__(self, nc, nrot=24, nslot=40):
        self.nc = nc
        ndma = nrot + nslot
        self.nrot = nrot
        self.nslot = nslot
        self.nslot_used = 0
        self.slots = {}
        self.ops = {e: [] for e in self.ENGS}
        self.cnt = {e: 0 for e in self.ENGS}
        self.known = {e: {} for e in self.ENGS}
        self.ndma = ndma
        self.dma_val = [0] * ndma
        self.dma_next = 0
        self.esem = {}
        self.dsem = []

    def _deps(self, eng, reads, writes):
        ev = {}

        def add(e):
            if e is None:
                return
            k, v = e
            if ev.get(k, 0) < v:
                ev[k] = v
        me = ("e", eng)
        for b in reads:
            add(b.w)
        for b in writes:
            if b.w is not None and b.w[0] != me:
                add(b.w)
            for r in b.r:
                if r[0] != me:
                    add(r)
        kn = self.known[eng]
        waits = []
        for k, v in ev.items():
            if kn.get(k, 0) < v:
                kn[k] = v
                waits.append((k, v))
        return waits

    def _commit(self, event, reads, writes):
        for b in reads:
            b.r.append(event)
            if len(b.r) > 64:
                m = {}
                for k, v in b.r:
                    if m.get(k, 0) < v:
                        m[k] = v
                b.r = list(m.items())
        for b in writes:
            b.w = event
            b.r = []

    def op(self, eng, fn, reads=(), writes=()):
        waits = self._deps(eng, reads, writes)
        if eng == "tensor":
            waits = [(k, v) for (k, v) in waits if k != ("e", eng)]
        self.cnt[eng] += 1
        event = (("e", eng), self.cnt[eng])
        self.known[eng][("e", eng)] = max(self.known[eng].get(("e", eng), 0), 0)
        self.ops[eng].append((waits, fn, event))
        self._commit(event, reads, writes)

    def dma(self, eng, fn, reads=(), writes=(), slot=None):
        waits = self._deps(eng, reads, writes)
        if slot is not None:
            if slot not in self.slots:
                assert self.nslot_used < self.nslot
                self.slots[slot] = self.nrot + self.nslot_used
                self.nslot_used += 1
            i = self.slots[slot]
            key = ("d", i)
        else:
            i = self.dma_next
            self.dma_next = (i + 1) % self.nrot
            key = ("d", i)
            if self.dma_val[i] > 0 and self.known[eng].get(key, 0) < self.dma_val[i]:
                self.known[eng][key] = self.dma_val[i]
                waits.append((key, self.dma_val[i]))
        self.dma_val[i] += 16
        event = (key, self.dma_val[i])
        self.ops[eng].append((waits, fn, event))
        self._commit(event, reads, writes)

    def barrier(self):
        allev = {}
        for e in self.ENGS:
            if self.cnt[e] > 0:
                allev[("e", e)] = self.cnt[e]
        for i in range(self.ndma):
            if self.dma_val[i] > 0:
                allev[("d", i)] = self.dma_val[i]
        waits = []
        for k, v in allev.items():
            if k == ("e", "sync"):
                continue
            if self.known["sync"].get(k, 0) < v:
                waits.append((k, v))
        self.cnt["sync"] += 1
        ev = (("e", "sync"), self.cnt["sync"])
        self.ops["sync"].append((waits, ("nop", C()), ev))
        allev[ev[0]] = ev[1]
        for e in self.ENGS:
            if e != "sync":
                self.ops[e].append(([ev], None, None))
            for k, v in allev.items():
                if self.known[e].get(k, 0) < v:
                    self.known[e][k] = v

    def emit(self):
        nc = self.nc
        with contextlib.ExitStack() as st:
            for e in self.ENGS:
                self.esem[e] = st.enter_context(nc.semaphore("s_" + e))
            for i in range(self.ndma):
                self.dsem.append(st.enter_context(nc.semaphore("d_%d" % i)))
            block = st.enter_context(nc.Block())

            def sem_of(k):
                return self.esem[k[1]] if k[0] == "e" else self.dsem[k[1]]

            def make(engname):
                def body(eng):
                    pend = []
                    for waits, fn, event in self.ops[engname]:
                        pend.extend(waits)
                        if fn is None:
                            continue
                        m = {}
                        for k, v in pend:
                            if m.get(k, 0) < v:
                                m[k] = v
                        pend = []
                        items = list(m.items())
                        for k, v in items[:-1]:
                            eng.wait_ge(sem_of(k), v)
                        ins = getattr(eng, fn[0])(*fn[1][0], **fn[1][1])
                        if items:
                            k, v = items[-1]
                            ins._wait_ge(sem_of(k), v)
                        k, v = event
                        ins.then_inc(sem_of(k), 16 if k[0] == "d" else 1)
                    for k, v in pend:
                        eng.wait_ge(sem_of(k), v)
                return body

            block.tensor(make("tensor"))
            block.vector(make("vector"))
            block.scalar(make("scalar"))
            block.gpsimd(make("gpsimd"))
            block.sync(make("sync"))


def build_program(SEQ):
    NTOK = SEQ + NSS * TS
    NKBP = SEQ // 128
    SKS = 9 * 128
    SK = SEQ + NSS * SKS
    NKB = NKBP + NSS * 9
    nc = bass.Bass("TRN2", target_bir_lowering=False)
    P = Prog(nc)

    def din(name, shape, dt=F32):
        return nc.dram_tensor(name, list(shape), dt, kind="ExternalInput").ap()

    def dout(name, shape, dt=F32):
        return nc.dram_tensor(name, list(shape), dt, kind="ExternalOutput").ap()

    def dscr(name, shape, dt):
        return nc.dram_tensor(name, list(shape), dt, kind="Internal").ap()

    x_p = din("x_p", [SEQ, D]); x_s = din("x_s", [NSS * TS, D])
    c_all = din("c_all", [1 + NSS, D])
    ckv_c = din("ckv_c", [DEPTH, NSS, PAST, 256]); kr_c = din("kr_c", [DEPTH, NSS, PAST, 32])
    st_pool = din("st_pool", [DEPTH, NSS, 15, 512]); st_conv = din("st_conv", [DEPTH, NSS, 2, 512])
    w_ada = din("w_ada", [DEPTH, D, 6 * D]); b_ada = din("b_ada", [DEPTH, 6 * D])
    g_mix = din("g_mix", [DEPTH, D]); w_in = din("w_in", [DEPTH, D, D_IN])
    g_q = din("g_q", [DEPTH, 512]); w_uq = din("w_uq", [DEPTH, 512, NH, 96])
    g_kv = din("g_kv", [DEPTH, 256]); w_ukv = din("w_ukv", [DEPTH, 256, NH, 128])
    w_pool = din("w_pool", [DEPTH, 4, 128, 128]); pool_scale = din("pool_scale", [DEPTH, 512])
    conv_w = din("conv_w", [DEPTH, 3, 512]); w_branch = din("w_branch", [DEPTH, 3, 512, D])
    w_out = din("w_out", [DEPTH, D, D]); g_ffn = din("g_ffn", [DEPTH, D])
    peer_wq = din("peer_wq", [DEPTH, D, NH * 256]); peer_keys = din("peer_keys", [DEPTH, 2, 128, 128])
    peer_u = din("peer_u", [DEPTH, 16384, D]); peer_v = din("peer_v", [DEPTH, 16384, D])
    g_final = din("g_final", [D])
    rope_tab = din("rope_tab", [4, 96, NTOK]); rc0 = din("rc0", [128, 4, 16])

    y_p = dout("y_p", [SEQ, D]); y_s = dout("y_s", [NSS * TS, D])
    kv_p = dout("kv_p", [DEPTH, SEQ, 256]); kr_p = dout("kr_p", [DEPTH, SEQ, 32])
    pool_p = dout("pool_p", [DEPTH, 15, 512]); conv_p = dout("conv_p", [DEPTH, 2, 512])
    kv_s = dout("kv_s", [DEPTH, NSS * TS, 256]); kr_s = dout("kr_s", [DEPTH, NSS * TS, 32])
    pool_s = dout("pool_s", [DEPTH, NSS, 15, 512]); conv_s = dout("conv_s", [DEPTH, NSS, 2, 512])

    xT_scr = dscr("xT_scr", [128, 8, NTOK], F32)
    KT_scr = dscr("KT_scr", [NH, 96, SK], BF16)
    V_scr = dscr("V_scr", [NH, 128, NKB, 65], BF16)
    uv_scr = dscr("uv_scr", [128, 128, 2 * D], BF16)
    b_uvs = Buf("uv_scr")
    wbf_in = dscr("wbf_in", [D, D_IN], BF16); wbf_out = dscr("wbf_out", [D, D], BF16); wbf_pq = dscr("wbf_pq", [D, NH * 256], BF16)
    b_wscr = Buf("wscr")
    b_xT = Buf("xT_scr"); b_KT = Buf("KT_scr"); b_V = Buf("V_scr"); b_out = Buf("outs")

    blocks = []
    for t0 in range(0, SEQ, 512):
        blocks.append(dict(seq=0, t0=t0, W=min(512, SEQ - t0), off=t0))
    for s in range(NSS):
        blocks.append(dict(seq=1 + s, t0=0, W=TS, off=SEQ + s * TS))

    with contextlib.ExitStack() as st:
        def sb(name, shape, dt):
            return st.enter_context(nc.sbuf_tensor(name, list(shape), dt))

        def pst(name, shape, dt):
            return st.enter_context(nc.psum_tensor(name, list(shape), dt))

        V = lambda fn, r=(), w=(): P.op("vector", fn, r, w)
        A = lambda fn, r=(), w=(): P.op("scalar", fn, r, w)
        G = lambda fn, r=(), w=(): P.op("gpsimd", fn, r, w)
        T = lambda fn, r=(), w=(): P.op("tensor", fn, r, w)
        DS = lambda fn, r=(), w=(), slot=None: P.dma("sync", fn, r, w, slot)
        DG = lambda fn, r=(), w=(), slot=None: P.dma("gpsimd", fn, r, w, slot)

        uid = [0]
        ps = [None] * 8
        pb = [None] * 8
        ps_ctx = []

        def renew_psum():
            while ps_ctx:
                ps_ctx.pop().__exit__(None, None, None)
            for i in range(8):
                uid[0] += 1
                ctx = nc.psum_tensor("ps%d_%d" % (i, uid[0]), [128, 512], F32)
                ps[i] = ctx.__enter__()
                ps_ctx.append(ctx)
                pb[i] = Buf("ps%d" % i)

        def phase():
            P.barrier()
            renew_psum()
        rot = [0]
        tgl = [0]

        def gps():
            i = rot[0]
            rot[0] = (i + 1) % 3
            return ps[i], pb[i]

        ident = sb("ident", [128, 128], F32); identb = sb("identb", [128, 128], BF16)
        iot = sb("iot", [128, 128], F32); pidx = sb("pidx", [128, 1], F32)
        onesb = sb("onesb", [128, 128], BF16); epsc = sb("epsc", [128, 1], F32)
        sel65 = sb("sel65", [128, 64], F32)
        b_const = Buf("const")
        xT = sb("xT", [128, 8, 512], F32); b_x = Buf("xT")
        hT = sb("hT", [128, 8, 512], BF16); b_h = Buf("hT")
        wbuf = [sb("wbuf%d" % i, [128, 8, 512], BF16) for i in range(2)]
        b_wbuf = [Buf("wbuf0"), Buf("wbuf1")]
        wrot = [0]
        cT = sb("cT", [128, 8, 1 + NSS], BF16); cTf = sb("cTf", [128, 8, 1 + NSS], F32); b_cT = Buf("cT")
        modT = sb("modT", [128, 48, 1 + NSS], F32); b_mod = Buf("modT")
        G1 = sb("G1", [128, 8, 1 + NSS], F32); G2 = sb("G2", [128, 8, 1 + NSS], F32)
        vecs = sb("vecs", [128, 64], F32)
        b_vecs = Buf("vecs")
        badaT = sb("badaT", [128, 48], F32)
        wpool = sb("wpool", [128, 4, 128], BF16); b_wpool = Buf("wpool")
        kT12 = sb("kT12", [128, 2, 128], BF16); b_kT12 = Buf("kT12")
        halo_p = sb("halo_p", [128, 4, 16], F32); b_halo_p = Buf("halo_p")
        halo_c = sb("halo_c", [128, 4, 2], F32); b_halo_c = Buf("halo_c")
        stg = sb("stg", [128, 1024], F32); b_stg = Buf("stg")
        stg2 = sb("stg2", [128, 1024], F32); b_stg2 = Buf("stg2")

        class AR:
            def __init__(self):
                self.stack = []

            @property
            def off(self):
                return len(self.stack)

            @off.setter
            def off(self, mark):
                while len(self.stack) > mark:
                    self.stack.pop().__exit__(None, None, None)

            def reset(self):
                self.off = 0

            def alloc(self, name, nelem, dt):
                uid[0] += 1
                ctx = nc.sbuf_tensor("%s_%d" % (name, uid[0]), [128, nelem], dt)
                t = ctx.__enter__()
                self.stack.append(ctx)
                return t[:, 0:nelem], Buf(name)
        ar = AR()

        renew_psum()
        G(("iota", C(iot[:], [[1, 128]], base=0, channel_multiplier=0, allow_small_or_imprecise_dtypes=True)), w=[b_const])
        G(("iota", C(pidx[:], [[0, 1]], base=0, channel_multiplier=1, allow_small_or_imprecise_dtypes=True)), w=[b_const])
        V(("tensor_scalar", C(out=ident[:], in0=iot[:], scalar1=pidx[:, 0:1], scalar2=None, op0=ALU.is_equal)), r=[b_const], w=[b_const])
        V(("tensor_copy", C(out=identb[:], in_=ident[:])), r=[b_const], w=[b_const])
        V(("memset", C(onesb[:], 1.0)), w=[b_const])
        V(("memset", C(epsc[:], EPS)), w=[b_const])
        V(("tensor_scalar", C(out=sel65[:], in0=iot[:, 0:64], scalar1=0.0, scalar2=None, op0=ALU.mult)), r=[b_const], w=[b_const])
        V(("tensor_scalar", C(out=sel65[:], in0=sel65[:], scalar1=pidx[:, 0:1], scalar2=64.0, op0=ALU.add, op1=ALU.is_equal)), r=[b_const], w=[b_const])

        for s_i in range(1 + NSS):
            DS(("dma_start", C(out=cTf[:, :, s_i], in_=c_all[s_i].rearrange("(k p) -> p k", p=128), allow_slow_non_contiguous=True)), w=[b_cT])
        A(("activation", C(out=cT[:], in_=cTf[:], func=AF.Silu)), r=[b_cT], w=[b_cT])

        def load_w(src2d, c0, ncols, nk, rb=()):
            i = wrot[0]
            wrot[0] = 1 - i
            wt, bw = wbuf[i], b_wbuf[i]
            view = wt[:, 0:nk, 0:ncols]
            DG(("dma_start", C(out=view, in_=src2d.rearrange("(k p) c -> p k c", p=128)[:, :, c0:c0 + ncols])), r=list(rb), w=[bw], slot="wbuf%d" % i)
            return wt, bw

        def mm(psap, pbuf, pairs, rbufs):
            n = len(pairs)
            for i, (l, r) in enumerate(pairs):
                T(("matmul", C(psap, lhsT=l, rhs=r, start=(i == 0), stop=(i == n - 1))), r=rbufs, w=[pbuf])

        def rms_stats(src_fn, nk, dim, W, srcbufs, name):
            sq, bsq = ar.alloc(name + "_sq", 2 * 512, BF16)
            rstd, brs = ar.alloc(name + "_rstd", 512, F32)
            p_, pb_ = gps()
            for k in range(nk):
                sqk = sq[:, (k % 2) * 512:(k % 2) * 512 + W]
                A(("activation", C(out=sqk, in_=src_fn(k), func=AF.Square)), r=srcbufs, w=[bsq])
                T(("matmul", C(p_[:, 0:W], lhsT=onesb[:], rhs=sqk, start=(k == 0), stop=(k == nk - 1))), r=[bsq, b_const], w=[pb_])
            A(("activation", C(out=rstd[:, 0:W], in_=p_[:, 0:W], func=AF.Sqrt, scale=1.0 / dim, bias=epsc[:, 0:1])), r=[pb_, b_const], w=[brs])
            V(("reciprocal", C(out=rstd[:, 0:W], in_=rstd[:, 0:W])), r=[brs], w=[brs])
            return rstd, brs

        def transpose_out(src_fn, nk, W, dst_fn, srcbufs, rows_per_k=128, part0=0):
            for t0 in range(0, W, 128):
                n = min(128, W - t0)
                for kg in range(0, nk, 4):
                    ng = min(4, nk - kg)
                    p_, pb_ = gps()
                    for k in range(ng):
                        T(("transpose", C(p_[0:n, k * rows_per_k:(k + 1) * rows_per_k], src_fn(kg + k)[:, t0:t0 + n],
                                                                ident[part0:part0 + rows_per_k, part0:part0 + rows_per_k])), r=srcbufs + [b_const], w=[pb_])
                    tgl[0] = 1 - tgl[0]
                    s_, bs_ = (stg, b_stg) if tgl[0] == 0 else (stg2, b_stg2)
                    A(("copy", C(out=s_[0:n, 0:ng * rows_per_k], in_=p_[0:n, 0:ng * rows_per_k])), r=[pb_], w=[bs_])
                    DS(("dma_start", C(out=dst_fn(t0, n, kg * rows_per_k, (kg + ng) * rows_per_k), in_=s_[0:n, 0:ng * rows_per_k])), r=[bs_], w=[b_out])

        for blk in blocks:
            W, off = blk["W"], blk["off"]
            for t0 in range(0, W, 128):
                n = min(128, W - t0)
                src = (x_p[off + t0:off + t0 + n, :] if blk["seq"] == 0 else x_s[off - SEQ + t0:off - SEQ + t0 + n, :])
                DS(("dma_start", C(out=stg[0:n, :], in_=src)), w=[b_stg])
                for k in range(8):
                    p_, pb_ = gps()
                    T(("transpose", C(p_[:, 0:n], stg[0:n, k * 128:(k + 1) * 128], ident[0:n, 0:n])), r=[b_stg, b_const], w=[pb_])
                    A(("copy", C(out=xT[:, k, t0:t0 + n], in_=p_[:, 0:n])), r=[pb_], w=[b_x])
            DS(("dma_start", C(out=xT_scr[:, :, off:off + W], in_=xT[:, :, 0:W])), r=[b_x], w=[b_xT])

        for l in range(DEPTH):
            phase()
            ar.reset()
            def colload(dst, src1d, n):
                DS(("dma_start", C(out=dst, in_=src1d.rearrange("(k p) -> p k", p=128), allow_slow_non_contiguous=True)), w=[b_vecs])
            colload(vecs[:, 0:8], g_mix[l], 8); colload(vecs[:, 8:16], g_ffn[l], 8)
            colload(vecs[:, 16:20], g_q[l], 4); colload(vecs[:, 20:22], g_kv[l], 2)
            colload(vecs[:, 22:26], pool_scale[l], 4)
            for j in range(3):
                colload(vecs[:, 26 + 4 * j:30 + 4 * j], conv_w[l, j], 4)
            colload(vecs[:, 38:46], g_final, 8)
            colload(badaT[:, :], b_ada[l], 48)
            DG(("dma_start", C(out=wpool[:], in_=w_pool[l].rearrange("g c d -> c g d"))), w=[b_wpool])
            for s2 in range(2):
                DS(("dma_start", C(out=stg[:, 0:128], in_=peer_keys[l, s2])), w=[b_stg])
                p_, pb_ = gps()
                T(("transpose", C(p_[:, 0:128], stg[:, 0:128], ident[:])), r=[b_stg, b_const], w=[pb_])
                A(("copy", C(out=kT12[:, s2, :], in_=p_[:, 0:128])), r=[pb_], w=[b_kT12])
            for g in range(12):
                wt, bw = load_w(w_ada[l], g * 512, 512, 8)
                p_, pb_ = gps()
                for mi in range(4):
                    mm(p_[:, mi * 8:mi * 8 + 1 + NSS], pb_, [(wt[:, k, mi * 128:(mi + 1) * 128], cT[:, k, :]) for k in range(8)], [bw, b_cT])
                V(("tensor_tensor", C(out=modT[:, 4 * g:4 * g + 4, :], in0=p_[:, 0:32].rearrange("p (m s) -> p m s", s=8)[:, :, 0:1 + NSS],
                                                        in1=badaT[:, 4 * g:4 * g + 4].unsqueeze(2).to_broadcast([128, 4, 1 + NSS]), op=ALU.add)),
                  r=[pb_, b_vecs], w=[b_mod])
            for (Gx, c0, m0) in ((G1, 0, 8), (G2, 8, 32)):
                V(("tensor_scalar", C(out=Gx[:], in0=modT[:, m0:m0 + 8, :], scalar1=1.0, scalar2=None, op0=ALU.add)), r=[b_mod], w=[b_mod])
                V(("tensor_tensor", C(out=Gx[:], in0=Gx[:], in1=vecs[:, c0:c0 + 8].unsqueeze(2).to_broadcast([128, 8, 1 + NSS]), op=ALU.mult)), r=[b_mod, b_vecs], w=[b_mod])

            for (wsrc_, wdst_, ntot_) in ((w_in[l], wbf_in, D_IN), (w_out[l], wbf_out, D), (peer_wq[l], wbf_pq, NH * 256)):
                for c0 in range(0, ntot_, 512):
                    ncols = min(512, ntot_ - c0)
                    wt, bw = load_w(wsrc_, c0, ncols, 8)
                    DS(("dma_start", C(out=wdst_.rearrange("(k p) c -> p k c", p=128)[:, :, c0:c0 + ncols], in_=wt[:, 0:8, 0:ncols])), r=[bw], w=[b_wscr])
            phase(); ar.reset()
            ub = [ar.alloc("pub%d" % i, 1024, BF16) for i in range(2)]
            vb = [ar.alloc("pvb%d" % i, 1024, BF16) for i in range(2)]
            uTp = [ar.alloc("puT%d" % i, 1024, BF16) for i in range(2)]
            ptb = ps[3][:].bitcast(BF16)
            for i in range(128):
                u_, bu_ = ub[i % 2]; v_, bv_ = vb[i % 2]; uT_, buT_ = uTp[i % 2]
                DG(("dma_start", C(out=u_, in_=peer_u[l, i * 128:(i + 1) * 128, :])), w=[bu_], slot="pub%d" % (i % 2))
                DG(("dma_start", C(out=v_, in_=peer_v[l, i * 128:(i + 1) * 128, :])), w=[bv_], slot="pvb%d" % (i % 2))
                for k in range(8):
                    T(("transpose", C(ptb[:, k * 128:(k + 1) * 128], u_[:, k * 128:(k + 1) * 128], identb[:])), r=[bu_, b_const], w=[pb[3]])
                if i % 2 == 0:
                    A(("copy", C(out=uT_, in_=ptb[:, 0:1024])), r=[pb[3]], w=[buT_])
                else:
                    V(("tensor_copy", C(out=uT_, in_=ptb[:, 0:1024])), r=[pb[3]], w=[buT_])
                DS(("dma_start", C(out=uv_scr[i, :, 0:D], in_=uT_)), r=[buT_], w=[b_uvs], slot="puTs%d" % (i % 2))
                DS(("dma_start", C(out=uv_scr[i, :, D:2 * D], in_=v_)), r=[bv_], w=[b_uvs], slot="pvs%d" % (i % 2))
            for s in range(NSS):
                phase(); ar.reset()
                ckvb, b_ckvb = ar.alloc("c_ckvb", 2 * PAST, BF16)
                krT, b_krT = ar.alloc("c_krT", PAST, BF16)
                ckvb3 = ckvb.rearrange("p (k t) -> p k t", k=2)
                for kb in range(PAST // 128):
                    DS(("dma_start", C(out=stg[:, 0:256], in_=ckv_c[l, s, kb * 128:(kb + 1) * 128, :])), w=[b_stg])
                    V(("memset", C(stg2[:, 0:64], 0.0)), w=[b_stg2])
                    DS(("dma_start", C(out=stg2[:, 64:96], in_=kr_c[l, s, kb * 128:(kb + 1) * 128, :])), w=[b_stg2])
                    for k in range(2):
                        p_, pb_ = gps()
                        T(("transpose", C(p_[:, 0:128], stg[:, k * 128:(k + 1) * 128], ident[:])), r=[b_stg, b_const], w=[pb_])
                        A(("copy", C(out=ckvb3[:, k, kb * 128:(kb + 1) * 128], in_=p_[:, 0:128])), r=[pb_], w=[b_ckvb])
                    p_, pb_ = gps()
                    T(("transpose", C(p_[0:96, 0:128], stg2[:, 0:96], ident[:])), r=[b_stg2, b_const], w=[pb_])
                    A(("copy", C(out=krT[64:96, kb * 128:(kb + 1) * 128], in_=p_[64:96, 0:128])), r=[pb_], w=[b_krT])
                wukv, b_wukv = ar.alloc("c_wukv", 2 * 1024, BF16)
                wukv4 = wukv.rearrange("p (k h c) -> p k h c", k=2, h=NH)
                DG(("dma_start", C(out=wukv4, in_=w_ukv[l].rearrange("(k p) h c -> p k h c", p=128))), w=[b_wukv])
                for c0 in range(0, PAST, 512):
                    expand_kv_args = (ckvb3, b_ckvb, krT, b_krT, wukv4, b_wukv, c0, 512, SEQ + s * SKS + c0)
                    _expand_kv(P, ar, T, A, V, DS, gps, KT_scr, V_scr, b_KT, b_V, *expand_kv_args)

            for bi, blk in enumerate(blocks):
                seq, t0b, W, off = blk["seq"], blk["t0"], blk["W"], blk["off"]
                nt = (W + 127) // 128
                kbase = 0 if seq == 0 else SEQ + (seq - 1) * SKS
                kpos = t0b if seq == 0 else PAST
                phase(); ar.reset()
                DS(("dma_start", C(out=xT[:, :, 0:W], in_=xT_scr[:, :, off:off + W])), r=[b_xT], w=[b_x])
                tabs, b_tabs = ar.alloc("tabs", 4 * 512, F32)
                tabs3 = tabs.rearrange("p (a t) -> p a t", a=4)
                QT, b_QT = ar.alloc("QT", NH * 512, BF16)
                QT3 = QT.rearrange("p (h t) -> p h t", h=NH)
                poolT, b_poolT = ar.alloc("poolT", 4 * 512, BF16); pool3 = poolT.rearrange("p (k t) -> p k t", k=4)
                convT, b_convT = ar.alloc("convT", 4 * 512, BF16); conv3 = convT.rearrange("p (k t) -> p k t", k=4)
                markA = ar.off
                DS(("dma_start", C(out=tabs3[64:96, :, 0:W], in_=rope_tab[:, 64:96, off:off + W].rearrange("a p t -> p a t"))), w=[b_tabs])
                rstd, brs = rms_stats(lambda k: xT[:, k, 0:W], 8, D, W, [b_x], "n1")
                tmp, btmp = ar.alloc("n1_tmp", 2 * 512, F32)
                for k in range(8):
                    tk = tmp[:, (k % 2) * 512:(k % 2) * 512 + W]
                    V(("tensor_tensor", C(out=tk, in0=xT[:, k, 0:W], in1=rstd[:, 0:W], op=ALU.mult)), r=[b_x, brs], w=[btmp])
                    A(("activation", C(out=hT[:, k, 0:W], in_=tk, func=AF.Identity, scale=G1[:, k, seq:seq + 1], bias=modT[:, k, seq:seq + 1])),
                      r=[btmp, b_mod], w=[b_h])

                def proj_cols(c0, ncols, evac):
                    wt, bw = load_w(wbf_in, c0, ncols, 8, rb=[b_wscr])
                    for mi in range((ncols + 127) // 128):
                        m = min(128, ncols - mi * 128)
                        p_, pb_ = gps()
                        mm(p_[0:m, 0:W], pb_, [(wt[:, k, mi * 128:mi * 128 + m], hT[:, k, 0:W]) for k in range(8)], [bw, b_h])
                        evac(mi, p_, pb_)

                phase(); ar.off = markA
                cqT, b_cq = ar.alloc("cqT", 4 * 512, F32)
                cq3 = cqT.rearrange("p (k t) -> p k t", k=4)
                proj_cols(0, 512, lambda mi, p_, pb_: A(("copy", C(out=cq3[:, mi, 0:W], in_=p_[:, 0:W])), r=[pb_], w=[b_cq]))
                rq, brq = rms_stats(lambda k: cq3[:, k, 0:W], 4, 512, W, [b_cq], "nq")
                cqn, b_cqn = ar.alloc("cqn", 4 * 512, BF16)
                cqn3 = cqn.rearrange("p (k t) -> p k t", k=4)
                for k in range(4):
                    V(("scalar_tensor_tensor", C(out=cqn3[:, k, 0:W], in0=cq3[:, k, 0:W], scalar=vecs[:, 16 + k:17 + k], in1=rq[:, 0:W], op0=ALU.mult, op1=ALU.mult)),
                      r=[b_cq, brq, b_vecs], w=[b_cqn])
                wuq, b_wuq = ar.alloc("wuq", 4 * 768, BF16); wuqs, b_wuqs = ar.alloc("wuqs", 4 * 768, BF16)
                wuq4 = wuq.rearrange("p (k h c) -> p k h c", k=4, h=NH); wuqs4 = wuqs.rearrange("p (k h c) -> p k h c", k=4, h=NH)
                wsrc = w_uq[l].rearrange("(k p) h c -> p k h c", p=128)
                DG(("dma_start", C(out=wuq4, in_=wsrc)), w=[b_wuq])
                for k in range(4):
                    DG(("dma_start", C(out=wuqs4[:, k, :, 0:64], in_=wsrc[:, k, :, 0:64])), w=[b_wuqs])
                    DG(("dma_start", C(out=wuqs4[:, k, :, 64:80], in_=wsrc[:, k, :, 80:96])), w=[b_wuqs])
                    DG(("dma_start", C(out=wuqs4[:, k, :, 80:96], in_=wsrc[:, k, :, 64:80])), w=[b_wuqs])
                rtmp, b_rtmp = ar.alloc("rtmp", 512, F32)
                for h in range(NH):
                    p1, pb1 = gps()
                    mm(p1[0:96, 0:W], pb1, [(wuq4[:, k, h, :], cqn3[:, k, 0:W]) for k in range(4)], [b_wuq, b_cqn])
                    p2, pb2 = gps()
                    mm(p2[0:96, 0:W], pb2, [(wuqs4[:, k, h, :], cqn3[:, k, 0:W]) for k in range(4)], [b_wuqs, b_cqn])
                    A(("activation", C(out=QT3[0:64, h, 0:W], in_=p1[0:64, 0:W], func=AF.Identity, scale=ATTN_SCALE)), r=[pb1], w=[b_QT])
                    V(("tensor_tensor", C(out=rtmp[64:96, 0:W], in0=p1[64:96, 0:W], in1=tabs3[64:96, 0, 0:W], op=ALU.mult)), r=[pb1, b_tabs], w=[b_rtmp])
                    V(("tensor_tensor", C(out=QT3[64:96, h, 0:W], in0=p2[64:96, 0:W], in1=tabs3[64:96, 1, 0:W], op=ALU.mult)), r=[pb2, b_tabs], w=[b_QT])
                    V(("tensor_tensor", C(out=QT3[64:96, h, 0:W], in0=QT3[64:96, h, 0:W], in1=rtmp[64:96, 0:W], op=ALU.add)), r=[b_rtmp, b_QT], w=[b_QT])

                phase(); ar.off = markA
                rtmp, b_rtmp = ar.alloc("rtmp2", 512, F32)
                ckvT, b_ckv = ar.alloc("ckvT", 2 * 512, F32)
                ckv3 = ckvT.rearrange("p (k t) -> p k t", k=2)
                proj_cols(512, 256, lambda mi, p_, pb_: A(("copy", C(out=ckv3[:, mi, 0:W], in_=p_[:, 0:W])), r=[pb_], w=[b_ckv]))
                rk, brk = rms_stats(lambda k: ckv3[:, k, 0:W], 2, 256, W, [b_ckv], "nk")
                ckvb, b_ckvb = ar.alloc("ckvb", 2 * 512, BF16)
                ckvb3 = ckvb.rearrange("p (k t) -> p k t", k=2)
                for k in range(2):
                    V(("scalar_tensor_tensor", C(out=ckv3[:, k, 0:W], in0=ckv3[:, k, 0:W], scalar=vecs[:, 20 + k:21 + k], in1=rk[:, 0:W], op0=ALU.mult, op1=ALU.mult)),
                      r=[b_ckv, brk, b_vecs], w=[b_ckv])
                    A(("copy", C(out=ckvb3[:, k, 0:W], in_=ckv3[:, k, 0:W])), r=[b_ckv], w=[b_ckvb])
                kvdst = (lambda t0, n, a, b: kv_p[l, off + t0:off + t0 + n, a:b]) if seq == 0 else (lambda t0, n, a, b: kv_s[l, off - SEQ + t0:off - SEQ + t0 + n, a:b])
                transpose_out(lambda k: ckv3[:, k, 0:W], 2, W, kvdst, [b_ckv])

                krT, b_krT = ar.alloc("krT", 512, BF16)
                krF, b_krF = ar.alloc("krF", 512, F32)
                wkr, b_wkr = ar.alloc("wkr", 2 * 8 * 96, BF16)
                wkr4 = wkr.rearrange("p (a k c) -> p a k c", a=2, k=8)
                V(("memset", C(wkr[:], 0.0)), w=[b_wkr])
                wi = w_in[l].rearrange("(k p) c -> p k c", p=128)
                DG(("dma_start", C(out=wkr4[:, 0, :, 64:96], in_=wi[:, :, 768:800])), w=[b_wkr])
                DG(("dma_start", C(out=wkr4[:, 1, :, 64:80], in_=wi[:, :, 784:800])), w=[b_wkr])
                DG(("dma_start", C(out=wkr4[:, 1, :, 80:96], in_=wi[:, :, 768:784])), w=[b_wkr])
                p1, pb1 = gps()
                mm(p1[0:96, 0:W], pb1, [(wkr4[:, 0, k, :], hT[:, k, 0:W]) for k in range(8)], [b_wkr, b_h])
                p2, pb2 = gps()
                mm(p2[0:96, 0:W], pb2, [(wkr4[:, 1, k, :], hT[:, k, 0:W]) for k in range(8)], [b_wkr, b_h])
                V(("tensor_tensor", C(out=rtmp[64:96, 0:W], in0=p1[64:96, 0:W], in1=tabs3[64:96, 2, 0:W], op=ALU.mult)), r=[pb1, b_tabs], w=[b_rtmp])
                V(("tensor_tensor", C(out=krF[64:96, 0:W], in0=p2[64:96, 0:W], in1=tabs3[64:96, 3, 0:W], op=ALU.mult)), r=[pb2, b_tabs], w=[b_krF])
                V(("tensor_tensor", C(out=krF[64:96, 0:W], in0=krF[64:96, 0:W], in1=rtmp[64:96, 0:W], op=ALU.add)), r=[b_rtmp, b_krF], w=[b_krF])
                A(("copy", C(out=krT[64:96, 0:W], in_=krF[64:96, 0:W])), r=[b_krF], w=[b_krT])
                krdst = (lambda t0, n, a, b: kr_p[l, off + t0:off + t0 + n, a:b]) if seq == 0 else (lambda t0, n, a, b: kr_s[l, off - SEQ + t0:off - SEQ + t0 + n, a:b])
                transpose_out(lambda k: krF[64:96, 0:W], 1, W, krdst, [b_krF], rows_per_k=32, part0=64)

                wukv, b_wukv = ar.alloc("wukv", 2 * 1024, BF16)
                wukv4 = wukv.rearrange("p (k h c) -> p k h c", k=2, h=NH)
                DG(("dma_start", C(out=wukv4, in_=w_ukv[l].rearrange("(k p) h c -> p k h c", p=128))), w=[b_wukv])
                _expand_kv(P, ar, T, A, V, DS, gps, KT_scr, V_scr, b_KT, b_V, ckvb3, b_ckvb, krT, b_krT, wukv4, b_wukv, 0, W, kbase + kpos)

                phase(); ar.off = markA
                upe, b_upe = ar.alloc("upe", 4 * 528, F32); upe3 = upe.rearrange("p (k t) -> p k t", k=4)
                if t0b == 0:
                    if seq == 0:
                        V(("memset", C(halo_p[:], 0.0)), w=[b_halo_p])
                        V(("memset", C(halo_c[:], 0.0)), w=[b_halo_c])
                    else:
                        V(("memset", C(halo_p[:], 0.0)), w=[b_halo_p])
                        for k in range(4):
                            DS(("dma_start", C(out=halo_p[:, k, 1:16], in_=st_pool[l, seq - 1][:, k * 128:(k + 1) * 128].rearrange("t p -> p t"), allow_slow_non_contiguous=True)), w=[b_halo_p])
                            DS(("dma_start", C(out=halo_c[:, k, :], in_=st_conv[l, seq - 1][:, k * 128:(k + 1) * 128].rearrange("t p -> p t"), allow_slow_non_contiguous=True)), w=[b_halo_c])
                V(("tensor_copy", C(out=upe3[:, :, 0:16], in_=halo_p[:])), r=[b_halo_p], w=[b_upe])
                proj_cols(800, 512, lambda mi, p_, pb_: A(("copy", C(out=upe3[:, mi, 16:16 + W], in_=p_[:, 0:W])), r=[pb_], w=[b_upe]))
                V(("tensor_copy", C(out=halo_p[:], in_=upe3[:, :, W:W + 16])), r=[b_upe], w=[b_halo_p])
                pp, b_pp = ar.alloc("pp", 2 * 528, F32); pp3 = pp.rearrange("p (a t) -> p a t", a=2)
                dT, b_dT = ar.alloc("dT", 512, BF16)
                if seq == 0 and t0b == 0:
                    rc0t, b_rc0 = ar.alloc("rc0t", 64, F32); rc03 = rc0t.rearrange("p (k t) -> p k t", k=4)
                    DS(("dma_start", C(out=rc03, in_=rc0)), w=[b_rc0])
                for g, wdw in enumerate((2, 4, 8, 16)):
                    cur = upe3[:, g, :]
                    step, a = 1, 0
                    L = 16 + W
                    while step < wdw:
                        dst = pp3[:, a, :]
                        V(("tensor_tensor", C(out=dst[:, step:L], in0=cur[:, step:L], in1=cur[:, 0:L - step], op=ALU.add)),
                          r=[b_upe, b_pp], w=[b_pp])
                        cur = dst
                        step *= 2
                        a = 1 - a
                    V(("scalar_tensor_tensor", C(out=dT[:, 0:W], in0=cur[:, 16:16 + W], scalar=1.0 / wdw, in1=upe3[:, g, 16:16 + W], op0=ALU.mult, op1=ALU.subtract)),
                      r=[b_pp, b_upe], w=[b_dT])
                    if seq == 0 and t0b == 0:
                        V(("tensor_tensor", C(out=pp3[:, 1 - a if False else a, 0:16], in0=cur[:, 16:32], in1=rc03[:, g, :], op=ALU.mult)), r=[b_pp, b_rc0], w=[b_pp])
                        V(("tensor_tensor", C(out=dT[:, 0:16], in0=pp3[:, a, 0:16], in1=upe3[:, g, 16:32], op=ALU.subtract)), r=[b_pp, b_upe], w=[b_dT])
                    p_, pb_ = gps()
                    T(("matmul", C(p_[:, 0:W], lhsT=wpool[:, g, :], rhs=dT[:, 0:W], start=True, stop=True)), r=[b_wpool, b_dT], w=[pb_])
                    A(("activation", C(out=pool3[:, g, 0:W], in_=p_[:, 0:W], func=AF.Identity, scale=vecs[:, 22 + g:23 + g])), r=[pb_, b_vecs], w=[b_poolT])
                last_of_seq = (seq != 0) or (t0b + W == SEQ)
                if last_of_seq:
                    pdst = pool_p[l] if seq == 0 else pool_s[l, seq - 1]
                    p_, pb_ = gps()
                    for k in range(4):
                        T(("transpose", C(p_[0:15, k * 128:(k + 1) * 128], upe3[:, k, W + 1:W + 16], ident[:])), r=[b_upe, b_const], w=[pb_])
                    A(("copy", C(out=stg[0:15, 0:512], in_=p_[0:15, 0:512])), r=[pb_], w=[b_stg])
                    DS(("dma_start", C(out=pdst, in_=stg[0:15, 0:512])), r=[b_stg], w=[b_out])

                phase(); ar.off = markA
                uce, b_uce = ar.alloc("uce", 4 * 520, F32); uce3 = uce.rearrange("p (k t) -> p k t", k=4)
                bg, b_bg = ar.alloc("bg", 4 * 512, F32); bg3 = bg.rearrange("p (k t) -> p k t", k=4)
                cg, b_cg = ar.alloc("cg", 4 * 512, F32); cg3 = cg.rearrange("p (k t) -> p k t", k=4)
                V(("tensor_copy", C(out=uce3[:, :, 0:2], in_=halo_c[:])), r=[b_halo_c], w=[b_uce])
                proj_cols(1312, 512, lambda mi, p_, pb_: A(("copy", C(out=bg3[:, mi, 0:W], in_=p_[:, 0:W])), r=[pb_], w=[b_bg]))
                proj_cols(1824, 512, lambda mi, p_, pb_: A(("copy", C(out=cg3[:, mi, 0:W], in_=p_[:, 0:W])), r=[pb_], w=[b_cg]))
                proj_cols(2336, 512, lambda mi, p_, pb_: V(("tensor_tensor", C(out=uce3[:, mi, 2:2 + W], in0=p_[:, 0:W], in1=cg3[:, mi, 0:W], op=ALU.mult)), r=[pb_, b_cg], w=[b_uce]))
                V(("tensor_copy", C(out=halo_c[:], in_=uce3[:, :, W:W + 2])), r=[b_uce], w=[b_halo_c])
                for k in range(4):
                    yk = cg3[:, k, 0:W]
                    V(("tensor_scalar", C(out=yk, in0=uce3[:, k, 0:W], scalar1=vecs[:, 26 + k:27 + k], scalar2=None, op0=ALU.mult)), r=[b_uce, b_vecs, b_cg], w=[b_cg])
                    V(("scalar_tensor_tensor", C(out=yk, in0=uce3[:, k, 1:1 + W], scalar=vecs[:, 30 + k:31 + k], in1=yk, op0=ALU.mult, op1=ALU.add)), r=[b_uce, b_vecs, b_cg], w=[b_cg])
                    V(("scalar_tensor_tensor", C(out=yk, in0=uce3[:, k, 2:2 + W], scalar=vecs[:, 34 + k:35 + k], in1=yk, op0=ALU.mult, op1=ALU.add)), r=[b_uce, b_vecs, b_cg], w=[b_cg])
                    V(("tensor_tensor", C(out=conv3[:, k, 0:W], in0=yk, in1=bg3[:, k, 0:W], op=ALU.mult)), r=[b_cg, b_bg], w=[b_convT])
                if last_of_seq:
                    cdst = conv_p[l] if seq == 0 else conv_s[l, seq - 1]
                    p_, pb_ = gps()
                    for k in range(4):
                        T(("transpose", C(p_[0:2, k * 128:(k + 1) * 128], uce3[:, k, W:W + 2], ident[:])), r=[b_uce, b_const], w=[pb_])
                    A(("copy", C(out=stg2[0:2, 0:512], in_=p_[0:2, 0:512])), r=[pb_], w=[b_stg2])
                    DS(("dma_start", C(out=cdst, in_=stg2[0:2, 0:512])), r=[b_stg2], w=[b_out])

                phase()
                ar.off = markA
                mark = ar.off
                attnT, b_attn = ar.alloc("attnT", NH * 512, BF16); attn3 = attnT.rearrange("p (h t) -> p h t", h=NH)
                nkeys = (t0b + W) if seq == 0 else (PAST + TS)
                nkb = (nkeys + 127) // 128
                kb0 = kbase // 128
                KTh = []; Vh = []
                for i in range(2):
                    a_, b_ = ar.alloc("KTh%d" % i, nkb * 128, BF16); KTh.append((a_, b_))
                    a_, b_ = ar.alloc("Vh%d" % i, nkb * 65, BF16); Vh.append((a_.rearrange("p (k c) -> p k c", c=65), b_))
                PT = [ar.alloc("PT%d" % i, 512, BF16) for i in range(3)]
                osb, b_osb = ar.alloc("osb", 512, F32); rcs, b_rcs = ar.alloc("rcs", 512, F32)
                pti = 0
                for h in range(NH):
                    kt, bkt = KTh[h % 2]; vt, bvt = Vh[h % 2]
                    DS(("dma_start", C(out=kt[0:96, 0:nkeys], in_=KT_scr[h, :, kbase:kbase + nkeys])), r=[b_KT], w=[bkt])
                    DS(("dma_start", C(out=vt[:, 0:nkb, :], in_=V_scr[h, :, kb0:kb0 + nkb, :])), r=[b_V], w=[bvt])
                    po, pbo = ps[4 + h % 2], pb[4 + h % 2]
                    for kb in range(nkb):
                        kk = min(128, nkeys - kb * 128)
                        q0 = 0
                        diag = False
                        if seq == 0 and kb * 128 >= t0b:
                            q0 = kb * 128 - t0b
                            diag = True
                        nq = W - q0
                        p_, pb_ = gps()
                        T(("matmul", C(p_[0:kk, 0:nq], lhsT=kt[0:96, kb * 128:kb * 128 + kk], rhs=QT3[0:96, h, q0:W], start=True, stop=True)),
                          r=[bkt, b_QT], w=[pb_])
                        pt_, bpt_ = PT[pti]; pti = (pti + 1) % 3
                        A(("activation", C(out=pt_[0:kk, 0:nq], in_=p_[0:kk, 0:nq], func=AF.Exp)), r=[pb_], w=[bpt_])
                        if diag:
                            V(("memset", C(pt_[64:128, 0:64], 0.0)), w=[bpt_])
                        T(("matmul", C(po[0:65, q0:W], lhsT=vt[0:kk, kb, :], rhs=pt_[0:kk, 0:nq], start=(kb == 0), stop=(kb == nkb - 1))),
                          r=[bvt, bpt_], w=[pbo])
                    A(("copy", C(out=osb[0:65, 0:W], in_=po[0:65, 0:W])), r=[pbo], w=[b_osb])
                    p_, pb_ = gps()
                    T(("matmul", C(p_[0:64, 0:W], lhsT=sel65[0:65, :], rhs=osb[0:65, 0:W], start=True, stop=True)), r=[b_osb, b_const], w=[pb_])
                    V(("reciprocal", C(out=rcs[0:64, 0:W], in_=p_[0:64, 0:W])), r=[pb_], w=[b_rcs])
                    V(("tensor_tensor", C(out=attn3[0:64, h, 0:W], in0=osb[0:64, 0:W], in1=rcs[0:64, 0:W], op=ALU.mult)), r=[b_osb, b_rcs], w=[b_attn])

                phase()
                ar.off = mark + 1
                mixT, b_mix = ar.alloc("mixT", 8 * 512, BF16); mix3 = mixT.rearrange("p (k t) -> p k t", k=8)
                gall, b_gts = ar.alloc("gall", 24 * 512, F32); gall3 = gall.rearrange("p (c t) -> p c t", c=24)
                for gq in range(6):
                    proj_cols(2848 + gq * 512, 512, lambda mi, p_, pb_, gq=gq: A(("activation", C(out=gall3[:, gq * 4 + mi, 0:W], in_=p_[:, 0:W], func=AF.Sigmoid)), r=[pb_], w=[b_gts]))
                wba, b_wba = ar.alloc("wba", 8 * 128, BF16); wba3 = wba.rearrange("p (h c) -> p h c", h=NH)
                wbp, b_wbp = ar.alloc("wbp", 2 * 4 * 128, BF16); wbp4 = wbp.rearrange("p (n k c) -> p n k c", n=2, k=4)
                acc, b_acc = ar.alloc("acc", 512, F32)
                for m in range(8):
                    DG(("dma_start", C(out=wba3[0:64], in_=w_branch[l, 0].rearrange("(h p) c -> p h c", p=64)[:, :, m * 128:(m + 1) * 128])), w=[b_wba])
                    for n2 in range(2):
                        DG(("dma_start", C(out=wbp4[:, n2], in_=w_branch[l, 1 + n2].rearrange("(k p) c -> p k c", p=128)[:, :, m * 128:(m + 1) * 128])), w=[b_wbp])
                    gts3 = gall3[:, m::8, :]
                    for n in range(3):
                        p_, pb_ = gps()
                        if n == 0:
                            mm(p_[:, 0:W], pb_, [(wba3[0:64, h, :], attn3[0:64, h, 0:W]) for h in range(NH)], [b_wba, b_attn])
                        else:
                            src3, bsrc = (pool3, b_poolT) if n == 1 else (conv3, b_convT)
                            mm(p_[:, 0:W], pb_, [(wbp4[:, n - 1, k, :], src3[:, k, 0:W]) for k in range(4)], [b_wbp, bsrc])
                        if n == 0:
                            V(("tensor_tensor", C(out=acc[:, 0:W], in0=p_[:, 0:W], in1=gts3[:, 0, 0:W], op=ALU.mult)), r=[pb_, b_gts], w=[b_acc])
                        else:
                            V(("tensor_tensor", C(out=gts3[:, n, 0:W], in0=p_[:, 0:W], in1=gts3[:, n, 0:W], op=ALU.mult)), r=[pb_, b_gts], w=[b_gts])
                            if n == 1:
                                V(("tensor_tensor", C(out=acc[:, 0:W], in0=acc[:, 0:W], in1=gts3[:, 1, 0:W], op=ALU.add)), r=[b_gts, b_acc], w=[b_acc])
                            else:
                                V(("tensor_tensor", C(out=mix3[:, m, 0:W], in0=acc[:, 0:W], in1=gts3[:, 2, 0:W], op=ALU.add)), r=[b_gts, b_acc], w=[b_mix])
                for g in range(2):
                    wt, bw = load_w(wbf_out, g * 512, 512, 8, rb=[b_wscr])
                    for mi in range(4):
                        m = g * 4 + mi
                        p_, pb_ = gps()
                        mm(p_[:, 0:W], pb_, [(wt[:, k, mi * 128:(mi + 1) * 128], mix3[:, k, 0:W]) for k in range(8)], [bw, b_mix])
                        V(("scalar_tensor_tensor", C(out=xT[:, m, 0:W], in0=p_[:, 0:W], scalar=modT[:, 16 + m, seq:seq + 1], in1=xT[:, m, 0:W], op0=ALU.mult, op1=ALU.add)),
                          r=[pb_, b_mod, b_x], w=[b_x])

                phase(); ar.reset()
                rstd, brs = rms_stats(lambda k: xT[:, k, 0:W], 8, D, W, [b_x], "n2")
                tmp, btmp = ar.alloc("n2_tmp", 2 * 512, F32)
                for k in range(8):
                    tk = tmp[:, (k % 2) * 512:(k % 2) * 512 + W]
                    V(("tensor_tensor", C(out=tk, in0=xT[:, k, 0:W], in1=rstd[:, 0:W], op=ALU.mult)), r=[b_x, brs], w=[btmp])
                    A(("activation", C(out=hT[:, k, 0:W], in_=tk, func=AF.Identity, scale=G2[:, k, seq:seq + 1], bias=modT[:, 24 + k, seq:seq + 1])),
                      r=[btmp, b_mod], w=[b_h])
                for sb0 in range(0, W, 256):
                    SW = min(256, W - sb0)
                    phase(); ar.reset()
                    GT, b_GT = ar.alloc("GT", 128 * 256, BF16); GT3 = GT.rearrange("p (i n) -> p i n", i=128)
                    qT, b_qT = ar.alloc("qT", 16 * 256, BF16); qT3 = qT.rearrange("p (m t) -> p m t", m=16)
                    for g in range(4):
                        wt, bw = load_w(wbf_pq, g * 512, 512, 8, rb=[b_wscr])
                        for mi in range(4):
                            p_, pb_ = gps()
                            mm(p_[:, 0:SW], pb_, [(wt[:, k, mi * 128:(mi + 1) * 128], hT[:, k, sb0:sb0 + SW]) for k in range(8)], [bw, b_h])
                            A(("copy", C(out=qT3[:, g * 4 + mi, 0:SW], in_=p_[:, 0:SW])), r=[pb_], w=[b_qT])
                    mark_t = ar.off
                    for tt in range(0, SW, 128):
                        n = min(128, SW - tt)
                        phase(); ar.off = mark_t
                        _peer_topk(P, ar, T, A, V, G, gps, ps, pb, n, tt, qT3, b_qT, kT12, b_kT12, ident, iot, b_const, GT3, b_GT)
                    phase(); ar.off = mark_t
                    NUV = 4
                    uvt = [ar.alloc("uvt%d" % i, 2048, BF16) for i in range(NUV)]
                    gl = [ar.alloc("gl%d" % i, 256, F32) for i in range(2)]
                    wT = [ar.alloc("wT%d" % i, 256, BF16) for i in range(2)]
                    osb2, b_osb2 = ar.alloc("osb2", 1024, F32)
                    nts = (SW + 127) // 128

                    def stage2(i):
                        uv_, buv_ = uvt[i % NUV]; w_, bw_ = wT[i % 2]
                        v_ = uv_[:, D:2 * D]
                        for ti in range(nts):
                            n = min(128, SW - ti * 128)
                            for hf in range(2):
                                bk = 4 + ti * 2 + hf
                                T(("matmul", C(ps[bk][0:n, 0:512], lhsT=w_[:, ti * 128:ti * 128 + n], rhs=v_[:, hf * 512:(hf + 1) * 512],
                                               start=(i == 0), stop=(i == 127))), r=[buv_, bw_], w=[pb[bk]])

                    for i in range(128):
                        uv_, buv_ = uvt[i % NUV]; g_, bg_ = gl[i % 2]; w_, bw_ = wT[i % 2]
                        uT_ = uv_[:, 0:D]
                        DS(("dma_start", C(out=uv_, in_=uv_scr[i])), r=[b_uvs], w=[buv_], slot="uvt%d" % (i % NUV))
                        p_, pb_ = gps()
                        mm(p_[:, 0:SW], pb_, [(uT_[:, k * 128:(k + 1) * 128], hT[:, k, sb0:sb0 + SW]) for k in range(8)], [buv_, b_h])
                        A(("activation", C(out=g_[:, 0:SW], in_=p_[:, 0:SW], func=AF.Gelu)), r=[pb_], w=[bg_])
                        V(("tensor_tensor", C(out=w_[:, 0:SW], in0=g_[:, 0:SW], in1=GT3[:, i, 0:SW], op=ALU.mult)), r=[bg_, b_GT], w=[bw_])
                        if i >= 1:
                            stage2(i - 1)
                    stage2(127)
                    for ti in range(nts):
                        n = min(128, SW - ti * 128)
                        for hf in range(2):
                            bk = 4 + ti * 2 + hf
                            A(("copy", C(out=osb2[0:n, hf * 512:(hf + 1) * 512], in_=ps[bk][0:n, 0:512])), r=[pb[bk]], w=[b_osb2])
                        for k in range(8):
                            p_, pb_ = gps()
                            T(("transpose", C(p_[:, 0:n], osb2[0:n, k * 128:(k + 1) * 128], ident[0:n, 0:n])), r=[b_osb2, b_const], w=[pb_])
                            c0 = sb0 + ti * 128
                            V(("scalar_tensor_tensor", C(out=xT[:, k, c0:c0 + n], in0=p_[:, 0:n], scalar=modT[:, 40 + k, seq:seq + 1],
                                                                                    in1=xT[:, k, c0:c0 + n], op0=ALU.mult, op1=ALU.add)), r=[pb_, b_mod, b_x], w=[b_x])

                if l < DEPTH - 1:
                    DS(("dma_start", C(out=xT_scr[:, :, off:off + W], in_=xT[:, :, 0:W])), r=[b_x], w=[b_xT])
                else:
                    phase(); ar.reset()
                    rstd, brs = rms_stats(lambda k: xT[:, k, 0:W], 8, D, W, [b_x], "nf")
                    yT, b_yT = ar.alloc("yT", 8 * 512, F32); yT3 = yT.rearrange("p (k t) -> p k t", k=8)
                    for k in range(8):
                        V(("scalar_tensor_tensor", C(out=yT3[:, k, 0:W], in0=xT[:, k, 0:W], scalar=vecs[:, 38 + k:39 + k], in1=rstd[:, 0:W], op0=ALU.mult, op1=ALU.mult)),
                          r=[b_x, brs, b_vecs], w=[b_yT])
                    ydst = (lambda t0, n, a, b: y_p[off + t0:off + t0 + n, a:b]) if seq == 0 else (lambda t0, n, a, b: y_s[off - SEQ + t0:off - SEQ + t0 + n, a:b])
                    transpose_out(lambda k: yT3[:, k, 0:W], 8, W, ydst, [b_yT])

        P.barrier()
        ar.reset()
        while ps_ctx:
            ps_ctx.pop().__exit__(None, None, None)
        P.emit()
    return nc


def _expand_kv(P, ar, T, A, V, DS, gps, KT_scr, V_scr, b_KT, b_V, ckvb3, b_ckvb, krT, b_krT, wukv4, b_wukv, c0, W, kslot):
    P.barrier()
    mark = ar.off
    KTb, b_KTb = ar.alloc("KTb", NH * 512, BF16); KTb3 = KTb.rearrange("p (h t) -> p h t", h=NH)
    Vb, b_Vb = ar.alloc("Vb", 4 * NH * 65, BF16); Vb4 = Vb.rearrange("p (t h c) -> p t h c", t=4, h=NH)
    V(("memset", C(Vb[:], 1.0)), w=[b_Vb])
    for h in range(NH):
        p_, pb_ = gps()
        for k in range(2):
            T(("matmul", C(p_[0:64, 0:W], lhsT=wukv4[:, k, h, 0:64], rhs=ckvb3[:, k, c0:c0 + W], start=(k == 0), stop=(k == 1))), r=[b_wukv, b_ckvb], w=[pb_])
        A(("copy", C(out=KTb3[0:64, h, 0:W], in_=p_[0:64, 0:W])), r=[pb_], w=[b_KTb])
        V(("tensor_copy", C(out=KTb3[64:96, h, 0:W], in_=krT[64:96, c0:c0 + W])), r=[b_krT], w=[b_KTb])
    DS(("dma_start", C(out=KT_scr[:, :, kslot:kslot + W].rearrange("h p t -> p h t"), in_=KTb3[0:96, :, 0:W])), r=[b_KTb], w=[b_KT])
    nt = (W + 127) // 128
    for t in range(nt):
        n = min(128, W - t * 128)
        p_, pb_ = gps()
        for k in range(2):
            T(("matmul", C(p_[0:n, 0:512].rearrange("p (h c) -> p h c", h=NH), lhsT=ckvb3[:, k, c0 + t * 128:c0 + t * 128 + n], rhs=wukv4[:, k, :, 64:128],
                                                       start=(k == 0), stop=(k == 1))), r=[b_wukv, b_ckvb], w=[pb_])
        A(("copy", C(out=Vb4[0:n, t, :, 0:64], in_=p_[0:n, 0:512].rearrange("p (h c) -> p h c", h=NH))), r=[pb_], w=[b_Vb])
    kb0 = kslot // 128
    p0 = kslot % 128
    if p0 == 0:
        full = W // 128
        if full > 0:
            for h in range(NH):
                DS(("dma_start", C(out=V_scr[h, :, kb0:kb0 + full, :], in_=Vb4[:, 0:full, h, :])), r=[b_Vb], w=[b_V])
        rem = W - full * 128
        if rem > 0:
            DS(("dma_start", C(out=V_scr[:, 0:rem, kb0 + full, :].rearrange("h p c -> p h c"), in_=Vb4[0:rem, full, :, :])), r=[b_Vb], w=[b_V])
    else:
        assert p0 + W <= 128
        DS(("dma_start", C(out=V_scr[:, p0:p0 + W, kb0, :].rearrange("h p c -> p h c"), in_=Vb4[0:W, 0, :, :])), r=[b_Vb], w=[b_V])
    ar.off = mark


def _peer_topk(P, ar, T, A, V, G, gps, ps, pb, n, tt, qT3, b_qT, kT12, b_kT12, ident, iot, b_const, GT3, b_GT):
    SS, bSS = ar.alloc("SS", 2048, F32)
    S2, bS2 = ar.alloc("S2", 1024, F32); S23 = S2.rearrange("p (h m) -> p h m", h=NH)
    cand, bc = ar.alloc("cand", 2048, F32); cand4 = cand.rearrange("p (h a b) -> p h a b", h=NH, a=16)
    oh, boh = ar.alloc("oh", 2048, F32); oh4 = oh.rearrange("p (h k a) -> p h k a", h=NH, k=16)
    S = []
    for s2 in range(2):
        s_ = SS[:, s2 * 1024:(s2 + 1) * 1024]
        s3 = s_.rearrange("p (h m) -> p h m", h=NH)
        for half in range(2):
            p_, pb_ = gps()
            for hh in range(4):
                h = half * 4 + hh
                T(("matmul", C(p_[0:n, hh * 128:(hh + 1) * 128], lhsT=qT3[:, 2 * h + s2, tt:tt + n], rhs=kT12[:, s2, :], start=True, stop=True)),
                  r=[b_qT, b_kT12], w=[pb_])
            A(("copy", C(out=s_[0:n, half * 512:(half + 1) * 512], in_=p_[0:n, 0:512])), r=[pb_], w=[bSS])
        S.append(s3)
    V16 = []; I16 = []
    for s2 in range(2):
        v_, bv_ = ar.alloc("V16_%d" % s2, 128, F32); i_, bi_ = ar.alloc("I16_%d" % s2, 128, U32); if_, bif_ = ar.alloc("I16f_%d" % s2, 128, F32)
        v3 = v_.rearrange("p (h k) -> p h k", h=NH); i3 = i_.rearrange("p (h k) -> p h k", h=NH)
        s3 = S[s2]
        for h in range(NH):
            V(("max", C(out=v3[0:n, h, 0:8], in_=s3[0:n, h, :])), r=[bSS], w=[bv_])
            V(("match_replace", C(out=S23[0:n, h, :], in_to_replace=v3[0:n, h, 0:8], in_values=s3[0:n, h, :], imm_value=NEG)), r=[bSS, bv_], w=[bS2])
            V(("max", C(out=v3[0:n, h, 8:16], in_=S23[0:n, h, :])), r=[bS2], w=[bv_])
            V(("max_index", C(out=i3[0:n, h, 0:8], in_max=v3[0:n, h, 0:8], in_values=s3[0:n, h, :])), r=[bSS, bv_], w=[bi_])
            V(("max_index", C(out=i3[0:n, h, 8:16], in_max=v3[0:n, h, 8:16], in_values=S23[0:n, h, :])), r=[bS2, bv_], w=[bi_])
        V(("tensor_copy", C(out=if_[0:n, :], in_=i_[0:n, :])), r=[bi_], w=[bif_])
        V16.append((v3, bv_)); I16.append((if_.rearrange("p (h k) -> p h k", h=NH), bif_))
    V(("tensor_tensor", C(out=cand4[0:n], in0=V16[0][0][0:n].unsqueeze(3).to_broadcast([n, NH, 16, 16]), in1=V16[1][0][0:n].unsqueeze(2).to_broadcast([n, NH, 16, 16]), op=ALU.add)),
      r=[V16[0][1], V16[1][1]], w=[bc])
    P.barrier()
    bc2 = Buf("cand2")
    SC, bSC = ar.alloc("SC", 128, F32); SC3 = SC.rearrange("p (h k) -> p h k", h=NH)
    SEL, bSEL = ar.alloc("SEL", 128, U32); SEL3 = SEL.rearrange("p (h k) -> p h k", h=NH)
    candf = cand.rearrange("p (h c) -> p h c", h=NH); cand2f = SS.rearrange("p (h c) -> p h c", h=NH)
    for h in range(NH):
        V(("max", C(out=SC3[0:n, h, 0:8], in_=candf[0:n, h, :])), r=[bc], w=[bSC])
        V(("match_replace", C(out=cand2f[0:n, h, :], in_to_replace=SC3[0:n, h, 0:8], in_values=candf[0:n, h, :], imm_value=NEG)), r=[bc, bSC], w=[bc2])
        V(("max", C(out=SC3[0:n, h, 8:16], in_=cand2f[0:n, h, :])), r=[bc2], w=[bSC])
        V(("max_index", C(out=SEL3[0:n, h, 0:8], in_max=SC3[0:n, h, 0:8], in_values=candf[0:n, h, :])), r=[bc, bSC], w=[bSEL])
        V(("max_index", C(out=SEL3[0:n, h, 8:16], in_max=SC3[0:n, h, 8:16], in_values=cand2f[0:n, h, :])), r=[bc2, bSC], w=[bSEL])
    AB = []
    for which in range(2):
        u_, bu_ = ar.alloc("abu%d" % which, 128, U32); f_, bf_ = ar.alloc("abf%d" % which, 128, F32)
        if which == 0:
            V(("tensor_scalar", C(out=u_[0:n, :], in0=SEL[0:n, :], scalar1=4, scalar2=None, op0=ALU.logical_shift_right)), r=[bSEL], w=[bu_])
        else:
            V(("tensor_scalar", C(out=u_[0:n, :], in0=SEL[0:n, :], scalar1=15, scalar2=None, op0=ALU.bitwise_and)), r=[bSEL], w=[bu_])
        V(("tensor_copy", C(out=f_[0:n, :], in_=u_[0:n, :])), r=[bu_], w=[bf_])
        AB.append((f_.rearrange("p (h k) -> p h k", h=NH), bf_))
    P.barrier()
    bio = Buf("io16")
    io16 = cand; io4 = io16.rearrange("p (h k a) -> p h k a", h=NH, k=16)
    G(("iota", C(io16[:], [[0, 128], [1, 16]], base=0, channel_multiplier=0, allow_small_or_imprecise_dtypes=True)), w=[bio])
    slots = []
    for which in range(2):
        sl_, bsl_ = ar.alloc("slot%d" % which, 128, F32)
        ab3, bab = AB[which]; i3f, bif = I16[which]
        V(("tensor_tensor", C(out=oh4[0:n], in0=io4[0:n], in1=ab3[0:n].unsqueeze(3).to_broadcast([n, NH, 16, 16]), op=ALU.is_equal)), r=[bio, bab], w=[boh])
        V(("tensor_tensor", C(out=oh4[0:n], in0=oh4[0:n], in1=i3f[0:n].unsqueeze(2).to_broadcast([n, NH, 16, 16]), op=ALU.mult)), r=[boh, bif], w=[boh])
        V(("tensor_reduce", C(out=sl_[0:n, :], in_=oh.rearrange("p (s a) -> p s a", a=16)[0:n], axis=AX.X, op=ALU.add)), r=[boh], w=[bsl_])
        slots.append((sl_, bsl_))
    wg, bwg = ar.alloc("wg", 128, F32); wg3 = wg.rearrange("p (h k) -> p h k", h=NH)
    zs, bzs = ar.alloc("zs", 8, F32)
    V(("tensor_tensor", C(out=wg3[0:n], in0=SC3[0:n], in1=SC3[0:n, :, 0:1].to_broadcast([n, NH, 16]), op=ALU.subtract)), r=[bSC], w=[bwg])
    A(("activation", C(out=wg[0:n, :], in_=wg[0:n, :], func=AF.Exp)), r=[bwg], w=[bwg])
    V(("tensor_reduce", C(out=zs[0:n, :], in_=wg3[0:n], axis=AX.X, op=ALU.add)), r=[bwg], w=[bzs])
    V(("reciprocal", C(out=zs[0:n, :], in_=zs[0:n, :])), r=[bzs], w=[bzs])
    V(("tensor_tensor", C(out=wg3[0:n], in0=wg3[0:n], in1=zs[0:n, :].unsqueeze(2).to_broadcast([n, NH, 16]), op=ALU.mult)), r=[bwg, bzs], w=[bwg])
    tr, btr = ar.alloc("trn", 3 * 128, F32); tr3 = tr.rearrange("p (a t) -> p a t", a=3)
    for a, (src, bsrc) in enumerate((slots[0], slots[1], (wg, bwg))):
        p_, pb_ = gps()
        T(("transpose", C(p_[:, 0:n], src[0:n, :], ident[0:n, 0:n])), r=[bsrc, b_const], w=[pb_])
        A(("copy", C(out=tr3[:, a, 0:n], in_=p_[:, 0:n])), r=[pb_], w=[btr])
    A4 = [ar.alloc("A4_%d" % i, 512, BF16) for i in range(2)]
    B4 = [ar.alloc("B4_%d" % i, 512, BF16) for i in range(2)]
    gi = 0
    for t4 in range(0, n, 4):
        pg, pbg = ps[6 + gi % 2], pb[6 + gi % 2]
        a_, ba_ = A4[gi % 2]; b_, bb_ = B4[gi % 2]
        a3 = a_.rearrange("p (t i) -> p t i", t=4); b3 = b_.rearrange("p (t i) -> p t i", t=4)
        gi += 1
        m4 = min(4, n - t4)
        iob = iot[:, :].unsqueeze(1).to_broadcast([128, m4, 128])
        V(("tensor_tensor", C(out=a3[:, 0:m4, :], in0=iob, in1=tr3[:, 0, t4:t4 + m4].unsqueeze(2).to_broadcast([128, m4, 128]), op=ALU.is_equal)), r=[btr, b_const], w=[ba_])
        V(("tensor_tensor", C(out=a3[:, 0:m4, :], in0=a3[:, 0:m4, :], in1=tr3[:, 2, t4:t4 + m4].unsqueeze(2).to_broadcast([128, m4, 128]), op=ALU.mult)), r=[btr, ba_], w=[ba_])
        V(("tensor_tensor", C(out=b3[:, 0:m4, :], in0=iob, in1=tr3[:, 1, t4:t4 + m4].unsqueeze(2).to_broadcast([128, m4, 128]), op=ALU.is_equal)), r=[btr, b_const], w=[bb_])
        for j in range(m4):
            T(("matmul", C(pg[:, j * 128:(j + 1) * 128], lhsT=b3[:, j, :], rhs=a3[:, j, :], start=True, stop=True)), r=[ba_, bb_], w=[pbg])
        A(("copy", C(out=GT3[:, :, tt + t4:tt + t4 + m4].rearrange("p i n -> p n i"), in_=pg[:, 0:m4 * 128].rearrange("p (n i) -> p n i", i=128))), r=[pbg], w=[b_GT])


_CACHE = {}


def _rope_tables(SEQ):
    NTOK = SEQ + NSS * TS
    half = 16
    inv = (10000.0 ** (-np.arange(half, dtype=np.float32) / half)).astype(np.float32)
    pos = np.concatenate([np.arange(SEQ, dtype=np.float32)] + [PAST + np.arange(TS, dtype=np.float32)] * NSS).astype(np.float32)
    ang = (pos[None, :] * inv[:, None]).astype(np.float32)
    cos = np.cos(ang).astype(np.float32); sin = np.sin(ang).astype(np.float32)
    cosf = np.concatenate([cos, cos], 0); sins = np.concatenate([-sin, sin], 0)
    tab = np.zeros((4, 96, NTOK), np.float32)
    sc = np.float32(ATTN_SCALE)
    tab[0, 64:96] = cosf * sc; tab[1, 64:96] = sins * sc
    tab[2, 64:96] = cosf; tab[3, 64:96] = sins
    rc0 = np.zeros((128, 4, 16), np.float32)
    for g, w in enumerate((2, 4, 8, 16)):
        rc0[:, g, :] = 1.0 / np.minimum(np.arange(16) + 1, w)
    return tab, rc0


def kernel(x_prompt, x_sample, cache_kv_latent, cache_k_rope, state_pool, state_conv, c_prompt, c_sample,
           w_ada, b_ada, g_mix, w_in, g_q, w_uq, g_kv, w_ukv, w_pool, pool_scale, conv_w, w_branch, w_out,
           g_ffn, peer_wq, peer_keys, peer_u, peer_v, g_final):
    f = lambda a: np.ascontiguousarray(np.asarray(a, dtype=np.float32))
    x_prompt = f(x_prompt); x_sample = f(x_sample)
    B, SEQ, _ = x_prompt.shape
    DB = x_sample.shape[0]
    ncores = DB // NSS
    assert ncores == 8 and x_sample.shape[1] == TS
    if SEQ not in _CACHE:
        _CACHE[SEQ] = build_program(SEQ)
    nc = _CACHE[SEQ]
    tab, rc0 = _rope_tables(SEQ)
    ckv = f(cache_kv_latent); krc = f(cache_k_rope); sp = f(state_pool); scv = f(state_conv)
    cp = f(c_prompt); cs = f(c_sample)
    shared = {"w_ada": f(w_ada), "b_ada": f(b_ada), "g_mix": f(g_mix), "w_in": f(w_in), "g_q": f(g_q), "w_uq": f(w_uq),
              "g_kv": f(g_kv), "w_ukv": f(w_ukv), "w_pool": f(w_pool), "pool_scale": f(pool_scale), "conv_w": f(conv_w),
              "w_branch": f(w_branch), "w_out": f(w_out), "g_ffn": f(g_ffn), "peer_wq": f(peer_wq).reshape(DEPTH, D, NH * 256),
              "peer_keys": f(peer_keys), "peer_u": f(peer_u), "peer_v": f(peer_v), "g_final": f(g_final),
              "rope_tab": tab, "rc0": rc0}
    in_maps = []
    for c in range(ncores):
        b = c % B
        sl = slice(NSS * c, NSS * c + NSS)
        m = dict(shared)
        m["x_p"] = x_prompt[b]
        m["x_s"] = np.ascontiguousarray(x_sample[sl].reshape(NSS * TS, D))
        m["c_all"] = np.ascontiguousarray(np.concatenate([cp[b:b + 1], cs[sl]], 0))
        m["ckv_c"] = np.ascontiguousarray(ckv[:, sl]); m["kr_c"] = np.ascontiguousarray(krc[:, sl])
        m["st_pool"] = np.ascontiguousarray(sp[:, sl]); m["st_conv"] = np.ascontiguousarray(scv[:, sl])
        in_maps.append(m)
    res = run_bass_kernel_spmd(nc, in_maps, core_ids=list(range(ncores))).results
    y_prompt = np.stack([res[b]["y_p"] for b in range(B)], 0)
    y_sample = np.concatenate([res[c]["y_s"].reshape(NSS, TS, D) for c in range(ncores)], 0)
    p_kv = np.stack([res[b]["kv_p"] for b in range(B)], 1)
    p_kr = np.stack([res[b]["kr_p"] for b in range(B)], 1)
    p_pool = np.stack([res[b]["pool_p"] for b in range(B)], 1)
    p_conv = np.stack([res[b]["conv_p"] for b in range(B)], 1)
    s_kv = np.concatenate([res[c]["kv_s"].reshape(DEPTH, NSS, TS, 256) for c in range(ncores)], 1)
    s_kr = np.concatenate([res[c]["kr_s"].reshape(DEPTH, NSS, TS, 32) for c in range(ncores)], 1)
    s_pool = np.concatenate([res[c]["pool_s"] for c in range(ncores)], 1)
    s_conv = np.concatenate([res[c]["conv_s"] for c in range(ncores)], 1)
    return (y_prompt, y_sample, p_kv, p_kr, p_pool, p_conv, s_kv, s_kr, s_pool, s_conv)
```
